# Optimizing a Trainium2 kernel written in Bass

```python
import math
import jax, jax.numpy as jnp
from jax import lax
import numpy as np

D_MODEL = 1024
BATCH = 8
SEQ = 4096
DEPTH = 2

N_MIXERS = 4
HEADS_PER_GROUP = 4
GROUP_WIDTH = D_MODEL // N_MIXERS
HD = GROUP_WIDTH // HEADS_PER_GROUP
MIX_WIDTH = N_MIXERS * GROUP_WIDTH

NSA_HEADS = HEADS_PER_GROUP
NSA_CMP_LEN = 32
NSA_CMP_STRIDE = 16
NSA_CMP_HIDDEN = 4 * HD
NSA_SEL_LEN = 64
NSA_TOPK = 16
NSA_WINDOW = 512

DIFF_HEADS = HEADS_PER_GROUP
DIFF_QK_DIM = HD // 2
DIFF_V_DIM = HD

GLA_HEADS = HEADS_PER_GROUP
GLA_DK = HD // 2
GLA_DV = HD
GLA_GATE_RANK = 16
GLA_TAU = 16.0

HG_HEADS = HEADS_PER_GROUP
HG_DK = HD
HG_DV = HD

CHUNK = 64
Q_BLOCK = 128

D_FF = 7 * D_MODEL // 2
N_EXPERTS = 8
TOP_K = 2
D_FF_EXPERT = 7 * D_MODEL // 2
PLE_DIM = 256
EPS = 1e-6
NEG = -1e30
BIG = 1e30

IN_SPLITS = (
    NSA_HEADS * HD, HD, HD, HD, HD, HD, HD, NSA_HEADS * 3,
    DIFF_HEADS * 2 * DIFF_QK_DIM, DIFF_HEADS * 2 * DIFF_QK_DIM, DIFF_HEADS * DIFF_V_DIM,
    GLA_HEADS * GLA_DK, GLA_HEADS * GLA_DK, GLA_HEADS * GLA_DV, GLA_GATE_RANK, GLA_HEADS * GLA_DV,
    HG_HEADS * HG_DK, HG_HEADS * HG_DK, HG_HEADS * HG_DV, HG_HEADS * HG_DV,
)
IN_COLS = sum(IN_SPLITS)

kernel_name = 'hybrid_nsa_diff_gla_hgrn2_moe_block'


def rms_norm(x, gain):
    xf = x.astype(jnp.float32)
    y = xf * lax.rsqrt(jnp.mean(xf * xf, axis=-1, keepdims=True) + EPS)
    return (y * gain.astype(jnp.float32)).astype(x.dtype)


def split_cols(u, sizes):
    offsets = np.cumsum(sizes)[:-1].tolist()
    return jnp.split(u, offsets, axis=-1)


def chunked_gated_linear_attention(q, k, v, log_g):
    B, S, H, DK = q.shape
    DV = v.shape[-1]
    n_chunks = S // CHUNK

    def to_chunks(t):
        return t.astype(jnp.float32).reshape(B, n_chunks, CHUNK, H, -1).transpose(1, 0, 3, 2, 4)

    qc, kc, vc = to_chunks(q), to_chunks(k), to_chunks(v)
    bc = jnp.cumsum(to_chunks(log_g), axis=3)
    causal = np.tril(np.ones((CHUNK, CHUNK), dtype=bool))[None, None, :, :, None]

    def step(state, inp):
        q_, k_, v_, b_ = inp
        o_inter = jnp.einsum('bhik,bhkv->bhiv', q_ * jnp.exp(b_), state)
        rel = jnp.where(causal, b_[:, :, :, None, :] - b_[:, :, None, :, :], NEG)
        attn = jnp.einsum('bhik,bhjk,bhijk->bhij', q_, k_, jnp.exp(rel))
        o_intra = jnp.einsum('bhij,bhjv->bhiv', attn, v_)
        b_last = b_[:, :, -1:, :]
        new_state = (jnp.exp(b_last[:, :, 0, :, None]) * state
                     + jnp.einsum('bhjk,bhjv->bhkv', k_ * jnp.exp(b_last - b_), v_))
        return new_state, o_inter + o_intra

    state0 = jnp.zeros((B, H, DK, DV), jnp.float32)
    _, o = lax.scan(step, state0, (qc, kc, vc, bc))
    return o.transpose(1, 0, 3, 2, 4).reshape(B, S, H, DV).astype(v.dtype)


def nsa_attention(q, k_cmp, v_cmp, k_slc, v_slc, k_win, v_win, gate_logits,
                  cmp_pos, cmp_w1, cmp_w2, qk_gain):
    B, S, H, _ = q.shape
    scale = HD ** -0.5
    q = rms_norm(q, qk_gain[0])
    t_pos = np.arange(S)

    n_cmp = (S - NSA_CMP_LEN) // NSA_CMP_STRIDE + 1
    cmp_idx = np.arange(n_cmp)[:, None] * NSA_CMP_STRIDE + np.arange(NSA_CMP_LEN)[None, :]

    def compress(t, j):
        blocks = t[:, cmp_idx] + cmp_pos[j]
        hidden = jax.nn.silu(blocks.reshape(B, n_cmp, NSA_CMP_LEN * HD) @ cmp_w1[j])
        return hidden @ cmp_w2[j]

    kc = rms_norm(compress(k_cmp, 0), qk_gain[1])
    vc = compress(v_cmp, 1)
    cmp_ok = cmp_idx[:, -1][None, :] <= t_pos[:, None]
    s_c = jnp.einsum('bthd,bnd->bhtn', q, kc).astype(jnp.float32) * scale
    p_c = jax.nn.softmax(jnp.where(cmp_ok, s_c, NEG), axis=-1) * cmp_ok
    o_c = jnp.einsum('bhtn,bnd->bthd', p_c.astype(vc.dtype), vc)

    n_sel = S // NSA_SEL_LEN
    k_sel = min(NSA_TOPK, n_sel)
    sel_start = np.arange(n_sel) * NSA_SEL_LEN
    cmp_start = cmp_idx[:, 0]
    overlap = ((cmp_start[:, None] <= sel_start[None, :] + NSA_SEL_LEN - 1)
               & (cmp_start[:, None] + NSA_CMP_LEN - 1 >= sel_start[None, :])).astype(np.float32)
    importance = jnp.einsum('bhtn,nm->btm', p_c, overlap)
    blk = np.arange(n_sel)[None, :]
    cur = (t_pos // NSA_SEL_LEN)[:, None]
    forced = (blk == 0) | (blk == cur) | (blk == cur - 1)
    importance = jnp.where(forced, BIG,
                           jnp.where(sel_start[None, :] <= t_pos[:, None], importance, NEG))
    _, sel = lax.top_k(lax.stop_gradient(importance), k_sel)

    kb = rms_norm(k_slc, qk_gain[2]).reshape(B, n_sel, NSA_SEL_LEN, HD)
    vb = v_slc.reshape(B, n_sel, NSA_SEL_LEN, HD)
    kw_pad = jnp.pad(rms_norm(k_win, qk_gain[3]), ((0, 0), (NSA_WINDOW, 0), (0, 0)))
    vw_pad = jnp.pad(v_win, ((0, 0), (NSA_WINDOW, 0), (0, 0)))
    band = NSA_WINDOW + Q_BLOCK
    gather_blocks = jax.vmap(lambda t, i: t[i])

    def block_body(args):
        qb, selb, t0 = args
        tq = t0 + jnp.arange(Q_BLOCK)
        kg = gather_blocks(kb, selb)
        vg = gather_blocks(vb, selb)
        s = jnp.einsum('bqhd,bqnld->bhqnl', qb, kg).astype(jnp.float32) * scale
        kpos = selb[..., None] * NSA_SEL_LEN + jnp.arange(NSA_SEL_LEN)
        s = jnp.where((kpos <= tq[None, :, None, None])[:, None], s, NEG)
        p = jax.nn.softmax(s.reshape(B, H, Q_BLOCK, k_sel * NSA_SEL_LEN), axis=-1).reshape(s.shape)
        o_s = jnp.einsum('bhqnl,bqnld->bqhd', p.astype(vg.dtype), vg)
        kwb = lax.dynamic_slice_in_dim(kw_pad, t0, band, axis=1)
        vwb = lax.dynamic_slice_in_dim(vw_pad, t0, band, axis=1)
        s = jnp.einsum('bqhd,bkd->bhqk', qb, kwb).astype(jnp.float32) * scale
        kp = t0 - NSA_WINDOW + jnp.arange(band)
        m = ((kp[None, :] <= tq[:, None]) & (kp[None, :] > tq[:, None] - NSA_WINDOW)
             & (kp[None, :] >= 0))
        p = jax.nn.softmax(jnp.where(m, s, NEG), axis=-1)
        o_w = jnp.einsum('bhqk,bkd->bqhd', p.astype(vwb.dtype), vwb)
        return o_s, o_w

    nq = S // Q_BLOCK
    qs = q.reshape(B, nq, Q_BLOCK, H, HD).transpose(1, 0, 2, 3, 4)
    sels = sel.reshape(B, nq, Q_BLOCK, k_sel).transpose(1, 0, 2, 3)
    t0s = jnp.arange(nq, dtype=jnp.int32) * Q_BLOCK
    o_s, o_w = lax.map(block_body, (qs, sels, t0s))
    o_s = o_s.transpose(1, 0, 2, 3, 4).reshape(B, S, H, HD)
    o_w = o_w.transpose(1, 0, 2, 3, 4).reshape(B, S, H, HD)

    g = jax.nn.sigmoid(gate_logits.astype(jnp.float32)).reshape(B, S, H, 3).astype(q.dtype)
    o = g[..., 0:1] * o_c + g[..., 1:2] * o_s + g[..., 2:3] * o_w
    return o.reshape(B, S, H * HD)


def diff_attention(q, k, v, qk_gain, lam, sub_gain, layer_idx):
    B, S, H = q.shape[:3]
    q = rms_norm(q, qk_gain[0])
    k = rms_norm(k, qk_gain[1])
    lam_init = 0.8 - 0.6 * math.exp(-0.3 * layer_idx)
    lam32 = lam.astype(jnp.float32)
    lam_full = (jnp.exp(jnp.sum(lam32[0] * lam32[1])) - jnp.exp(jnp.sum(lam32[2] * lam32[3]))
                + lam_init)
    scale = DIFF_QK_DIM ** -0.5
    outs = []
    for blk in range(S // Q_BLOCK):
        t0, t1 = blk * Q_BLOCK, (blk + 1) * Q_BLOCK
        s = jnp.einsum('bqhcd,bkhcd->bhcqk', q[:, t0:t1], k[:, :t1]).astype(jnp.float32) * scale
        mask = np.arange(t1)[None, :] <= np.arange(t0, t1)[:, None]
        p = jax.nn.softmax(jnp.where(mask, s, NEG), axis=-1)
        w = p[:, :, 0] - lam_full * p[:, :, 1]
        outs.append(jnp.einsum('bhqk,bkhd->bqhd', w.astype(v.dtype), v[:, :t1]))
    o = jnp.concatenate(outs, axis=1)
    o = rms_norm(o, sub_gain) * (1.0 - lam_init)
    return o.reshape(B, S, H * DIFF_V_DIM)


def gla_mixer(q, k, v, g_lr, og, w_gate2, b_gate, norm_gain):
    B, S = q.shape[:2]
    log_a = jax.nn.log_sigmoid((g_lr @ w_gate2 + b_gate).astype(jnp.float32)) / GLA_TAU
    shp = (B, S, GLA_HEADS, GLA_DK)
    o = chunked_gated_linear_attention(q.reshape(shp) * (GLA_DK ** -0.5), k.reshape(shp),
                                       v.reshape(B, S, GLA_HEADS, GLA_DV), log_a.reshape(shp))
    o = rms_norm(o, norm_gain) * jax.nn.silu(og.reshape(B, S, GLA_HEADS, GLA_DV))
    return o.reshape(B, S, GLA_HEADS * GLA_DV)


def hgrn2_mixer(q, f_pre, i_in, og, lb, norm_gain):
    B, S = q.shape[:2]
    z = f_pre.astype(jnp.float32)
    lb = lb.astype(jnp.float32)
    f = lb + (1.0 - lb) * jax.nn.sigmoid(z)
    log_f = jnp.log(f)
    one_minus_f = (1.0 - lb) * jax.nn.sigmoid(-z)
    shp = (B, S, HG_HEADS, HG_DK)
    o = chunked_gated_linear_attention(q.reshape(shp), one_minus_f.reshape(shp).astype(q.dtype),
                                       i_in.reshape(B, S, HG_HEADS, HG_DV), log_f.reshape(shp))
    o = rms_norm(o, norm_gain) * jax.nn.silu(og.reshape(B, S, HG_HEADS, HG_DV))
    return o.reshape(B, S, HG_HEADS * HG_DV)


def swiglu(x, w_gate, w_up, w_down):
    return (jax.nn.silu(x @ w_gate) * (x @ w_up)) @ w_down


def moe_swiglu(x, router, w_gate, w_up, w_down):
    logits = (x @ router).astype(jnp.float32)
    top_val, top_idx = lax.top_k(logits, TOP_K)
    top_w = jax.nn.softmax(top_val, axis=-1)
    combine = jnp.sum(jax.nn.one_hot(top_idx, N_EXPERTS, dtype=jnp.float32) * top_w[..., None], axis=-2)
    out = jnp.zeros_like(x)
    for e in range(N_EXPERTS):
        out = out + combine[..., e:e + 1].astype(x.dtype) * swiglu(x, w_gate[e], w_up[e], w_down[e])
    return out


def setup_inputs(seed: int = 0) -> dict:
    key = jax.random.key(seed)
    keys = iter(jax.random.split(key, 32))

    def nrm(shape, scale):
        return scale * jax.random.normal(next(keys), shape, jnp.float32)

    def gain(shape):
        return 1.0 + 0.02 * jax.random.normal(next(keys), shape, jnp.float32)

    n_dense = (DEPTH + 1) // 2
    n_moe = DEPTH // 2
    return {
        'x': nrm((BATCH, SEQ, D_MODEL), 1.0),
        'p': nrm((DEPTH, BATCH, SEQ, PLE_DIM), 1.0),
        'norm_attn': gain((DEPTH, D_MODEL)),
        'w_in': nrm((DEPTH, D_MODEL, IN_COLS), D_MODEL ** -0.5),
        'w_out': nrm((DEPTH, MIX_WIDTH, D_MODEL), MIX_WIDTH ** -0.5),
        'nsa_cmp_pos': nrm((DEPTH, 2, NSA_CMP_LEN, HD), 0.1),
        'nsa_cmp_w1': nrm((DEPTH, 2, NSA_CMP_LEN * HD, NSA_CMP_HIDDEN), (NSA_CMP_LEN * HD) ** -0.5),
        'nsa_cmp_w2': nrm((DEPTH, 2, NSA_CMP_HIDDEN, HD), NSA_CMP_HIDDEN ** -0.5),
        'nsa_qk_gain': gain((DEPTH, 4, HD)),
        'diff_qk_gain': gain((DEPTH, 2, DIFF_QK_DIM)),
        'diff_lambda': nrm((DEPTH, 4, DIFF_QK_DIM), 0.1),
        'diff_norm': gain((DEPTH, DIFF_V_DIM)),
        'gla_w_gate2': nrm((DEPTH, GLA_GATE_RANK, GLA_HEADS * GLA_DK), GLA_GATE_RANK ** -0.5),
        'gla_b_gate': nrm((DEPTH, GLA_HEADS * GLA_DK), 0.1),
        'gla_norm': gain((DEPTH, GLA_DV)),
        'hgrn_lb_logits': nrm((DEPTH, HG_HEADS * HG_DK), 1.0),
        'hgrn_norm': gain((DEPTH, HG_DV)),
        'norm_ffn': gain((DEPTH, D_MODEL)),
        'ffn_w_gate': nrm((n_dense, D_MODEL, D_FF), D_MODEL ** -0.5),
        'ffn_w_up': nrm((n_dense, D_MODEL, D_FF), D_MODEL ** -0.5),
        'ffn_w_down': nrm((n_dense, D_FF, D_MODEL), D_FF ** -0.5),
        'moe_router': nrm((n_moe, D_MODEL, N_EXPERTS), D_MODEL ** -0.5),
        'moe_w_gate': nrm((n_moe, N_EXPERTS, D_MODEL, D_FF_EXPERT), D_MODEL ** -0.5),
        'moe_w_up': nrm((n_moe, N_EXPERTS, D_MODEL, D_FF_EXPERT), D_MODEL ** -0.5),
        'moe_w_down': nrm((n_moe, N_EXPERTS, D_FF_EXPERT, D_MODEL), D_FF_EXPERT ** -0.5),
        'ple_norm': gain((DEPTH, D_MODEL)),
        'ple_w_gate': nrm((DEPTH, D_MODEL, D_MODEL), D_MODEL ** -0.5),
        'ple_w_proj': nrm((DEPTH, PLE_DIM, D_MODEL), PLE_DIM ** -0.5),
    }


def reference(x, p, norm_attn, w_in, w_out, nsa_cmp_pos, nsa_cmp_w1, nsa_cmp_w2, nsa_qk_gain,
              diff_qk_gain, diff_lambda, diff_norm, gla_w_gate2, gla_b_gate, gla_norm,
              hgrn_lb_logits, hgrn_norm, norm_ffn, ffn_w_gate, ffn_w_up, ffn_w_down,
              moe_router, moe_w_gate, moe_w_up, moe_w_down, ple_norm, ple_w_gate, ple_w_proj):
    B, S, _ = x.shape
    lb_p = jax.nn.softmax(hgrn_lb_logits.astype(jnp.float32), axis=0)
    lower_bounds = jnp.cumsum(lb_p, axis=0) - lb_p[0]
    h = x
    for i in range(DEPTH):
        a = rms_norm(h, norm_attn[i])
        u = a @ w_in[i]
        (nsa_q, k_cmp, v_cmp, k_slc, v_slc, k_win, v_win, nsa_g,
         d_q, d_k, d_v,
         g_q, g_k, g_v, g_lr, g_og,
         r_q, r_f, r_i, r_og) = split_cols(u, IN_SPLITS)
        o_a = nsa_attention(nsa_q.reshape(B, S, NSA_HEADS, HD), k_cmp, v_cmp, k_slc, v_slc,
                            k_win, v_win, nsa_g, nsa_cmp_pos[i], nsa_cmp_w1[i], nsa_cmp_w2[i],
                            nsa_qk_gain[i])
        o_b = diff_attention(d_q.reshape(B, S, DIFF_HEADS, 2, DIFF_QK_DIM),
                             d_k.reshape(B, S, DIFF_HEADS, 2, DIFF_QK_DIM),
                             d_v.reshape(B, S, DIFF_HEADS, DIFF_V_DIM),
                             diff_qk_gain[i], diff_lambda[i], diff_norm[i], i)
        o_c = gla_mixer(g_q, g_k, g_v, g_lr, g_og, gla_w_gate2[i], gla_b_gate[i], gla_norm[i])
        o_d = hgrn2_mixer(r_q, r_f, r_i, r_og, lower_bounds[i], hgrn_norm[i])
        mix = jnp.concatenate([o_a, o_b, o_c, o_d], axis=-1)
        h = h + mix @ w_out[i]
        c = rms_norm(h, norm_ffn[i])
        if i % 2 == 0:
            h = h + swiglu(c, ffn_w_gate[i // 2], ffn_w_up[i // 2], ffn_w_down[i // 2])
        else:
            h = h + moe_swiglu(c, moe_router[i // 2], moe_w_gate[i // 2], moe_w_up[i // 2],
                               moe_w_down[i // 2])
        gate = jax.nn.sigmoid(rms_norm(h, ple_norm[i]) @ ple_w_gate[i])
        h = h + (p[i] @ ple_w_proj[i]) * gate
    return h
```

```python
import math
import os
import contextlib
import numpy as np
import concourse.bass as bass
import concourse.mybir as mybir
from concourse.bass_utils import run_bass_kernel_spmd
from concourse.alu_op_type import AluOpType as ALU

F32 = mybir.dt.float32
BF16 = mybir.dt.bfloat16
I32 = mybir.dt.int32
AF = mybir.ActivationFunctionType
AX = mybir.AxisListType

NSLOT = 8
D = 1024
INC = 3228
DFF = 3584
NEGM = -30000.0
EPS = 1e-6


def box_of(ap):
    t = ap.tensor
    rowlen = 1
    for s in list(t.shape)[1:]:
        rowlen *= int(s)
    if str(ap.space) != 'DRAM':
        rowlen = int(ap.ap[0][0]) if len(ap.ap) > 0 and int(ap.ap[0][0]) > 0 else rowlen
    off = int(ap.offset)
    p0 = off // rowlen
    f0 = off % rowlen
    pe = 0
    fe = 0
    for (st, cnt) in ap.ap:
        st = int(st); cnt = int(cnt)
        if cnt <= 1 or st == 0:
            continue
        if st % rowlen == 0:
            pe += (st // rowlen) * (cnt - 1)
        else:
            fe += abs(st) * (cnt - 1)
    sz = mybir.dt.size(ap.dtype)
    if str(ap.space) == 'PSUM':
        b0 = (f0 * sz) // 2048 * 2048
        b1 = ((f0 + fe + 1) * sz + 2047) // 2048 * 2048
        q0 = p0 // 32 * 32
        q1 = (p0 + pe + 1 + 31) // 32 * 32
        return (t.name, q0, q1, b0, b1)
    return (t.name, p0, p0 + pe + 1, f0 * sz, (f0 + fe + 1) * sz)


def overlap(a, b):
    return a[1] < b[2] and b[1] < a[2] and a[3] < b[4] and b[3] < a[4]


def covers(a, b):
    return a[1] <= b[1] and a[2] >= b[2] and a[3] <= b[3] and a[4] >= b[4]


def isap(v):
    return v is not None and not isinstance(v, (int, float))


class Prog:
    def __init__(self, nc):
        self.nc = nc
        self.ops = []
        self.track = {}
        self.dma_n = {}

    def op(self, eng, fn, reads=(), writes=(), dma=False):
        idx = len(self.ops)
        rb = [box_of(a) for a in reads]
        wb = [box_of(a) for a in writes]
        wb = wb + [b for b in rb if b[0] == 'psum']
        rb = [b for b in rb if b[0] != 'psum']
        deps = set()
        for b in rb:
            for e in self.track.setdefault(b[0], []):
                if e[3] and overlap(e[0], b):
                    deps.add(e[1])
        for b in wb:
            for e in self.track.setdefault(b[0], []):
                if overlap(e[0], b):
                    deps.add(e[1])
        for b in wb:
            lst = self.track[b[0]]
            lst[:] = [e for e in lst if not covers(b, e[0])]
            lst.append((b, idx, eng, True, dma))
        for b in rb:
            lst = self.track[b[0]]
            lst[:] = [e for e in lst if not (e[2] == eng and (not e[3]) and (not e[4]) and (not dma) and covers(b, e[0]))]
            lst.append((b, idx, eng, False, dma))
        deps.discard(idx)
        o = dict(eng=eng, fn=fn, deps=deps, dma=dma, signal=dma)
        if dma:
            n = self.dma_n.get(eng, 0)
            self.dma_n[eng] = n + 1
            o['slot'] = n % NSLOT
            o['val'] = 16 * (n // NSLOT + 1)
            o['dma_idx'] = n
        self.ops.append(o)
        for d in deps:
            p = self.ops[d]
            if p['eng'] == eng and not p['dma'] and not dma and eng == 'pe':
                continue
            p['signal'] = True
        return idx

    def emit(self):
        nc = self.nc
        engs = ['pe', 'act', 'dve', 'pool', 'sp']
        with contextlib.ExitStack() as st:
            csem = {e: st.enter_context(nc.semaphore('c_' + e)) for e in engs}
            dsem = {}
            for e in self.dma_n:
                dsem[e] = [st.enter_context(nc.semaphore('d_%s_%d' % (e, i))) for i in range(NSLOT)]
            cnt = {e: 0 for e in engs}
            for o in self.ops:
                if o['dma']:
                    o['sem'] = dsem[o['eng']][o['slot']]
                elif o['signal']:
                    cnt[o['eng']] += 1
                    o['sem'] = csem[o['eng']]
                    o['val'] = cnt[o['eng']]
            per = {e: [] for e in engs}
            for i, o in enumerate(self.ops):
                per[o['eng']].append(i)
            self.max_cnt = dict(cnt)
            block = st.enter_context(nc.Block())
            ops = self.ops

            def run(ename, engine):
                seen = {}
                for i in per[ename]:
                    o = ops[i]
                    waits = {}
                    for d in o['deps']:
                        p = ops[d]
                        if p['eng'] == ename and not p['dma'] and not o['dma'] and ename == 'pe':
                            continue
                        key = id(p['sem'])
                        if key not in waits or waits[key][1] < p['val']:
                            waits[key] = (p['sem'], p['val'])
                    if o['dma'] and o['dma_idx'] >= NSLOT:
                        s = o['sem']
                        v = o['val'] - 16
                        key = id(s)
                        if key not in waits or waits[key][1] < v:
                            waits[key] = (s, v)
                    for key, (s, v) in waits.items():
                        if seen.get(key, 0) >= v:
                            continue
                        engine.wait_ge(s, v)
                        seen[key] = v
                    ins = o['fn'](engine)
                    if o['signal']:
                        ins.then_inc(o['sem'], 16 if o['dma'] else 1)
                if ename in dsem:
                    n = self.dma_n[ename]
                    for sl in range(NSLOT):
                        k = (n - sl + NSLOT - 1) // NSLOT
                        if k > 0:
                            engine.wait_ge(dsem[ename][sl], 16 * k)

            block.tensor(lambda e: run('pe', e))
            block.scalar(lambda e: run('act', e))
            block.vector(lambda e: run('dve', e))
            block.gpsimd(lambda e: run('pool', e))
            block.sync(lambda e: run('sp', e))

    def dma(self, q, out, in_, **kw):
        return self.op(q, lambda e: e.dma_start(out=out, in_=in_, **kw), [in_], [out], dma=True)

    def mm(self, out, lhsT, rhs, start=True, stop=True):
        return self.op('pe', lambda e: e.matmul(out=out, lhsT=lhsT, rhs=rhs, start=start, stop=stop, skip_group_check=True), [lhsT, rhs], [out])

    def tr(self, out, in_, ident):
        return self.op('pe', lambda e: e.transpose(out=out, in_=in_, identity=ident), [in_, ident], [out])

    def act(self, out, in_, func, bias=None, scale=None, accum=None):
        kw = {}
        rd = [in_]
        wr = [out]
        if bias is not None:
            kw['bias'] = bias
            if isap(bias):
                rd.append(bias)
        if scale is not None:
            kw['scale'] = scale
            if isap(scale):
                rd.append(scale)
        if accum is not None:
            kw['accum_out'] = accum
            wr.append(accum)
        return self.op('act', lambda e: e.activation(out=out, in_=in_, func=func, **kw), rd, wr)

    def ts(self, out, in0, s1, s2=None, op0=ALU.mult, op1=None, eng='dve', accum=None):
        rd = [in0] + [s for s in (s1, s2) if isap(s)]
        wr = [out] + ([accum] if accum is not None else [])
        kw = {}
        if op1 is not None:
            kw['op1'] = op1
        if accum is not None:
            kw['accum_out'] = accum
        return self.op(eng, lambda e: e.tensor_scalar(out=out, in0=in0, scalar1=s1, scalar2=s2, op0=op0, **kw), rd, wr)

    def tt(self, out, in0, in1, op, eng='dve'):
        return self.op(eng, lambda e: e.tensor_tensor(out=out, in0=in0, in1=in1, op=op), [in0, in1], [out])

    def stt(self, out, in0, scalar, in1, op0, op1):
        rd = [in0, in1] + ([scalar] if isap(scalar) else [])
        return self.op('dve', lambda e: e.scalar_tensor_tensor(out=out, in0=in0, scalar=scalar, in1=in1, op0=op0, op1=op1), rd, [out])

    def copy(self, out, in_, eng='dve'):
        if eng == 'act':
            return self.op('act', lambda e: e.copy(out=out, in_=in_), [in_], [out])
        return self.op(eng, lambda e: e.tensor_copy(out=out, in_=in_), [in_], [out])

    def memset(self, ap, val, eng='pool'):
        return self.op(eng, lambda e: e.memset(ap, val), [], [ap])

    def reduce(self, out, in_, op=ALU.add, axis=AX.X):
        return self.op('dve', lambda e: e.tensor_reduce(out=out, in_=in_, axis=axis, op=op), [in_], [out])

    def recip(self, out, in_):
        return self.op('dve', lambda e: e.reciprocal(out=out, in_=in_), [in_], [out])

    def asel(self, out, in_, pattern, base, cm, fill, cmp=ALU.is_ge):
        return self.op('pool', lambda e: e.affine_select(out=out, in_=in_, pattern=pattern, compare_op=cmp, fill=fill, base=base, channel_multiplier=cm), [in_], [out])


class Arena:
    def __init__(self, t, nwords):
        self.t = t
        self.n = nwords
        self.off = 0
        self.peak = 0

    def mark(self):
        return self.off

    def reset(self, m):
        self.off = m

    def alloc(self, free_shape, dtype, parts=128):
        n = 1
        for s in free_shape:
            n *= s
        sz = mybir.dt.size(dtype)
        words = (n * sz + 3) // 4
        words = (words + 7) // 8 * 8
        assert self.off + words <= self.n, "arena overflow %d+%d>%d" % (self.off, words, self.n)
        ap = self.t[0:parts, self.off:self.off + words]
        self.off += words
        self.peak = max(self.peak, self.off)
        if dtype != F32:
            ap = ap.bitcast(dtype)
        ap = ap[:, 0:n]
        if len(free_shape) == 2:
            ap = ap.rearrange("p (a b) -> p a b", a=free_shape[0])
        elif len(free_shape) == 3:
            ap = ap.rearrange("p (a b c) -> p a b c", a=free_shape[0], b=free_shape[1])
        return ap


def rstd_from_ss(P, out, ss, n):
    P.act(out, ss, AF.Sqrt, bias=EPS, scale=1.0 / n)
    P.recip(out, out)


class Ctx:
    pass


def build(S=4096, dbg=None, phases=None):
    NT = S // 128
    NQB = S // 512
    NSEL = S // 64
    NCMP = S // 16 - 1
    NBT = (NCMP + 127) // 128
    nc = bass.Bass("TRN2", target_bir_lowering=False)

    def din(name, shape):
        return nc.dram_tensor(name, shape, F32, kind="ExternalInput").ap()

    I = {}
    I['x'] = din("x", [S, D])
    I['p'] = din("p", [2, S, 256])
    for name, shape in [("norm_attn", [2, D]), ("w_in", [2, D, INC]), ("w_out", [2, D, D]),
                        ("nsa_cmp_pos", [2, 2, 32, 64]), ("nsa_cmp_w1", [2, 2, 2048, 256]), ("nsa_cmp_w2", [2, 2, 256, 64]),
                        ("nsa_qk_gain", [2, 4, 64]), ("diff_qk_gain", [2, 2, 32]), ("diff_lambda", [2, 4, 32]),
                        ("diff_norm", [2, 64]), ("gla_w_gate2", [2, 16, 128]), ("gla_b_gate", [2, 128]), ("gla_norm", [2, 64]),
                        ("hgrn_lb_logits", [2, 256]), ("hgrn_norm", [2, 64]), ("norm_ffn", [2, D]),
                        ("ffn_w_gate", [1, D, DFF]), ("ffn_w_up", [1, D, DFF]), ("ffn_w_down", [1, DFF, D]),
                        ("moe_router", [1, D, 8]), ("moe_w_gate", [1, 8, D, DFF]), ("moe_w_up", [1, 8, D, DFF]),
                        ("moe_w_down", [1, 8, DFF, D]), ("ple_norm", [2, D]), ("ple_w_gate", [2, D, D]), ("ple_w_proj", [2, 256, D])]:
        I[name] = din(name, shape)
    Y = nc.dram_tensor("y", [S, D], F32, kind="ExternalOutput").ap()

    def scratch(name, shape, dt):
        kind = "ExternalOutput" if (dbg and name in dbg) else "Internal"
        return nc.dram_tensor(name, shape, dt, kind=kind).ap()

    U = scratch("U", [S, INC], F32)
    MIX = scratch("MIX", [S, D], BF16)
    H = scratch("H", [S, D], F32)
    CTD = scratch("CTD", [128, 8, S], BF16)
    COMB = scratch("COMB", [8, S], F32)

    with contextlib.ExitStack() as st:
        ARW = 49 * 1024
        arena_t = st.enter_context(nc.sbuf_tensor("arena", [128, ARW], F32))
        psum = st.enter_context(nc.psum_tensor("psum", [128, 8, 512], F32))
        P = Prog(nc)
        A = Arena(arena_t, ARW)
        C = Ctx()
        C.nc, C.P, C.A, C.psum, C.I = nc, P, A, psum, I
        C.S, C.NT, C.NQB, C.NSEL, C.NCMP, C.NBT = S, NT, NQB, NSEL, NCMP, NBT
        C.U, C.MIX, C.H, C.Y = U, MIX, H, Y
        C.CTD, C.COMB = CTD, COMB
        C.bank = lambda b: psum[:, b, :]
        C.bankbf = lambda b: psum[:, b, :].bitcast(BF16)

        C.identf = A.alloc([128], F32)
        C.ident = A.alloc([128], BF16)
        P.memset(C.identf, 1.0)
        P.asel(C.identf, C.identf, [[1, 128]], 0, -1, 0.0, cmp=ALU.is_equal)
        P.copy(C.ident, C.identf, 'dve')
        C.ones_row = A.alloc([128], F32, parts=1)
        P.memset(C.ones_row, 1.0)
        C.Cm = A.alloc([4, 512], BF16)
        C.Cw = A.alloc([4, 512], BF16)
        tmpf = A.alloc([512], F32)
        for r in range(4):
            P.memset(tmpf, 0.0)
            P.asel(tmpf, tmpf, [[1, 512]], -128 * r, -1, NEGM)
            P.copy(C.Cm[:, r, :], tmpf, 'dve')
            P.memset(tmpf, 0.0)
            P.asel(tmpf, tmpf, [[-1, 512]], 128 * r - 1, 1, NEGM)
            P.copy(C.Cw[:, r, :], tmpf, 'dve')

        ph = phases or ['inproj', 'nsa', 'diff', 'gla', 'hgrn', 'out']
        for L in range(2):
            src = I['x'] if L == 0 else H
            dst = H if L == 0 else Y
            if ('inproj', L) in ph or 'inproj' in ph:
                phase_inproj(C, L, src)
            if 'nsa' in ph:
                phase_nsa(C, L)
            if 'diff' in ph:
                phase_diff(C, L)
            if 'gla' in ph:
                phase_gla(C, L, hgrn=False)
            if 'hgrn' in ph:
                phase_gla(C, L, hgrn=True)
            if 'out' in ph:
                phase_out(C, L, src, dst)
            if dbg and dbg.get('stop_after_layer') == L:
                break
        P.emit()
        C.peak = A.peak
    nc._ctx = C
    return nc


def load_cast(P, out, in_, q='pool'):
    n = out.shape[-1]
    if n <= 2048:
        P.dma(q, out, in_)
    else:
        k = (n + 2047) // 2048
        step = (n + k - 1) // k
        for c0 in range(0, n, step):
            c1 = min(n, c0 + step)
            P.dma(q, out[..., c0:c1], in_[..., c0:c1])


def norm_transpose(C, xt, gT, hn, aT, tb, ss, rs, junk):
    P = C.P
    P.act(junk, xt, AF.Square, accum=ss)
    rstd_from_ss(P, rs, ss, D)
    P.ts(hn, xt, rs[:, 0:1], None, ALU.mult)
    pT = C.bankbf(tb).rearrange("p (a b) -> p a b", a=8)
    for c in range(8):
        P.tr(pT[:, c, :], hn[:, c * 128:(c + 1) * 128], C.ident)
    P.tt(aT, pT, gT.unsqueeze(2).to_broadcast([128, 8, 128]), ALU.mult)


def phase_inproj(C, L, src):
    P, A, I = C.P, C.A, C.I
    m = A.mark()
    win = A.alloc([8, INC], BF16)
    for kc in range(8):
        load_cast(P, win[:, kc, :], I['w_in'][L, kc * 128:(kc + 1) * 128, :])
    gT = A.alloc([8], F32)
    P.dma('sp', gT, I['norm_attn'][L].rearrange("(c p) -> p c", p=128), allow_slow_non_contiguous=True)
    xts = [A.alloc([D], F32) for _ in range(2)]
    junk = A.alloc([D], BF16)
    hn = A.alloc([D], BF16)
    aTs = [A.alloc([8, 128], BF16) for _ in range(2)]
    us = [A.alloc([INC], F32) for _ in range(2)]
    ss = A.alloc([1], F32)
    rs = A.alloc([1], F32)
    chunks = [(c0, min(INC, c0 + 512)) for c0 in range(0, INC, 512)]
    P.dma('sp', xts[0], src[0:128, :])
    bk = 0
    for t in range(C.NT):
        xt = xts[t % 2]
        if t + 1 < C.NT:
            P.dma('sp', xts[(t + 1) % 2], src[(t + 1) * 128:(t + 2) * 128, :])
        aT = aTs[t % 2]
        norm_transpose(C, xt, gT, hn, aT, t % 2, ss, rs, junk)
        u = us[t % 2]
        for ci, (c0, c1) in enumerate(chunks):
            b = 2 + (bk % 6)
            bk += 1
            pb = C.bank(b)[:, 0:c1 - c0]
            for kc in range(8):
                P.mm(pb, aT[:, kc, :], win[:, kc, c0:c1], start=(kc == 0), stop=(kc == 7))
            if ci % 2 == 0:
                P.copy(u[:, c0:c1], pb, 'act')
            else:
                P.copy(u[:, c0:c1], pb, 'dve')
        P.dma('sp', C.U[t * 128:(t + 1) * 128, :], u)
    A.reset(m)


def bcast_load(P, out, vec, q='sp'):
    P.dma(q, out, vec.partition_broadcast(128))


def phase_nsa(C, L):
    P, A, I = C.P, C.A, C.I
    S, NT, NQB, NSEL, NCMP, NBT = C.S, C.NT, C.NQB, C.NSEL, C.NCMP, C.NBT
    m0 = A.mark()
    QT = A.alloc([2, S], BF16)
    KsT = A.alloc([S], BF16)
    KwT = A.alloc([S], BF16)
    KVcT = A.alloc([S], BF16)
    Vs = A.alloc([NT, 66], BF16)
    Vw = A.alloc([NT, 66], BF16)
    G = A.alloc([NT, 12], F32)
    MIXA = A.alloc([NT, 256], F32)
    SelT = A.alloc([S], BF16)
    Rall = A.alloc([S], BF16)
    gcol = A.alloc([4], F32)
    gsrc = I['nsa_qk_gain'][L]
    for hh in range(2):
        P.dma('sp', gcol[hh * 64:(hh + 1) * 64, :], gsrc.rearrange("j d -> d j"), allow_slow_non_contiguous=True)
    P.ts(gcol[:, 0:1], gcol[:, 0:1], 0.125, None, ALU.mult)
    P.memset(Vs[:, :, 64:65], 1.0)
    P.memset(Vw[:, :, 64:65], 1.0)
    m1 = A.mark()
    rf = A.alloc([S], F32, parts=64)
    P.memset(rf, 1.0)
    P.asel(rf, rf, [[1, S]], 0, -64, 0.0)
    P.asel(rf, rf, [[-1, S]], 63, 64, 0.0)
    P.copy(Rall[0:64], rf, 'dve')
    P.dma('sp', Rall[64:128, :], Rall[0:64, :])
    A.reset(m1)

    if os.environ.get('NSA_STOP') == '0':
        A.reset(m0)
        return
    m1 = A.mark()
    uts = [A.alloc([652], F32) for _ in range(2)]
    sq = A.alloc([640], F32)
    ssq = A.alloc([10], F32)
    rsq = A.alloc([10], F32)
    nb16 = A.alloc([5, 128], BF16)
    P.dma('sp', uts[0], C.U[0:128, 0:652])
    for t in range(NT):
        ut = uts[t % 2]
        if t + 1 < NT:
            P.dma('sp', uts[(t + 1) % 2], C.U[(t + 1) * 128:(t + 2) * 128, 0:652])
        SK = os.environ.get('NSA_SKIP', '').split(',')
        P.act(sq, ut[:, 0:640], AF.Square)
        P.reduce(ssq, sq.rearrange("p (a b) -> p a b", a=10))
        rstd_from_ss(P, rsq, ssq, 64)
        if 'nb' not in SK:
            P.tt(nb16[:, 0:2, :].rearrange("p a (h d) -> p (a h) d", h=2), ut[:, 0:256].rearrange("p (h d) -> p h d", h=4),
                 rsq[:, 0:4].unsqueeze(2).to_broadcast([128, 4, 64]), ALU.mult)
            for hh in range(2):
                P.ts(nb16[:, 2, hh * 64:(hh + 1) * 64], ut[:, 384:448], rsq[:, 6:7], None, ALU.mult)
                P.ts(nb16[:, 3, hh * 64:(hh + 1) * 64], ut[:, 512:576], rsq[:, 8:9], None, ALU.mult)
        if 'pc' not in SK:
            P.copy(nb16[:, 4, :], ut[:, 256:384], 'pool')
        ts_ = slice(t * 128, (t + 1) * 128)
        if 'tr' not in SK:
            pT = C.bankbf(t % 2).rearrange("p (a b) -> p a b", a=8)
            for c in range(5):
                P.tr(pT[:, c, :], nb16[:, c, :], C.ident)
            if 'ev1' not in SK:
                P.act(QT[:, :, ts_], pT[:, 0:2, :], AF.Identity, scale=gcol[:, 0:1])
            if 'ev2' not in SK:
                P.ts(KsT[:, ts_], pT[:, 2, :], gcol[:, 2:3], None, ALU.mult)
                P.ts(KwT[:, ts_], pT[:, 3, :], gcol[:, 3:4], None, ALU.mult)
            if 'ev3' not in SK:
                P.copy(KVcT[:, ts_], pT[:, 4, :], 'act')
        if 'v' not in SK:
            P.copy(Vs[:, t, 0:64], ut[:, 448:512], 'pool')
            P.copy(Vw[:, t, 0:64], ut[:, 576:640], 'pool')
        if 'g' not in SK:
            P.act(G[:, t, :], ut[:, 640:652], AF.Sigmoid)
    if not os.environ.get('NSA_NORESET'):
        A.reset(m1)

    if os.environ.get('NSA_STOP') == 'a':
        A.reset(m0)
        return
    m1 = A.mark()
    w1 = A.alloc([32, 256], BF16)
    SKB = os.environ.get('NSA_SKIPB', '').split(',')
    for j in range(2):
        w1v = I['nsa_cmp_w1'][L, j].rearrange("(l d) h -> d l h", d=64)
        for l0 in range(0, 32, 8):
            if 'w1' not in SKB:
                P.dma('pool', w1[j * 64:(j + 1) * 64, l0:l0 + 8, :], w1v[:, l0:l0 + 8, :])
    w2 = A.alloc([2, 2, 64], BF16)
    for j in range(2):
        if 'w2' not in SKB:
            P.dma('pool', w2[:, j, :, :], I['nsa_cmp_w2'][L, j].rearrange("(t p) d -> p t d", p=128))
    posl = A.alloc([128], F32, parts=32)
    posT = A.alloc([32], BF16)
    if 'pos' not in SKB:
        for j in range(2):
            P.dma('sp', posl[:, j * 64:(j + 1) * 64], I['nsa_cmp_pos'][L, j])
        ppos = C.bank(2)[:, 0:32]
        P.tr(ppos, posl, C.identf[0:32, 0:32])
        P.copy(posT, ppos, 'dve')
    if os.environ.get('NSA_STOP') == 'b1':
        A.reset(m0)
        return
    hidT = A.alloc([2, 2, 256], BF16)
    biasc = A.alloc([4], F32)
    rhs_c = A.alloc([NBT, 129], F32)
    kcT = A.alloc([NBT * 128], BF16)
    kcn = A.alloc([128], BF16)
    kss = A.alloc([1], F32)
    krs = A.alloc([1], F32)
    kjunk = A.alloc([64], F32)
    for kv in range(2):
        rows = slice(kv * 64, (kv + 1) * 64)
        for ht in range(2):
            pb = C.bank(2 + ht)[:, 0:NCMP]
            for l in range(32):
                P.mm(pb, w1[rows, l, ht * 128:(ht + 1) * 128], KVcT[rows, l:l + 16 * (NCMP - 1) + 1:16], start=(l == 0), stop=(l == 31))
            pbb = C.bank(4 + ht)[:, 0:1]
            for l in range(32):
                P.mm(pbb, w1[rows, l, ht * 128:(ht + 1) * 128], posT[rows, l:l + 1], start=(l == 0), stop=(l == 31))
            bc = biasc[:, kv * 2 + ht:kv * 2 + ht + 1]
            P.copy(bc, pbb, 'dve')
            P.act(hidT[:, kv, ht, 0:NCMP], pb, AF.Silu, bias=bc)
    if os.environ.get('NSA_STOP') == 'b2':
        A.reset(m0)
        return
    for bt in range(NBT):
        nb = min(128, NCMP - bt * 128)
        bs = slice(bt * 128, bt * 128 + nb)
        pk = C.bank(2)[0:nb, 0:64]
        for ht in range(2):
            P.mm(pk, hidT[:, 0, ht, bs], w2[:, 0, ht, :], start=(ht == 0), stop=(ht == 1))
        P.act(kjunk[0:nb], pk, AF.Square, accum=kss[0:nb])
        rstd_from_ss(P, krs[0:nb], kss[0:nb], 64)
        for hh in range(2):
            P.ts(kcn[0:nb, hh * 64:(hh + 1) * 64], pk, krs[0:nb, 0:1], None, ALU.mult)
        pkt = C.bankbf(3)[:, 0:nb]
        P.tr(pkt, kcn[0:nb, :], C.ident[0:nb, 0:nb])
        P.ts(kcT[:, bs], pkt, gcol[:, 1:2], None, ALU.mult)
        pv = C.bank(4)[0:nb, 0:64]
        for ht in range(2):
            P.mm(pv, hidT[:, 1, ht, bs], w2[:, 1, ht, :], start=(ht == 0), stop=(ht == 1))
        P.copy(rhs_c[0:nb, bt, 0:64], pv, 'act')
        ov = rhs_c[:, bt, 64:64 + NSEL]
        P.memset(rhs_c[:, bt, 64:129], 1.0)
        P.asel(ov, ov, [[64, NSEL]], 63 - 16 * 128 * bt, -16, 0.0)
        P.asel(ov, ov, [[-64, NSEL]], 31 + 16 * 128 * bt, 16, 0.0)

    if os.environ.get('NSA_STOP') == 'b':
        A.reset(m0)
        return
    PcT = A.alloc([4, NBT, 512], F32)
    BSb = A.alloc([128], F32)
    BIGV = 1.0e4
    P.memset(BSb, 0.0)
    P.memset(BSb[0:64, 63:65], BIGV)
    P.memset(BSb[0:64, 65:128], -BIGV)
    P.memset(BSb[64:128, 64:66], BIGV)
    P.memset(BSb[64:128, 66:128], -BIGV)
    imp = A.alloc([64], F32)
    imp2 = A.alloc([64], F32)
    m8a = A.alloc([8], F32)
    m8b = A.alloc([8], F32)
    seln = A.alloc([128], BF16)
    P.memset(seln, 0.0)
    den = A.alloc([4], F32)
    cmb = A.alloc([4], F32)
    for tb in range(NQB):
        T0 = tb * 512
        live = []
        for h in range(4):
            hr = slice((h % 2) * 64, (h % 2) * 64 + 64)
            for bt in range(NBT):
                nb = min(128, NCMP - bt * 128)
                if 16 * (128 * bt) + 31 > T0 + 511:
                    continue
                live.append((h, bt))
                pb = C.bank(2 + (h * NBT + bt) % 4)[0:nb, :]
                P.mm(pb, kcT[hr, bt * 128:bt * 128 + nb], QT[hr, h // 2, T0:T0 + 512])
                P.act(PcT[0:nb, h, bt, :], pb, AF.Exp)
                P.asel(PcT[0:nb, h, bt, :], PcT[0:nb, h, bt, :], [[1, 512]], T0 - 2048 * bt - 31, -16, 0.0)
        for qs in range(4):
            t = tb * 4 + qs
            pos = [C.bank(6)[:, 0:258].rearrange("p (a b) -> p a b", a=2), C.bank(7)[:, 0:258].rearrange("p (a b) -> p a b", a=2)]
            for h in range(4):
                bts = [bt for (hh, bt) in live if hh == h]
                o = pos[h // 2][:, h % 2, :]
                if not bts:
                    continue
                for i, bt in enumerate(bts):
                    nb = min(128, NCMP - bt * 128)
                    P.mm(o, PcT[0:nb, h, bt, qs * 128:(qs + 1) * 128], rhs_c[0:nb, bt, :], start=(h % 2 == 0 and i == 0), stop=(i == len(bts) - 1))
            if not live:
                P.memset(MIXA[:, t, :], 0.0, 'dve')
                P.memset(imp[:, 0:NSEL], 0.0, 'dve')
            else:
                for h in range(4):
                    o = pos[h // 2][:, h % 2, :]
                    P.ts(den[:, h:h + 1], o[:, 128:129], 1e-30, None, ALU.max)
                    P.recip(den[:, h:h + 1], den[:, h:h + 1])
                    if h == 0:
                        P.ts(imp[:, 0:NSEL], o[:, 64:64 + NSEL], den[:, 0:1], None, ALU.mult)
                    else:
                        P.stt(imp[:, 0:NSEL], o[:, 64:64 + NSEL], den[:, h:h + 1], imp[:, 0:NSEL], ALU.mult, ALU.add)
                    P.tt(cmb[:, h:h + 1], den[:, h:h + 1], G[:, t, 3 * h:3 * h + 1], ALU.mult)
                    P.ts(MIXA[:, t, h * 64:(h + 1) * 64], o[:, 0:64], cmb[:, h:h + 1], None, ALU.mult)
            if NSEL > 16:
                P.tt(imp[:, 0:NSEL], imp[:, 0:NSEL], BSb[:, 64 - 2 * t:64 - 2 * t + NSEL], ALU.add)
                P.memset(imp[:, 0:1], BIGV, 'dve')
                P.op('dve', lambda e: e.max(out=m8a, in_=imp[:, 0:NSEL]), [imp[:, 0:NSEL]], [m8a])
                P.op('dve', lambda e: e.match_replace(out=imp2[:, 0:NSEL], in_to_replace=m8a, in_values=imp[:, 0:NSEL], imm_value=-1e9), [imp[:, 0:NSEL], m8a], [imp2[:, 0:NSEL]])
                P.op('dve', lambda e: e.max(out=m8b, in_=imp2[:, 0:NSEL]), [imp2[:, 0:NSEL]], [m8b])
                P.ts(seln[:, 0:NSEL], imp[:, 0:NSEL], m8b[:, 7:8], NEGM, ALU.is_lt, ALU.mult)
                P.ts(seln[:, 64:64 + NSEL], imp[:, 0:NSEL], m8b[:, 7:8], NEGM, ALU.is_lt, ALU.mult)
            pst = C.bankbf(t % 2)[:, 0:128]
            P.tr(pst, seln, C.ident)
            P.copy(SelT[:, t * 128:(t + 1) * 128], pst, 'act')
    A.reset(m1)

    if os.environ.get('NSA_STOP') == 'c':
        A.reset(m0)
        return
    m1 = A.mark()
    PTs = [A.alloc([512], BF16) for _ in range(3)]
    rd2 = A.alloc([8], F32)
    pi = 0
    oi = 0
    for branch in range(2):
        KT = KsT if branch == 0 else KwT
        V = Vs if branch == 0 else Vw
        for h in range(4):
            hr = slice((h % 2) * 64, (h % 2) * 64 + 64)
            for qb in range(NQB):
                kts = list(range(0, 4 * qb + 4)) if branch == 0 else list(range(max(0, 4 * qb - 4), 4 * qb + 4))
                ob = C.bank(6 + oi % 2)[:, 0:260].rearrange("p (a b) -> p a b", a=4)
                oi += 1
                first = True
                lastkt = {qs: 4 * qb + qs for qs in range(4)}
                for kt in kts:
                    pb = C.bank(2 + pi % 4)
                    ks = slice(kt * 128, (kt + 1) * 128)
                    diag = kt >= 4 * qb
                    P.mm(pb, KT[hr, ks], QT[hr, h // 2, qb * 512:(qb + 1) * 512], start=True, stop=False)
                    if branch == 0:
                        r0 = (h % 2) * 64
                        P.mm(pb, Rall[r0:r0 + NSEL, ks], SelT[r0:r0 + NSEL, qb * 512:(qb + 1) * 512], start=False, stop=not diag)
                        if diag:
                            P.mm(pb, C.ident, C.Cm[:, kt - 4 * qb, :], start=False, stop=True)
                    else:
                        mk = C.Cm[:, kt - 4 * qb, :] if diag else C.Cw[:, kt - (4 * qb - 4), :]
                        P.mm(pb, C.ident, mk, start=False, stop=True)
                    PT = PTs[pi % 3]
                    pi += 1
                    P.act(PT, pb, AF.Exp)
                    for qs in range(4):
                        if diag and qs < kt - 4 * qb:
                            continue
                        if (not diag) and branch == 1 and qs > kt - (4 * qb - 4):
                            continue
                        P.mm(ob[:, qs, :], PT[:, qs * 128:(qs + 1) * 128], V[:, kt, 0:65], start=first, stop=(kt == lastkt[qs]))
                        first = False
                for qs in range(4):
                    t = qb * 4 + qs
                    P.recip(rd2[:, qs:qs + 1], ob[:, qs, 64:65])
                    P.tt(rd2[:, 4 + qs:5 + qs], rd2[:, qs:qs + 1], G[:, t, 3 * h + 1 + branch:3 * h + 2 + branch], ALU.mult)
                    P.stt(MIXA[:, t, h * 64:(h + 1) * 64], ob[:, qs, 0:64], rd2[:, 4 + qs:5 + qs], MIXA[:, t, h * 64:(h + 1) * 64], ALU.mult, ALU.add)
    mb = A.alloc([NT, 256], BF16)
    P.copy(mb, MIXA, 'dve')
    P.dma('sp', C.MIX[:, 0:256].rearrange("(t p) c -> p t c", p=128), mb)
    A.reset(m1)
    A.reset(m0)


def bcast_row(C, out_sb, row_dram, n, scale=None, q='sp'):
    P, A = C.P, C.A
    m = A.mark()
    tmp = A.alloc([n], F32, parts=1)
    P.dma(q, tmp, row_dram.rearrange("(o n) -> o n", o=1))
    for c0 in range(0, n, 512):
        c1 = min(n, c0 + 512)
        pb = C.bank(2 + (c0 // 512) % 2)[:, 0:c1 - c0]
        P.mm(pb, C.ones_row, tmp[0:1, c0:c1])
        if scale is None:
            P.copy(out_sb[:, c0:c1], pb, 'dve')
        else:
            P.ts(out_sb[:, c0:c1], pb, scale, None, ALU.mult)
    A.reset(m)


def phase_diff(C, L):
    P, A, I = C.P, C.A, C.I
    S, NT, NQB = C.S, C.NT, C.NQB
    m0 = A.mark()
    lam_init = 0.8 - 0.6 * math.exp(-0.3 * L)
    DQT = A.alloc([3, S], BF16)
    DKT = A.alloc([3, S], BF16)
    DV = A.alloc([NT, 4, 66], BF16)
    MIXB = A.alloc([NT, 256], BF16)
    gcol = A.alloc([2], F32)
    gsrc = I['diff_qk_gain'][L]
    for r in range(4):
        P.dma('sp', gcol[r * 32:(r + 1) * 32, :], gsrc.rearrange("j d -> d j"), allow_slow_non_contiguous=True)
    P.ts(gcol[:, 0:1], gcol[:, 0:1], 32 ** -0.5, None, ALU.mult)
    P.memset(DV[:, :, :, 64:66], 1.0)
    lam1 = A.alloc([128], F32, parts=1)
    P.dma('sp', lam1, I['diff_lambda'][L].rearrange("(o a) b -> o (a b)", o=1))
    lp = A.alloc([64], F32, parts=1)
    lsum = A.alloc([2], F32, parts=1)
    P.tt(lp[:, 0:32], lam1[:, 0:32], lam1[:, 32:64], ALU.mult)
    P.tt(lp[:, 32:64], lam1[:, 64:96], lam1[:, 96:128], ALU.mult)
    P.reduce(lsum, lp.rearrange("p (a b) -> p a b", a=2))
    P.act(lsum, lsum, AF.Exp)
    lf = A.alloc([1], F32, parts=1)
    P.tt(lf, lsum[:, 1:2], lsum[:, 0:1], ALU.subtract)
    P.ts(lf, lf, -lam_init, None, ALU.add)
    neglam = A.alloc([1], F32)
    pbl = C.bank(2)[:, 0:1]
    P.mm(pbl, C.ones_row, lf[0:1, 0:1])
    P.copy(neglam, pbl, 'dve')
    subg = A.alloc([64], F32)
    bcast_row(C, subg, I['diff_norm'][L], 64, scale=(1.0 - lam_init))

    m1 = A.mark()
    uts = [A.alloc([768], F32) for _ in range(2)]
    sq = A.alloc([512], F32)
    ssq = A.alloc([16], F32)
    rsq = A.alloc([16], F32)
    nb16 = A.alloc([4, 128], BF16)
    P.dma('sp', uts[0], C.U[0:128, 652:1420])
    for t in range(NT):
        ut = uts[t % 2]
        if t + 1 < NT:
            P.dma('sp', uts[(t + 1) % 2], C.U[(t + 1) * 128:(t + 2) * 128, 652:1420])
        P.act(sq, ut[:, 0:512], AF.Square)
        P.reduce(ssq, sq.rearrange("p (a b) -> p a b", a=16))
        rstd_from_ss(P, rsq, ssq, 32)
        P.tt(nb16.rearrange("p a (g d) -> p (a g) d", g=4), ut[:, 0:512].rearrange("p (g d) -> p g d", g=16),
             rsq.unsqueeze(2).to_broadcast([128, 16, 32]), ALU.mult)
        pT = C.bankbf(t % 2).rearrange("p (a b) -> p a b", a=8)
        nbf = nb16.rearrange("p a b -> p (a b)")
        for qk in range(2):
            for j in range(3):
                w = 96 if j < 2 else 64
                P.tr(pT[0:w, qk * 3 + j, :], nbf[:, qk * 256 + j * 96:qk * 256 + j * 96 + w], C.ident)
        ts_ = slice(t * 128, (t + 1) * 128)
        P.ts(DQT[0:96, :, ts_], pT[0:96, 0:3, :], gcol[0:96, 0:1], None, ALU.mult)
        P.ts(DKT[0:96, :, ts_], pT[0:96, 3:6, :], gcol[0:96, 1:2], None, ALU.mult)
        P.copy(DV[:, t, :, 0:64], ut[:, 512:768].rearrange("p (h d) -> p h d", h=4), 'act')
    A.reset(m1)

    PTs = [A.alloc([512], BF16) for _ in range(3)]
    rd = A.alloc([4], F32)
    otmp = A.alloc([64], F32)
    o2 = A.alloc([64], F32)
    junk = A.alloc([64], F32)
    ss1 = A.alloc([1], F32)
    rs1 = A.alloc([1], F32)
    pi = 0
    oi = 0
    for h in range(4):
        hb = h // 2
        for qb in range(NQB):
            obs = [C.bank(4 + 2 * (oi % 2) + c)[:, 0:260].rearrange("p (a b) -> p a b", a=4) for c in range(2)]
            oi += 1
            first = [True, True]
            for kt in range(4 * qb + 4):
                ks = slice(kt * 128, (kt + 1) * 128)
                diag = kt >= 4 * qb
                for c in range(2):
                    g = h * 2 + c
                    hb = g // 3
                    r0 = (g % 3) * 32
                    rows = slice(r0, r0 + 32)
                    pb = C.bank(pi % 4)
                    P.mm(pb, DKT[rows, hb, ks], DQT[rows, hb, qb * 512:(qb + 1) * 512], start=True, stop=not diag)
                    if diag:
                        P.mm(pb, C.ident, C.Cm[:, kt - 4 * qb, :], start=False, stop=True)
                    PT = PTs[pi % 3]
                    pi += 1
                    P.act(PT, pb, AF.Exp)
                    for qs in range(4):
                        if diag and qs < kt - 4 * qb:
                            continue
                        P.mm(obs[c][:, qs, :], PT[:, qs * 128:(qs + 1) * 128], DV[:, kt, h, 0:65], start=first[c], stop=(kt == 4 * qb + qs))
                        first[c] = False
            for qs in range(4):
                t = qb * 4 + qs
                P.recip(rd[:, 0:1], obs[0][:, qs, 64:65])
                P.recip(rd[:, 1:2], obs[1][:, qs, 64:65])
                P.tt(rd[:, 2:3], rd[:, 1:2], neglam, ALU.mult)
                P.ts(otmp, obs[1][:, qs, 0:64], rd[:, 2:3], None, ALU.mult)
                P.stt(o2, obs[0][:, qs, 0:64], rd[:, 0:1], otmp, ALU.mult, ALU.add)
                P.act(junk, o2, AF.Square, accum=ss1)
                rstd_from_ss(P, rs1, ss1, 64)
                P.stt(MIXB[:, t, h * 64:(h + 1) * 64], o2, rs1[:, 0:1], subg, ALU.mult, ALU.mult)
    P.dma('sp', C.MIX[:, 256:512].rearrange("(t p) c -> p t c", p=128), MIXB)
    A.reset(m0)


def phase_gla(C, L, hgrn):
    P, A, I = C.P, C.A, C.I
    S, NT = C.S, C.NT
    m0 = A.mark()
    if hgrn:
        c0, W, NCH, DK = 2204, 1024, 256, 64
        qo, ko, vo, ogo = 0, 256, 512, 768
        mixcol = 768
        qscale = 1.0
    else:
        c0, W, NCH, DK = 1420, 784, 128, 32
        qo, ko, vo, lro, ogo = 0, 128, 256, 512, 528
        mixcol = 512
        qscale = 32 ** -0.5
    L1 = A.alloc([128], F32)
    L2 = A.alloc([128], F32)
    L3 = A.alloc([128], F32)
    IND = A.alloc([2], F32)
    P.memset(L1, 1.0)
    P.asel(L1, L1, [[1, 128]], 0, -1, 0.0)
    P.memset(L1[0:64, 64:128], 0.0)
    P.memset(L3, 1.0)
    P.asel(L3, L3, [[-1, 128]], -1, 1, 0.0)
    P.memset(L3[64:128, 0:64], 0.0)
    P.memset(L2, 0.0)
    P.memset(L2[0:32, 0:64], 1.0)
    P.memset(L2[64:96, 64:128], 1.0)
    P.tt(L2, L1, L2, ALU.subtract, eng='pool')
    P.memset(IND, 0.0)
    P.memset(IND[0:64, 0:1], 1.0)
    P.memset(IND[64:128, 1:2], 1.0)
    gn = A.alloc([64], F32)
    bcast_row(C, gn, I['hgrn_norm' if hgrn else 'gla_norm'][L], 64)
    if hgrn:
        lb = A.alloc([256], F32)
        oml = A.alloc([256], F32)
        if L == 0:
            P.memset(lb, 0.0)
            P.memset(oml, 1.0)
        else:
            l0 = A.alloc([256], F32)
            bcast_row(C, lb, I['hgrn_lb_logits'][1], 256)
            bcast_row(C, l0, I['hgrn_lb_logits'][0], 256)
            P.tt(lb, lb, l0, ALU.subtract)
            P.act(lb, lb, AF.Sigmoid)
            P.ts(oml, lb, -1.0, 1.0, ALU.mult, ALU.add)
    else:
        w_aug = A.alloc([128], F32, parts=33)
        glrT = A.alloc([128], F32, parts=33)
        P.memset(w_aug, 0.0)
        P.memset(glrT, 0.0)
        P.memset(glrT[32:33, :], 1.0)
        P.dma('sp', w_aug[0:16, :], I['gla_w_gate2'][L])
        P.dma('sp', w_aug[32:33, :], I['gla_b_gate'][L].rearrange("(o n) -> o n", o=1))
    S32 = A.alloc([4, 64], F32)
    P.memset(S32, 0.0)
    ring = [A.alloc([4, 64], BF16) for _ in range(4)]
    ATs = [A.alloc([4, 128], BF16) for _ in range(2)]
    for a_ in ATs:
        P.memset(a_, 0.0)
    MIXC = A.alloc([NT, 256], BF16)
    Af32 = A.alloc([4, 128], F32)
    uts = [A.alloc([W], F32) for _ in range(2)]
    la = A.alloc([NCH], F32)
    kk = A.alloc([NCH], F32)
    e_ = A.alloc([NCH], F32)
    E = A.alloc([4, NCH], F32)
    qt16 = A.alloc([3, NCH], BF16)
    ks16 = A.alloc([NCH], BF16)
    v16 = A.alloc([256], BF16)
    T16 = A.alloc([12, 128], BF16)
    DEC = A.alloc([4, 2], F32)
    o_sb = A.alloc([256], F32)
    sq = A.alloc([256], F32)
    ss4 = A.alloc([4], F32)
    rs4 = A.alloc([4], F32)
    sil = A.alloc([256], F32)
    GS = os.environ.get('GLA_STOP')
    P.dma('sp', uts[0], C.U[0:128, c0:c0 + W])
    for t in range(NT):
        ut = uts[t % 2]
        if t + 1 < NT:
            P.dma('sp', uts[(t + 1) % 2], C.U[(t + 1) * 128:(t + 2) * 128, c0:c0 + W])
        Q = ut[:, qo:qo + NCH]
        if hgrn:
            P.act(e_, ut[:, ko:ko + NCH], AF.Exp, scale=-1.0)
            P.ts(e_, e_, 1.0, None, ALU.add)
            P.recip(e_, e_)
            P.tt(e_, e_, oml, ALU.mult)
            P.tt(e_, e_, lb, ALU.add)
            P.act(la, e_, AF.Ln)
            P.ts(kk, e_, -1.0, 1.0, ALU.mult, ALU.add)
            K = kk
        else:
            pg = C.bank(0)[0:16, 0:128]
            P.tr(pg, ut[:, lro:lro + 16], C.identf)
            P.copy(glrT[0:16, :], pg, 'dve')
            pz = C.bank(1)[:, 0:128]
            P.mm(pz, glrT[0:33, :], w_aug[0:33, :])
            P.act(e_, pz, AF.Exp, scale=-1.0)
            P.act(e_, e_, AF.Ln, bias=1.0)
            P.ts(la, e_, -1.0 / 16.0, None, ALU.mult)
            K = ut[:, ko:ko + NCH]
        if GS == 'la':
            continue
        pB = [C.bank(2 + i)[:, 0:NCH] for i in range(3)]
        P.mm(pB[0], L1, la)
        P.mm(pB[1], L2, la)
        P.mm(pB[2], L3, la)
        pdec = C.bank(0)[:, 256:264].rearrange("p (a b) -> p a b", a=4)
        for h in range(4):
            P.mm(pdec[0:DK, h, :], la[:, h * DK:(h + 1) * DK], IND, start=(h == 0), stop=True)
        P.act(E[:, 0, :], pB[0], AF.Exp)
        P.act(E[:, 1, :], pB[1], AF.Exp)
        P.act(E[:, 2, :], pB[1], AF.Exp, scale=-1.0)
        P.act(E[:, 3, :], pB[2], AF.Exp)
        P.act(DEC[0:DK], pdec[0:DK], AF.Exp)
        if GS == 'cum':
            continue
        P.stt(qt16[:, 0, :], Q, qscale, E[:, 0, :], ALU.mult, ALU.mult)
        P.stt(qt16[:, 1, :], Q, qscale, E[:, 1, :], ALU.mult, ALU.mult)
        P.tt(qt16[:, 2, :], K, E[:, 2, :], ALU.mult)
        P.tt(ks16, K, E[:, 3, :], ALU.mult)
        P.copy(v16, ut[:, vo:vo + 256], 'pool')
        pTa = C.bankbf(5).rearrange("p (a b) -> p a b", a=8)
        pTb = C.bankbf(6).rearrange("p (a b) -> p a b", a=8)
        for wi in range(3):
            for h in range(4):
                idx = wi * 4 + h
                dstp = pTa[0:DK, idx, :] if idx < 8 else pTb[0:DK, idx - 8, :]
                P.tr(dstp, qt16[:, wi, h * DK:(h + 1) * DK], C.ident)
        P.copy(T16[0:DK, 0:8, :], pTa[0:DK, 0:8, :], 'act')
        P.copy(T16[0:DK, 8:12, :], pTb[0:DK, 0:4, :], 'act')
        if GS == 'tr':
            continue
        pA = C.bank(7).rearrange("p (a b) -> p a b", a=4)
        for h in range(4):
            P.mm(pA[:, h, :], T16[0:DK, 8 + h, :], T16[0:DK, 4 + h, :], start=(h == 0), stop=True)
        AT = ATs[t % 2]
        P.copy(Af32, pA, 'act')
        P.asel(AT[0:64, :, 0:64], Af32[0:64, :, 0:64], [[0, 4], [1, 64]], 0, -1, 0.0)
        P.asel(AT[64:128, :, 64:128], Af32[64:128, :, 64:128], [[0, 4], [1, 64]], 0, -1, 0.0)
        if GS == 'A':
            continue
        pU = [C.bank(1)[:, 0:256].rearrange("p (a b) -> p a b", a=4), C.bank(2)[:, 0:256].rearrange("p (a b) -> p a b", a=4)]
        for c in range(2):
            cr = slice(64 * c, 64 * c + 64)
            for h in range(4):
                P.mm(pU[c][0:DK, h, :], ks16[cr, h * DK:(h + 1) * DK], v16[cr, h * 64:(h + 1) * 64], start=(h == 0), stop=True)
        for c in range(2):
            n = 2 * t + c
            P.tt(S32[0:DK], S32[0:DK], DEC[0:DK, :, c:c + 1].to_broadcast([DK, 4, 64]), ALU.mult)
            P.tt(S32[0:DK], S32[0:DK], pU[c][0:DK], ALU.add)
            P.copy(ring[n % 4][0:DK], S32[0:DK], 'pool')
        if GS == 'U':
            continue
        pO = C.bank(3)[:, 0:256].rearrange("p (a b) -> p a b", a=4)
        for h in range(4):
            P.mm(pO[:, h, :], AT[:, h, :], v16[:, h * 64:(h + 1) * 64], start=(h == 0), stop=False)
        for h in range(4):
            for c in range(2):
                n_prev = 2 * t + c - 1
                if n_prev < 0:
                    continue
                P.mm(pO[64 * c:64 * c + 64, h, :], T16[0:DK, h, 64 * c:64 * c + 64], ring[n_prev % 4][0:DK, h, :], start=False, stop=True)
        if GS == 'O':
            continue
        P.copy(o_sb, pO.rearrange("p a b -> p (a b)"), 'act')
        P.tt(sq, o_sb, o_sb, ALU.mult)
        P.reduce(ss4, sq.rearrange("p (a b) -> p a b", a=4))
        rstd_from_ss(P, rs4, ss4, 64)
        P.act(sil, ut[:, ogo:ogo + 256], AF.Silu)
        P.tt(sil.rearrange("p (a b) -> p a b", a=4), sil.rearrange("p (a b) -> p a b", a=4), gn.unsqueeze(1).to_broadcast([128, 4, 64]), ALU.mult)
        P.tt(o_sb.rearrange("p (a b) -> p a b", a=4), o_sb.rearrange("p (a b) -> p a b", a=4), rs4.unsqueeze(2).to_broadcast([128, 4, 64]), ALU.mult)
        P.tt(MIXC[:, t, :], o_sb, sil, ALU.mult)
    P.dma('sp', C.MIX[:, mixcol:mixcol + 256].rearrange("(t p) c -> p t c", p=128), MIXC)
    A.reset(m0)


def phase_out(C, L, src, dst):
    P, A, I = C.P, C.A, C.I
    S, NT = C.S, C.NT
    moe = (L % 2 == 1)
    NBLK = min(S, 1024)
    NB5 = NBLK // 512
    TPB = NBLK // 128
    m0 = A.mark()
    wout = A.alloc([8, D], BF16)
    for kc in range(8):
        P.dma('pool', wout[:, kc, :], I['w_out'][L, kc * 128:(kc + 1) * 128, :])
    gbc = A.alloc([D], F32)
    bcast_row(C, gbc, I['norm_ffn'][L], D)
    if moe:
        rt32 = A.alloc([8, 8], F32)
        P.dma('sp', rt32, I['moe_router'][0].rearrange("(kc p) e -> p kc e", p=128))
        lg = A.alloc([8], F32)
        v8 = A.alloc([8], F32)
        ex = A.alloc([8], F32)
        sm = A.alloc([4], F32)
        msk = A.alloc([8], F32)
        comb = A.alloc([8], F32)
        combT = A.alloc([128], F32, parts=8)
    hts = [A.alloc([D], F32) for _ in range(2)]
    mts = [A.alloc([D], BF16) for _ in range(2)]
    mT = A.alloc([8, 128], BF16)
    h1 = A.alloc([D], F32)
    junk = A.alloc([D], BF16)
    c32 = A.alloc([D], F32)
    cT32 = A.alloc([8, 128], F32)
    cT16 = A.alloc([8, 128], BF16)
    ss = A.alloc([1], F32)
    rs = A.alloc([1], F32)
    P.dma('sp', hts[0], src[0:128, :])
    P.dma('sp', mts[0], C.MIX[0:128, :])
    for t in range(NT):
        ht, mt = hts[t % 2], mts[t % 2]
        if t + 1 < NT:
            P.dma('sp', hts[(t + 1) % 2], src[(t + 1) * 128:(t + 2) * 128, :])
            P.dma('sp', mts[(t + 1) % 2], C.MIX[(t + 1) * 128:(t + 2) * 128, :])
        pT = C.bankbf(0).rearrange("p (a b) -> p a b", a=8)
        for c in range(8):
            P.tr(pT[:, c, :], mt[:, c * 128:(c + 1) * 128], C.ident)
        P.copy(mT, pT, 'act')
        for hf in range(2):
            pb = C.bank(1 + hf)
            for kc in range(8):
                P.mm(pb, mT[:, kc, :], wout[:, kc, hf * 512:(hf + 1) * 512], start=(kc == 0), stop=(kc == 7))
            P.tt(h1[:, hf * 512:(hf + 1) * 512], pb, ht[:, hf * 512:(hf + 1) * 512], ALU.add)
        P.dma('sp', dst[t * 128:(t + 1) * 128, :], h1)
        P.act(junk, h1, AF.Square, accum=ss)
        rstd_from_ss(P, rs, ss, D)
        P.stt(c32, h1, rs[:, 0:1], gbc, ALU.mult, ALU.mult)
        for hf in range(2):
            pc = C.bank(3 + hf).rearrange("p (a b) -> p a b", a=4)
            for c in range(4):
                P.tr(pc[:, c, :], c32[:, (hf * 4 + c) * 128:(hf * 4 + c + 1) * 128], C.identf)
            P.copy(cT16[:, hf * 4:(hf + 1) * 4, :], pc, 'act')
            if moe:
                P.copy(cT32[:, hf * 4:(hf + 1) * 4, :], pc, 'dve')
        P.dma('sp', C.CTD[:, :, t * 128:(t + 1) * 128], cT16)
        if moe:
            pl = C.bank(5)[:, 0:8]
            for kc in range(8):
                P.mm(pl, cT32[:, kc, :], rt32[:, kc, :], start=(kc == 0), stop=(kc == 7))
            P.copy(lg, pl, 'dve')
            P.op('dve', lambda e: e.max(out=v8, in_=lg), [lg], [v8])
            P.ts(sm[:, 0:1], v8[:, 0:1], -1.0, None, ALU.mult)
            P.act(ex, lg, AF.Exp, bias=sm[:, 0:1])
            P.act(sm[:, 1:2], v8[:, 1:2], AF.Exp, bias=sm[:, 0:1])
            P.ts(sm[:, 2:3], sm[:, 1:2], 1.0, None, ALU.add)
            P.recip(sm[:, 2:3], sm[:, 2:3])
            P.ts(msk, lg, v8[:, 1:2], None, ALU.is_ge)
            P.stt(comb, ex, sm[:, 2:3], msk, ALU.mult, ALU.mult)
            pcm = C.bank(6)[0:8, 0:128]
            P.tr(pcm, comb, C.identf)
            P.copy(combT, pcm, 'dve')
            P.dma('sp', C.COMB[:, t * 128:(t + 1) * 128], combT)
    A.reset(m0)

    wpg = A.alloc([8, D], BF16)
    for kc in range(8):
        P.dma('pool', wpg[:, kc, :], I['ple_w_gate'][L, kc * 128:(kc + 1) * 128, :])
    wpp = A.alloc([2, D], BF16)
    for j in range(2):
        P.dma('pool', wpp[:, j, :], I['ple_w_proj'][L, j * 128:(j + 1) * 128, :])
    gpT = A.alloc([8], F32)
    P.dma('sp', gpT, I['ple_norm'][L].rearrange("(c p) -> p c", p=128), allow_slow_non_contiguous=True)
    CT = A.alloc([8, NBLK], BF16)
    hidden = A.alloc([28, NBLK], BF16)
    accT = A.alloc([8, NBLK], F32)
    wgs = [A.alloc([8, 256], BF16) for _ in range(2)]
    wus = [A.alloc([8, 256], BF16) for _ in range(2)]
    wds = [A.alloc([14, 128], BF16) for _ in range(2)]
    sg = [A.alloc([512], BF16) for _ in range(2)]
    tmpo = A.alloc([512], F32)
    if moe:
        sel_all = A.alloc([8, 128], F32, parts=8)
        P.memset(sel_all, 1.0)
        P.asel(sel_all, sel_all, [[1, 8], [0, 128]], 0, -1, 0.0, cmp=ALU.is_equal)
        cmbT = A.alloc([NBLK], F32, parts=8)
        cbc = A.alloc([NBLK], F32)
        WG = lambda e: I['moe_w_gate'][0, e]
        WU = lambda e: I['moe_w_up'][0, e]
        WD = lambda e: I['moe_w_down'][0, e]
        NE = 8
    else:
        WG = lambda e: I['ffn_w_gate'][0]
        WU = lambda e: I['ffn_w_up'][0]
        WD = lambda e: I['ffn_w_down'][0]
        NE = 1
    h1t = A.alloc([D], F32)
    h2 = h1t
    hn = A.alloc([D], BF16)
    aT = A.alloc([8, 128], BF16)
    gate = A.alloc([D], F32)
    pt32 = A.alloc([256], F32)
    pt16 = A.alloc([256], BF16)
    ppT = A.alloc([2, 128], BF16)
    junk = gate[:, 0:512].bitcast(BF16)
    ss = A.alloc([1], F32)
    rs = A.alloc([1], F32)

    def load_gu(e, fg, buf):
        wgv = WG(e).rearrange("(kc p) f -> p kc f", p=128)
        wuv = WU(e).rearrange("(kc p) f -> p kc f", p=128)
        P.dma('pool', wgs[buf], wgv[:, :, fg * 256:(fg + 1) * 256])
        P.dma('pool', wus[buf], wuv[:, :, fg * 256:(fg + 1) * 256])

    def load_d(e, dh, buf):
        dc, hf_ = dh // 2, dh % 2
        wdv = WD(e).rearrange("(f p) d -> p f d", p=128)
        for f0 in range(0, 14, 7):
            P.dma('pool', wds[buf][:, f0:f0 + 7, :], wdv[:, hf_ * 14 + f0:hf_ * 14 + f0 + 7, dc * 128:(dc + 1) * 128])

    gi = 0
    di = 0
    for blk in range(S // NBLK):
        T0 = blk * NBLK
        P.dma('sp', CT, C.CTD[:, :, T0:T0 + NBLK])
        if moe:
            P.dma('sp', cmbT, C.COMB[:, T0:T0 + NBLK])
        for e in range(NE):
            if moe:
                for nb in range(NB5):
                    pbc = C.bank(6 + nb % 2)
                    P.mm(pbc, sel_all[0:8, e, :], cmbT[0:8, nb * 512:(nb + 1) * 512])
                    P.copy(cbc[:, nb * 512:(nb + 1) * 512], pbc, 'act')
            load_gu(e, 0, gi % 2)
            for fg in range(14):
                if fg + 1 < 14:
                    load_gu(e, fg + 1, (gi + 1) % 2)
                wg, wu = wgs[gi % 2], wus[gi % 2]
                gi += 1
                for fc in range(2):
                    f = fg * 2 + fc
                    for nb in range(NB5):
                        pg = C.bank(0 + (f * NB5 + nb) % 2)
                        pu = C.bank(2 + (f * NB5 + nb) % 2)
                        for kc in range(8):
                            P.mm(pg, wg[:, kc, fc * 128:(fc + 1) * 128], CT[:, kc, nb * 512:(nb + 1) * 512], start=(kc == 0), stop=(kc == 7))
                        for kc in range(8):
                            P.mm(pu, wu[:, kc, fc * 128:(fc + 1) * 128], CT[:, kc, nb * 512:(nb + 1) * 512], start=(kc == 0), stop=(kc == 7))
                        sgb = sg[(f * NB5 + nb) % 2]
                        P.act(sgb, pg, AF.Silu)
                        P.tt(hidden[:, f, nb * 512:(nb + 1) * 512], sgb, pu, ALU.mult)
            load_d(e, 0, di % 2)
            for dc in range(8):
                pos_ = [C.bank(4 + nb) for nb in range(NB5)]
                for hf_ in range(2):
                    dh = dc * 2 + hf_
                    if dh + 1 < 16:
                        load_d(e, dh + 1, (di + 1) % 2)
                    wd = wds[di % 2]
                    di += 1
                    for nb in range(NB5):
                        for f in range(14):
                            ff = hf_ * 14 + f
                            P.mm(pos_[nb], wd[:, f, :], hidden[:, ff, nb * 512:(nb + 1) * 512], start=(ff == 0), stop=(ff == 27))
                for nb in range(NB5):
                    po = pos_[nb]
                    dstp = accT[:, dc, nb * 512:(nb + 1) * 512]
                    if not moe:
                        P.copy(dstp, po, 'act')
                    elif e == 0:
                        P.tt(dstp, po, cbc[:, nb * 512:(nb + 1) * 512], ALU.mult)
                    else:
                        P.tt(tmpo, po, cbc[:, nb * 512:(nb + 1) * 512], ALU.mult)
                        P.tt(dstp, dstp, tmpo, ALU.add)
        for tl in range(TPB):
            t = blk * TPB + tl
            rowsl = slice(t * 128, (t + 1) * 128)
            P.dma('sp', h1t, dst[rowsl, :])
            P.dma('sp', pt32, I['p'][L, rowsl, :])
            for hf in range(2):
                pc = C.bank(hf).rearrange("p (a b) -> p a b", a=4)
                for c in range(4):
                    P.tr(pc[:, c, :], accT[:, hf * 4 + c, tl * 128:(tl + 1) * 128], C.identf)
                P.tt(h2[:, hf * 512:(hf + 1) * 512], pc.rearrange("p a b -> p (a b)"), h1t[:, hf * 512:(hf + 1) * 512], ALU.add)
            P.act(junk, h2, AF.Square, accum=ss)
            rstd_from_ss(P, rs, ss, D)
            P.ts(hn, h2, rs[:, 0:1], None, ALU.mult)
            pT = C.bankbf(2).rearrange("p (a b) -> p a b", a=8)
            for c in range(8):
                P.tr(pT[:, c, :], hn[:, c * 128:(c + 1) * 128], C.ident)
            P.tt(aT, pT, gpT.unsqueeze(2).to_broadcast([128, 8, 128]), ALU.mult)
            P.copy(pt16, pt32, 'act')
            pP = C.bankbf(3).rearrange("p (a b) -> p a b", a=8)
            for j in range(2):
                P.tr(pP[:, j, :], pt16[:, j * 128:(j + 1) * 128], C.ident)
            P.copy(ppT, pP[:, 0:2, :], 'act')
            for hf in range(2):
                pgt = C.bank(4 + hf)
                for kc in range(8):
                    P.mm(pgt, aT[:, kc, :], wpg[:, kc, hf * 512:(hf + 1) * 512], start=(kc == 0), stop=(kc == 7))
                P.act(gate[:, hf * 512:(hf + 1) * 512], pgt, AF.Sigmoid)
                ppp = C.bank(6 + hf)
                for j in range(2):
                    P.mm(ppp, ppT[:, j, :], wpp[:, j, hf * 512:(hf + 1) * 512], start=(j == 0), stop=(j == 1))
                P.tt(gate[:, hf * 512:(hf + 1) * 512], ppp, gate[:, hf * 512:(hf + 1) * 512], ALU.mult)
            P.tt(h2, h2, gate, ALU.add)
            P.dma('sp', dst[rowsl, :], h2)
    A.reset(m0)


_CACHE = {}


def kernel(**inputs):
    S = 4096
    if 'nc' not in _CACHE:
        _CACHE['nc'] = build(S)
    nc = _CACHE['nc']
    names = [k for k in inputs if k not in ('x', 'p')]
    in_maps = []
    for b in range(8):
        mp = {'x': np.ascontiguousarray(inputs['x'][b], dtype=np.float32),
              'p': np.ascontiguousarray(inputs['p'][:, b], dtype=np.float32)}
        for k in names:
            mp[k] = np.ascontiguousarray(inputs[k], dtype=np.float32)
        in_maps.append(mp)
    res = run_bass_kernel_spmd(nc, in_maps, core_ids=list(range(8)))
    out = np.stack([np.asarray(r["y"], dtype=np.float32) for r in res.results], axis=0)
    return out
```

```python
import math
import os
import contextlib
import numpy as np
import concourse.bass as bass
import concourse.mybir as mybir
from concourse.bass_utils import run_bass_kernel_spmd
from concourse.alu_op_type import AluOpType as ALU

F32 = mybir.dt.float32
BF16 = mybir.dt.bfloat16
I32 = mybir.dt.int32
AF = mybir.ActivationFunctionType
AX = mybir.AxisListType

NSLOT = 8
D = 1024
INC = 3228
DFF = 3584
NEGM = -30000.0
EPS = 1e-6


def box_of(ap):
    t = ap.tensor
    rowlen = 1
    for s in list(t.shape)[1:]:
        rowlen *= int(s)
    if str(ap.space) != 'DRAM':
        rowlen = int(ap.ap[0][0]) if len(ap.ap) > 0 and int(ap.ap[0][0]) > 0 else rowlen
    off = int(ap.offset)
    p0 = off // rowlen
    f0 = off % rowlen
    pe = 0
    fe = 0
    for (st, cnt) in ap.ap:
        st = int(st); cnt = int(cnt)
        if cnt <= 1 or st == 0:
            continue
        if st % rowlen == 0:
            pe += (st // rowlen) * (cnt - 1)
        else:
            fe += abs(st) * (cnt - 1)
    sz = mybir.dt.size(ap.dtype)
    if str(ap.space) == 'PSUM':
        b0 = (f0 * sz) // 2048 * 2048
        b1 = ((f0 + fe + 1) * sz + 2047) // 2048 * 2048
        q0 = p0 // 32 * 32
        q1 = (p0 + pe + 1 + 31) // 32 * 32
        return (t.name, q0, q1, b0, b1)
    return (t.name, p0, p0 + pe + 1, f0 * sz, (f0 + fe + 1) * sz)


def overlap(a, b):
    return a[1] < b[2] and b[1] < a[2] and a[3] < b[4] and b[3] < a[4]


def covers(a, b):
    return a[1] <= b[1] and a[2] >= b[2] and a[3] <= b[3] and a[4] >= b[4]


def isap(v):
    return v is not None and not isinstance(v, (int, float))


class Prog:
    def __init__(self, nc):
        self.nc = nc
        self.ops = []
        self.track = {}
        self.dma_n = {}

    def op(self, eng, fn, reads=(), writes=(), dma=False):
        idx = len(self.ops)
        rb = [box_of(a) for a in reads]
        wb = [box_of(a) for a in writes]
        wb = wb + [b for b in rb if b[0] == 'psum']
        rb = [b for b in rb if b[0] != 'psum']
        deps = set()
        for b in rb:
            for e in self.track.setdefault(b[0], []):
                if e[3] and overlap(e[0], b):
                    deps.add(e[1])
        for b in wb:
            for e in self.track.setdefault(b[0], []):
                if overlap(e[0], b):
                    deps.add(e[1])
        for b in wb:
            lst = self.track[b[0]]
            lst[:] = [e for e in lst if not covers(b, e[0])]
            lst.append((b, idx, eng, True, dma))
        for b in rb:
            lst = self.track[b[0]]
            lst[:] = [e for e in lst if not (e[2] == eng and (not e[3]) and (not e[4]) and (not dma) and covers(b, e[0]))]
            lst.append((b, idx, eng, False, dma))
        deps.discard(idx)
        o = dict(eng=eng, fn=fn, deps=deps, dma=dma, signal=dma)
        if dma:
            n = self.dma_n.get(eng, 0)
            self.dma_n[eng] = n + 1
            o['slot'] = n % NSLOT
            o['val'] = 16 * (n // NSLOT + 1)
            o['dma_idx'] = n
        self.ops.append(o)
        for d in deps:
            p = self.ops[d]
            if p['eng'] == eng and not p['dma'] and not dma and eng == 'pe':
                continue
            p['signal'] = True
        return idx

    def emit(self):
        nc = self.nc
        engs = ['pe', 'act', 'dve', 'pool', 'sp']
        with contextlib.ExitStack() as st:
            csem = {e: st.enter_context(nc.semaphore('c_' + e)) for e in engs}
            dsem = {}
            for e in self.dma_n:
                dsem[e] = [st.enter_context(nc.semaphore('d_%s_%d' % (e, i))) for i in range(NSLOT)]
            cnt = {e: 0 for e in engs}
            for o in self.ops:
                if o['dma']:
                    o['sem'] = dsem[o['eng']][o['slot']]
                elif o['signal']:
                    cnt[o['eng']] += 1
                    o['sem'] = csem[o['eng']]
                    o['val'] = cnt[o['eng']]
            per = {e: [] for e in engs}
            for i, o in enumerate(self.ops):
                per[o['eng']].append(i)
            self.max_cnt = dict(cnt)
            block = st.enter_context(nc.Block())
            ops = self.ops

            def run(ename, engine):
                seen = {}
                for i in per[ename]:
                    o = ops[i]
                    waits = {}
                    for d in o['deps']:
                        p = ops[d]
                        if p['eng'] == ename and not p['dma'] and not o['dma'] and ename == 'pe':
                            continue
                        key = id(p['sem'])
                        if key not in waits or waits[key][1] < p['val']:
                            waits[key] = (p['sem'], p['val'])
                    if o['dma'] and o['dma_idx'] >= NSLOT:
                        s = o['sem']
                        v = o['val'] - 16
                        key = id(s)
                        if key not in waits or waits[key][1] < v:
                            waits[key] = (s, v)
                    for key, (s, v) in waits.items():
                        if seen.get(key, 0) >= v:
                            continue
                        engine.wait_ge(s, v)
                        seen[key] = v
                    ins = o['fn'](engine)
                    if o['signal']:
                        ins.then_inc(o['sem'], 16 if o['dma'] else 1)
                if ename in dsem:
                    n = self.dma_n[ename]
                    for sl in range(NSLOT):
                        k = (n - sl + NSLOT - 1) // NSLOT
                        if k > 0:
                            engine.wait_ge(dsem[ename][sl], 16 * k)

            block.tensor(lambda e: run('pe', e))
            block.scalar(lambda e: run('act', e))
            block.vector(lambda e: run('dve', e))
            block.gpsimd(lambda e: run('pool', e))
            block.sync(lambda e: run('sp', e))

    def dma(self, q, out, in_, **kw):
        return self.op(q, lambda e: e.dma_start(out=out, in_=in_, **kw), [in_], [out], dma=True)

    def mm(self, out, lhsT, rhs, start=True, stop=True):
        return self.op('pe', lambda e: e.matmul(out=out, lhsT=lhsT, rhs=rhs, start=start, stop=stop, skip_group_check=True), [lhsT, rhs], [out])

    def tr(self, out, in_, ident):
        return self.op('pe', lambda e: e.transpose(out=out, in_=in_, identity=ident), [in_, ident], [out])

    def act(self, out, in_, func, bias=None, scale=None, accum=None):
        kw = {}
        rd = [in_]
        wr = [out]
        if bias is not None:
            kw['bias'] = bias
            if isap(bias):
                rd.append(bias)
        if scale is not None:
            kw['scale'] = scale
            if isap(scale):
                rd.append(scale)
        if accum is not None:
            kw['accum_out'] = accum
            wr.append(accum)
        return self.op('act', lambda e: e.activation(out=out, in_=in_, func=func, **kw), rd, wr)

    def ts(self, out, in0, s1, s2=None, op0=ALU.mult, op1=None, eng='dve', accum=None):
        rd = [in0] + [s for s in (s1, s2) if isap(s)]
        wr = [out] + ([accum] if accum is not None else [])
        kw = {}
        if op1 is not None:
            kw['op1'] = op1
        if accum is not None:
            kw['accum_out'] = accum
        return self.op(eng, lambda e: e.tensor_scalar(out=out, in0=in0, scalar1=s1, scalar2=s2, op0=op0, **kw), rd, wr)

    def tt(self, out, in0, in1, op, eng='dve'):
        return self.op(eng, lambda e: e.tensor_tensor(out=out, in0=in0, in1=in1, op=op), [in0, in1], [out])

    def stt(self, out, in0, scalar, in1, op0, op1):
        rd = [in0, in1] + ([scalar] if isap(scalar) else [])
        return self.op('dve', lambda e: e.scalar_tensor_tensor(out=out, in0=in0, scalar=scalar, in1=in1, op0=op0, op1=op1), rd, [out])

    def copy(self, out, in_, eng='dve'):
        if eng == 'act':
            return self.op('act', lambda e: e.copy(out=out, in_=in_), [in_], [out])
        return self.op(eng, lambda e: e.tensor_copy(out=out, in_=in_), [in_], [out])

    def memset(self, ap, val, eng='pool'):
        return self.op(eng, lambda e: e.memset(ap, val), [], [ap])

    def reduce(self, out, in_, op=ALU.add, axis=AX.X):
        return self.op('dve', lambda e: e.tensor_reduce(out=out, in_=in_, axis=axis, op=op), [in_], [out])

    def recip(self, out, in_):
        return self.op('dve', lambda e: e.reciprocal(out=out, in_=in_), [in_], [out])

    def asel(self, out, in_, pattern, base, cm, fill, cmp=ALU.is_ge):
        return self.op('pool', lambda e: e.affine_select(out=out, in_=in_, pattern=pattern, compare_op=cmp, fill=fill, base=base, channel_multiplier=cm), [in_], [out])


class Arena:
    def __init__(self, t, nwords):
        self.t = t
        self.n = nwords
        self.off = 0
        self.peak = 0

    def mark(self):
        return self.off

    def reset(self, m):
        self.off = m

    def alloc(self, free_shape, dtype, parts=128):
        n = 1
        for s in free_shape:
            n *= s
        sz = mybir.dt.size(dtype)
        words = (n * sz + 3) // 4
        words = (words + 7) // 8 * 8
        assert self.off + words <= self.n, "arena overflow %d+%d>%d" % (self.off, words, self.n)
        ap = self.t[0:parts, self.off:self.off + words]
        self.off += words
        self.peak = max(self.peak, self.off)
        if dtype != F32:
            ap = ap.bitcast(dtype)
        ap = ap[:, 0:n]
        if len(free_shape) == 2:
            ap = ap.rearrange("p (a b) -> p a b", a=free_shape[0])
        elif len(free_shape) == 3:
            ap = ap.rearrange("p (a b c) -> p a b c", a=free_shape[0], b=free_shape[1])
        return ap


def rstd_from_ss(P, out, ss, n):
    P.act(out, ss, AF.Sqrt, bias=EPS, scale=1.0 / n)
    P.recip(out, out)


class Ctx:
    pass


def build(S=4096, dbg=None, phases=None):
    NT = S // 128
    NQB = S // 512
    NSEL = S // 64
    NCMP = S // 16 - 1
    NBT = (NCMP + 127) // 128
    nc = bass.Bass("TRN2", target_bir_lowering=False)

    def din(name, shape):
        return nc.dram_tensor(name, shape, F32, kind="ExternalInput").ap()

    I = {}
    I['x'] = din("x", [S, D])
    I['p'] = din("p", [2, S, 256])
    for name, shape in [("norm_attn", [2, D]), ("w_in", [2, D, INC]), ("w_out", [2, D, D]),
                        ("nsa_cmp_pos", [2, 2, 32, 64]), ("nsa_cmp_w1", [2, 2, 2048, 256]), ("nsa_cmp_w2", [2, 2, 256, 64]),
                        ("nsa_qk_gain", [2, 4, 64]), ("diff_qk_gain", [2, 2, 32]), ("diff_lambda", [2, 4, 32]),
                        ("diff_norm", [2, 64]), ("gla_w_gate2", [2, 16, 128]), ("gla_b_gate", [2, 128]), ("gla_norm", [2, 64]),
                        ("hgrn_lb_logits", [2, 256]), ("hgrn_norm", [2, 64]), ("norm_ffn", [2, D]),
                        ("ffn_w_gate", [1, D, DFF]), ("ffn_w_up", [1, D, DFF]), ("ffn_w_down", [1, DFF, D]),
                        ("moe_router", [1, D, 8]), ("moe_w_gate", [1, 8, D, DFF]), ("moe_w_up", [1, 8, D, DFF]),
                        ("moe_w_down", [1, 8, DFF, D]), ("ple_norm", [2, D]), ("ple_w_gate", [2, D, D]), ("ple_w_proj", [2, 256, D])]:
        I[name] = din(name, shape)
    Y = nc.dram_tensor("y", [S, D], F32, kind="ExternalOutput").ap()

    def scratch(name, shape, dt):
        kind = "ExternalOutput" if (dbg and name in dbg) else "Internal"
        return nc.dram_tensor(name, shape, dt, kind=kind).ap()

    U = scratch("U", [S, INC], F32)
    MIX = scratch("MIX", [S, D], BF16)
    H = scratch("H", [S, D], F32)
    CTD = scratch("CTD", [128, 8, S], BF16)
    COMB = scratch("COMB", [8, S], F32)

    with contextlib.ExitStack() as st:
        ARW = 49 * 1024
        arena_t = st.enter_context(nc.sbuf_tensor("arena", [128, ARW], F32))
        psum = st.enter_context(nc.psum_tensor("psum", [128, 8, 512], F32))
        P = Prog(nc)
        A = Arena(arena_t, ARW)
        C = Ctx()
        C.nc, C.P, C.A, C.psum, C.I = nc, P, A, psum, I
        C.S, C.NT, C.NQB, C.NSEL, C.NCMP, C.NBT = S, NT, NQB, NSEL, NCMP, NBT
        C.U, C.MIX, C.H, C.Y = U, MIX, H, Y
        C.CTD, C.COMB = CTD, COMB
        C.bank = lambda b: psum[:, b, :]
        C.bankbf = lambda b: psum[:, b, :].bitcast(BF16)

        C.identf = A.alloc([128], F32)
        C.ident = A.alloc([128], BF16)
        P.memset(C.identf, 1.0)
        P.asel(C.identf, C.identf, [[1, 128]], 0, -1, 0.0, cmp=ALU.is_equal)
        P.copy(C.ident, C.identf, 'dve')
        C.ones_row = A.alloc([128], F32, parts=1)
        P.memset(C.ones_row, 1.0)
        C.Cm = A.alloc([4, 512], BF16)
        C.Cw = A.alloc([4, 512], BF16)
        tmpf = A.alloc([512], F32)
        for r in range(4):
            P.memset(tmpf, 0.0)
            P.asel(tmpf, tmpf, [[1, 512]], -128 * r, -1, NEGM)
            P.copy(C.Cm[:, r, :], tmpf, 'dve')
            P.memset(tmpf, 0.0)
            P.asel(tmpf, tmpf, [[-1, 512]], 128 * r - 1, 1, NEGM)
            P.copy(C.Cw[:, r, :], tmpf, 'dve')

        ph = phases or ['inproj', 'nsa', 'diff', 'gla', 'hgrn', 'out']
        for L in range(2):
            src = I['x'] if L == 0 else H
            dst = H if L == 0 else Y
            if ('inproj', L) in ph or 'inproj' in ph:
                phase_inproj(C, L, src)
            if 'nsa' in ph:
                phase_nsa(C, L)
            if 'diff' in ph:
                phase_diff(C, L)
            if 'gla' in ph:
                phase_gla(C, L, hgrn=False)
            if 'hgrn' in ph:
                phase_gla(C, L, hgrn=True)
            if 'out' in ph:
                phase_out(C, L, src, dst)
            if dbg and dbg.get('stop_after_layer') == L:
                break
        P.emit()
        C.peak = A.peak
    nc._ctx = C
    return nc


def load_cast(P, out, in_, q='pool'):
    n = out.shape[-1]
    if n <= 2048:
        P.dma(q, out, in_)
    else:
        k = (n + 2047) // 2048
        step = (n + k - 1) // k
        for c0 in range(0, n, step):
            c1 = min(n, c0 + step)
            P.dma(q, out[..., c0:c1], in_[..., c0:c1])


def norm_transpose(C, xt, gT, hn, aT, tb, ss, rs, junk):
    P = C.P
    P.act(junk, xt, AF.Square, accum=ss)
    rstd_from_ss(P, rs, ss, D)
    P.ts(hn, xt, rs[:, 0:1], None, ALU.mult)
    pT = C.bankbf(tb).rearrange("p (a b) -> p a b", a=8)
    for c in range(8):
        P.tr(pT[:, c, :], hn[:, c * 128:(c + 1) * 128], C.ident)
    P.tt(aT, pT, gT.unsqueeze(2).to_broadcast([128, 8, 128]), ALU.mult)


def phase_inproj(C, L, src):
    P, A, I = C.P, C.A, C.I
    m = A.mark()
    win = A.alloc([8, INC], BF16)
    for kc in range(8):
        load_cast(P, win[:, kc, :], I['w_in'][L, kc * 128:(kc + 1) * 128, :])
    gT = A.alloc([8], F32)
    P.dma('sp', gT, I['norm_attn'][L].rearrange("(c p) -> p c", p=128), allow_slow_non_contiguous=True)
    xts = [A.alloc([D], F32) for _ in range(2)]
    junk = A.alloc([D], BF16)
    hn = A.alloc([D], BF16)
    aTs = [A.alloc([8, 128], BF16) for _ in range(2)]
    us = [A.alloc([INC], F32) for _ in range(2)]
    ss = A.alloc([1], F32)
    rs = A.alloc([1], F32)
    chunks = [(c0, min(INC, c0 + 512)) for c0 in range(0, INC, 512)]
    P.dma('sp', xts[0], src[0:128, :])
    bk = 0
    for t in range(C.NT):
        xt = xts[t % 2]
        if t + 1 < C.NT:
            P.dma('sp', xts[(t + 1) % 2], src[(t + 1) * 128:(t + 2) * 128, :])
        aT = aTs[t % 2]
        norm_transpose(C, xt, gT, hn, aT, t % 2, ss, rs, junk)
        u = us[t % 2]
        for ci, (c0, c1) in enumerate(chunks):
            b = 2 + (bk % 6)
            bk += 1
            pb = C.bank(b)[:, 0:c1 - c0]
            for kc in range(8):
                P.mm(pb, aT[:, kc, :], win[:, kc, c0:c1], start=(kc == 0), stop=(kc == 7))
            if ci % 2 == 0:
                P.copy(u[:, c0:c1], pb, 'act')
            else:
                P.copy(u[:, c0:c1], pb, 'dve')
        P.dma('sp', C.U[t * 128:(t + 1) * 128, :], u)
    A.reset(m)


def run_pipelined(iters, la=2):
    n = len(iters)
    for i in range(n + la):
        if i < n:
            iters[i][0]()
        j = i - la
        if j >= 0:
            iters[j][1]()
            if iters[j][2] is not None:
                iters[j][2]()


def bcast_load(P, out, vec, q='sp'):
    P.dma(q, out, vec.partition_broadcast(128))


def phase_nsa(C, L):
    P, A, I = C.P, C.A, C.I
    S, NT, NQB, NSEL, NCMP, NBT = C.S, C.NT, C.NQB, C.NSEL, C.NCMP, C.NBT
    m0 = A.mark()
    QT = A.alloc([2, S], BF16)
    KsT = A.alloc([S], BF16)
    KwT = A.alloc([S], BF16)
    KVcT = A.alloc([S], BF16)
    Vs = A.alloc([NT, 66], BF16)
    Vw = A.alloc([NT, 66], BF16)
    G = A.alloc([NT, 12], F32)
    MIXA = A.alloc([NT, 256], F32)
    SelT = A.alloc([S], BF16)
    Rall = A.alloc([S], BF16)
    gcol = A.alloc([4], F32)
    gsrc = I['nsa_qk_gain'][L]
    for hh in range(2):
        P.dma('sp', gcol[hh * 64:(hh + 1) * 64, :], gsrc.rearrange("j d -> d j"), allow_slow_non_contiguous=True)
    P.ts(gcol[:, 0:1], gcol[:, 0:1], 0.125, None, ALU.mult)
    P.memset(Vs[:, :, 64:65], 1.0)
    P.memset(Vw[:, :, 64:65], 1.0)
    m1 = A.mark()
    rf = A.alloc([S], F32, parts=64)
    P.memset(rf, 1.0)
    P.asel(rf, rf, [[1, S]], 0, -64, 0.0)
    P.asel(rf, rf, [[-1, S]], 63, 64, 0.0)
    P.copy(Rall[0:64], rf, 'dve')
    P.dma('sp', Rall[64:128, :], Rall[0:64, :])
    A.reset(m1)

    if os.environ.get('NSA_STOP') == '0':
        A.reset(m0)
        return
    m1 = A.mark()
    uts = [A.alloc([652], F32) for _ in range(2)]
    sq = A.alloc([640], F32)
    ssq = A.alloc([10], F32)
    rsq = A.alloc([10], F32)
    nb16 = A.alloc([5, 128], BF16)
    P.dma('sp', uts[0], C.U[0:128, 0:652])
    for t in range(NT):
        ut = uts[t % 2]
        if t + 1 < NT:
            P.dma('sp', uts[(t + 1) % 2], C.U[(t + 1) * 128:(t + 2) * 128, 0:652])
        SK = os.environ.get('NSA_SKIP', '').split(',')
        P.act(sq, ut[:, 0:640], AF.Square)
        P.reduce(ssq, sq.rearrange("p (a b) -> p a b", a=10))
        rstd_from_ss(P, rsq, ssq, 64)
        if 'nb' not in SK:
            P.tt(nb16[:, 0:2, :].rearrange("p a (h d) -> p (a h) d", h=2), ut[:, 0:256].rearrange("p (h d) -> p h d", h=4),
                 rsq[:, 0:4].unsqueeze(2).to_broadcast([128, 4, 64]), ALU.mult)
            for hh in range(2):
                P.ts(nb16[:, 2, hh * 64:(hh + 1) * 64], ut[:, 384:448], rsq[:, 6:7], None, ALU.mult)
                P.ts(nb16[:, 3, hh * 64:(hh + 1) * 64], ut[:, 512:576], rsq[:, 8:9], None, ALU.mult)
        if 'pc' not in SK:
            P.copy(nb16[:, 4, :], ut[:, 256:384], 'pool')
        ts_ = slice(t * 128, (t + 1) * 128)
        if 'tr' not in SK:
            pT = C.bankbf(t % 2).rearrange("p (a b) -> p a b", a=8)
            for c in range(5):
                P.tr(pT[:, c, :], nb16[:, c, :], C.ident)
            if 'ev1' not in SK:
                P.act(QT[:, :, ts_], pT[:, 0:2, :], AF.Identity, scale=gcol[:, 0:1])
            if 'ev2' not in SK:
                P.ts(KsT[:, ts_], pT[:, 2, :], gcol[:, 2:3], None, ALU.mult)
                P.ts(KwT[:, ts_], pT[:, 3, :], gcol[:, 3:4], None, ALU.mult)
            if 'ev3' not in SK:
                P.copy(KVcT[:, ts_], pT[:, 4, :], 'act')
        if 'v' not in SK:
            P.copy(Vs[:, t, 0:64], ut[:, 448:512], 'pool')
            P.copy(Vw[:, t, 0:64], ut[:, 576:640], 'pool')
        if 'g' not in SK:
            P.act(G[:, t, :], ut[:, 640:652], AF.Sigmoid)
    if not os.environ.get('NSA_NORESET'):
        A.reset(m1)

    if os.environ.get('NSA_STOP') == 'a':
        A.reset(m0)
        return
    m1 = A.mark()
    w1 = A.alloc([32, 256], BF16)
    SKB = os.environ.get('NSA_SKIPB', '').split(',')
    for j in range(2):
        w1v = I['nsa_cmp_w1'][L, j].rearrange("(l d) h -> d l h", d=64)
        for l0 in range(0, 32, 8):
            if 'w1' not in SKB:
                P.dma('pool', w1[j * 64:(j + 1) * 64, l0:l0 + 8, :], w1v[:, l0:l0 + 8, :])
    w2 = A.alloc([2, 2, 64], BF16)
    for j in range(2):
        if 'w2' not in SKB:
            P.dma('pool', w2[:, j, :, :], I['nsa_cmp_w2'][L, j].rearrange("(t p) d -> p t d", p=128))
    posl = A.alloc([128], F32, parts=32)
    posT = A.alloc([32], BF16)
    if 'pos' not in SKB:
        for j in range(2):
            P.dma('sp', posl[:, j * 64:(j + 1) * 64], I['nsa_cmp_pos'][L, j])
        ppos = C.bank(2)[:, 0:32]
        P.tr(ppos, posl, C.identf[0:32, 0:32])
        P.copy(posT, ppos, 'dve')
    if os.environ.get('NSA_STOP') == 'b1':
        A.reset(m0)
        return
    hidT = A.alloc([2, 2, 256], BF16)
    biasc = A.alloc([4], F32)
    rhs_c = A.alloc([NBT, 129], F32)
    kcT = A.alloc([NBT * 128], BF16)
    kcn = A.alloc([128], BF16)
    kss = A.alloc([1], F32)
    krs = A.alloc([1], F32)
    kjunk = A.alloc([64], F32)
    for kv in range(2):
        rows = slice(kv * 64, (kv + 1) * 64)
        for ht in range(2):
            pb = C.bank(2 + ht)[:, 0:NCMP]
            for l in range(32):
                P.mm(pb, w1[rows, l, ht * 128:(ht + 1) * 128], KVcT[rows, l:l + 16 * (NCMP - 1) + 1:16], start=(l == 0), stop=(l == 31))
            pbb = C.bank(4 + ht)[:, 0:1]
            for l in range(32):
                P.mm(pbb, w1[rows, l, ht * 128:(ht + 1) * 128], posT[rows, l:l + 1], start=(l == 0), stop=(l == 31))
            bc = biasc[:, kv * 2 + ht:kv * 2 + ht + 1]
            P.copy(bc, pbb, 'dve')
            P.act(hidT[:, kv, ht, 0:NCMP], pb, AF.Silu, bias=bc)
    if os.environ.get('NSA_STOP') == 'b2':
        A.reset(m0)
        return
    for bt in range(NBT):
        nb = min(128, NCMP - bt * 128)
        bs = slice(bt * 128, bt * 128 + nb)
        pk = C.bank(2)[0:nb, 0:64]
        for ht in range(2):
            P.mm(pk, hidT[:, 0, ht, bs], w2[:, 0, ht, :], start=(ht == 0), stop=(ht == 1))
        P.act(kjunk[0:nb], pk, AF.Square, accum=kss[0:nb])
        rstd_from_ss(P, krs[0:nb], kss[0:nb], 64)
        for hh in range(2):
            P.ts(kcn[0:nb, hh * 64:(hh + 1) * 64], pk, krs[0:nb, 0:1], None, ALU.mult)
        pkt = C.bankbf(3)[:, 0:nb]
        P.tr(pkt, kcn[0:nb, :], C.ident[0:nb, 0:nb])
        P.ts(kcT[:, bs], pkt, gcol[:, 1:2], None, ALU.mult)
        pv = C.bank(4)[0:nb, 0:64]
        for ht in range(2):
            P.mm(pv, hidT[:, 1, ht, bs], w2[:, 1, ht, :], start=(ht == 0), stop=(ht == 1))
        P.copy(rhs_c[0:nb, bt, 0:64], pv, 'act')
        ov = rhs_c[:, bt, 64:64 + NSEL]
        P.memset(rhs_c[:, bt, 64:129], 1.0)
        P.asel(ov, ov, [[64, NSEL]], 63 - 16 * 128 * bt, -16, 0.0)
        P.asel(ov, ov, [[-64, NSEL]], 31 + 16 * 128 * bt, 16, 0.0)

    if os.environ.get('NSA_STOP') == 'b':
        A.reset(m0)
        return
    PcT = A.alloc([4, NBT, 512], F32)
    BSb = A.alloc([128], F32)
    BIGV = 1.0e4
    P.memset(BSb, 0.0)
    P.memset(BSb[0:64, 63:65], BIGV)
    P.memset(BSb[0:64, 65:128], -BIGV)
    P.memset(BSb[64:128, 64:66], BIGV)
    P.memset(BSb[64:128, 66:128], -BIGV)
    imp = A.alloc([64], F32)
    imp2 = A.alloc([64], F32)
    m8a = A.alloc([8], F32)
    m8b = A.alloc([8], F32)
    seln = A.alloc([128], BF16)
    P.memset(seln, 0.0)
    den = A.alloc([4], F32)
    cmb = A.alloc([4], F32)
    for tb in range(NQB):
        T0 = tb * 512
        live = []
        for h in range(4):
            hr = slice((h % 2) * 64, (h % 2) * 64 + 64)
            for bt in range(NBT):
                nb = min(128, NCMP - bt * 128)
                if 16 * (128 * bt) + 31 > T0 + 511:
                    continue
                live.append((h, bt))
                pb = C.bank(2 + (h * NBT + bt) % 4)[0:nb, :]
                P.mm(pb, kcT[hr, bt * 128:bt * 128 + nb], QT[hr, h // 2, T0:T0 + 512])
                P.act(PcT[0:nb, h, bt, :], pb, AF.Exp)
                P.asel(PcT[0:nb, h, bt, :], PcT[0:nb, h, bt, :], [[1, 512]], T0 - 2048 * bt - 31, -16, 0.0)
        for qs in range(4):
            t = tb * 4 + qs
            pos = [C.bank(6)[:, 0:258].rearrange("p (a b) -> p a b", a=2), C.bank(7)[:, 0:258].rearrange("p (a b) -> p a b", a=2)]
            for h in range(4):
                bts = [bt for (hh, bt) in live if hh == h]
                o = pos[h // 2][:, h % 2, :]
                if not bts:
                    continue
                for i, bt in enumerate(bts):
                    nb = min(128, NCMP - bt * 128)
                    P.mm(o, PcT[0:nb, h, bt, qs * 128:(qs + 1) * 128], rhs_c[0:nb, bt, :], start=(h % 2 == 0 and i == 0), stop=(i == len(bts) - 1))
            if not live:
                P.memset(MIXA[:, t, :], 0.0, 'dve')
                P.memset(imp[:, 0:NSEL], 0.0, 'dve')
            else:
                for h in range(4):
                    o = pos[h // 2][:, h % 2, :]
                    P.ts(den[:, h:h + 1], o[:, 128:129], 1e-30, None, ALU.max)
                    P.recip(den[:, h:h + 1], den[:, h:h + 1])
                    if h == 0:
                        P.ts(imp[:, 0:NSEL], o[:, 64:64 + NSEL], den[:, 0:1], None, ALU.mult)
                    else:
                        P.stt(imp[:, 0:NSEL], o[:, 64:64 + NSEL], den[:, h:h + 1], imp[:, 0:NSEL], ALU.mult, ALU.add)
                    P.tt(cmb[:, h:h + 1], den[:, h:h + 1], G[:, t, 3 * h:3 * h + 1], ALU.mult)
                    P.ts(MIXA[:, t, h * 64:(h + 1) * 64], o[:, 0:64], cmb[:, h:h + 1], None, ALU.mult)
            if NSEL > 16:
                P.tt(imp[:, 0:NSEL], imp[:, 0:NSEL], BSb[:, 64 - 2 * t:64 - 2 * t + NSEL], ALU.add)
                P.memset(imp[:, 0:1], BIGV, 'dve')
                P.op('dve', lambda e: e.max(out=m8a, in_=imp[:, 0:NSEL]), [imp[:, 0:NSEL]], [m8a])
                P.op('dve', lambda e: e.match_replace(out=imp2[:, 0:NSEL], in_to_replace=m8a, in_values=imp[:, 0:NSEL], imm_value=-1e9), [imp[:, 0:NSEL], m8a], [imp2[:, 0:NSEL]])
                P.op('dve', lambda e: e.max(out=m8b, in_=imp2[:, 0:NSEL]), [imp2[:, 0:NSEL]], [m8b])
                P.ts(seln[:, 0:NSEL], imp[:, 0:NSEL], m8b[:, 7:8], NEGM, ALU.is_lt, ALU.mult)
                P.ts(seln[:, 64:64 + NSEL], imp[:, 0:NSEL], m8b[:, 7:8], NEGM, ALU.is_lt, ALU.mult)
            pst = C.bankbf(t % 2)[:, 0:128]
            P.tr(pst, seln, C.ident)
            P.copy(SelT[:, t * 128:(t + 1) * 128], pst, 'act')
    A.reset(m1)

    if os.environ.get('NSA_STOP') == 'c':
        A.reset(m0)
        return
    m1 = A.mark()
    PTs = [A.alloc([512], BF16) for _ in range(3)]
    rd2 = A.alloc([8], F32)
    pi = 0
    oi = 0
    iters = []
    for branch in range(2):
        KT = KsT if branch == 0 else KwT
        V = Vs if branch == 0 else Vw
        for h in range(4):
            hr = slice((h % 2) * 64, (h % 2) * 64 + 64)
            for qb in range(NQB):
                kts = list(range(0, 4 * qb + 4)) if branch == 0 else list(range(max(0, 4 * qb - 4), 4 * qb + 4))
                ob = C.bank(6 + oi % 2)[:, 0:260].rearrange("p (a b) -> p a b", a=4)
                oi += 1
                first = True
                lastkt = {qs: 4 * qb + qs for qs in range(4)}
                for kt in kts:
                    pb = C.bank(2 + pi % 4)
                    PT = PTs[pi % 3]
                    pi += 1
                    ks = slice(kt * 128, (kt + 1) * 128)
                    diag = kt >= 4 * qb
                    pvs = []
                    for qs in range(4):
                        if diag and qs < kt - 4 * qb:
                            continue
                        if (not diag) and branch == 1 and qs > kt - (4 * qb - 4):
                            continue
                        pvs.append((qs, first, kt == lastkt[qs]))
                        first = False

                    def qk(pb=pb, PT=PT, KT=KT, hr=hr, ks=ks, h=h, qb=qb, diag=diag, kt=kt, branch=branch):
                        P.mm(pb, KT[hr, ks], QT[hr, h // 2, qb * 512:(qb + 1) * 512], start=True, stop=False)
                        if branch == 0:
                            r0 = (h % 2) * 64
                            P.mm(pb, Rall[r0:r0 + NSEL, ks], SelT[r0:r0 + NSEL, qb * 512:(qb + 1) * 512], start=False, stop=not diag)
                            if diag:
                                P.mm(pb, C.ident, C.Cm[:, kt - 4 * qb, :], start=False, stop=True)
                        else:
                            mk = C.Cm[:, kt - 4 * qb, :] if diag else C.Cw[:, kt - (4 * qb - 4), :]
                            P.mm(pb, C.ident, mk, start=False, stop=True)
                        P.act(PT, pb, AF.Exp)

                    def pv(PT=PT, ob=ob, pvs=pvs, V=V, kt=kt):
                        for (qs, st_, sp_) in pvs:
                            P.mm(ob[:, qs, :], PT[:, qs * 128:(qs + 1) * 128], V[:, kt, 0:65], start=st_, stop=sp_)
                    iters.append([qk, pv, None])

                def fin(ob=ob, qb=qb, h=h, branch=branch):
                    for qs in range(4):
                        t = qb * 4 + qs
                        P.recip(rd2[:, qs:qs + 1], ob[:, qs, 64:65])
                        P.tt(rd2[:, 4 + qs:5 + qs], rd2[:, qs:qs + 1], G[:, t, 3 * h + 1 + branch:3 * h + 2 + branch], ALU.mult)
                        P.stt(MIXA[:, t, h * 64:(h + 1) * 64], ob[:, qs, 0:64], rd2[:, 4 + qs:5 + qs], MIXA[:, t, h * 64:(h + 1) * 64], ALU.mult, ALU.add)
                iters[-1][2] = fin
    run_pipelined(iters)
    mb = A.alloc([NT, 256], BF16)
    P.copy(mb, MIXA, 'dve')
    P.dma('sp', C.MIX[:, 0:256].rearrange("(t p) c -> p t c", p=128), mb)
    A.reset(m1)
    A.reset(m0)


def bcast_row(C, out_sb, row_dram, n, scale=None, q='sp'):
    P, A = C.P, C.A
    m = A.mark()
    tmp = A.alloc([n], F32, parts=1)
    P.dma(q, tmp, row_dram.rearrange("(o n) -> o n", o=1))
    for c0 in range(0, n, 512):
        c1 = min(n, c0 + 512)
        pb = C.bank(2 + (c0 // 512) % 2)[:, 0:c1 - c0]
        P.mm(pb, C.ones_row, tmp[0:1, c0:c1])
        if scale is None:
            P.copy(out_sb[:, c0:c1], pb, 'dve')
        else:
            P.ts(out_sb[:, c0:c1], pb, scale, None, ALU.mult)
    A.reset(m)


def phase_diff(C, L):
    P, A, I = C.P, C.A, C.I
    S, NT, NQB = C.S, C.NT, C.NQB
    m0 = A.mark()
    lam_init = 0.8 - 0.6 * math.exp(-0.3 * L)
    DQT = A.alloc([3, S], BF16)
    DKT = A.alloc([3, S], BF16)
    DV = A.alloc([NT, 4, 66], BF16)
    MIXB = A.alloc([NT, 256], BF16)
    gcol = A.alloc([2], F32)
    gsrc = I['diff_qk_gain'][L]
    for r in range(4):
        P.dma('sp', gcol[r * 32:(r + 1) * 32, :], gsrc.rearrange("j d -> d j"), allow_slow_non_contiguous=True)
    P.ts(gcol[:, 0:1], gcol[:, 0:1], 32 ** -0.5, None, ALU.mult)
    P.memset(DV[:, :, :, 64:66], 1.0)
    lam1 = A.alloc([128], F32, parts=1)
    P.dma('sp', lam1, I['diff_lambda'][L].rearrange("(o a) b -> o (a b)", o=1))
    lp = A.alloc([64], F32, parts=1)
    lsum = A.alloc([2], F32, parts=1)
    P.tt(lp[:, 0:32], lam1[:, 0:32], lam1[:, 32:64], ALU.mult)
    P.tt(lp[:, 32:64], lam1[:, 64:96], lam1[:, 96:128], ALU.mult)
    P.reduce(lsum, lp.rearrange("p (a b) -> p a b", a=2))
    P.act(lsum, lsum, AF.Exp)
    lf = A.alloc([1], F32, parts=1)
    P.tt(lf, lsum[:, 1:2], lsum[:, 0:1], ALU.subtract)
    P.ts(lf, lf, -lam_init, None, ALU.add)
    neglam = A.alloc([1], F32)
    pbl = C.bank(2)[:, 0:1]
    P.mm(pbl, C.ones_row, lf[0:1, 0:1])
    P.copy(neglam, pbl, 'dve')
    subg = A.alloc([64], F32)
    bcast_row(C, subg, I['diff_norm'][L], 64, scale=(1.0 - lam_init))

    m1 = A.mark()
    uts = [A.alloc([768], F32) for _ in range(2)]
    sq = A.alloc([512], F32)
    ssq = A.alloc([16], F32)
    rsq = A.alloc([16], F32)
    nb16 = A.alloc([4, 128], BF16)
    P.dma('sp', uts[0], C.U[0:128, 652:1420])
    for t in range(NT):
        ut = uts[t % 2]
        if t + 1 < NT:
            P.dma('sp', uts[(t + 1) % 2], C.U[(t + 1) * 128:(t + 2) * 128, 652:1420])
        P.act(sq, ut[:, 0:512], AF.Square)
        P.reduce(ssq, sq.rearrange("p (a b) -> p a b", a=16))
        rstd_from_ss(P, rsq, ssq, 32)
        P.tt(nb16.rearrange("p a (g d) -> p (a g) d", g=4), ut[:, 0:512].rearrange("p (g d) -> p g d", g=16),
             rsq.unsqueeze(2).to_broadcast([128, 16, 32]), ALU.mult)
        pT = C.bankbf(t % 2).rearrange("p (a b) -> p a b", a=8)
        nbf = nb16.rearrange("p a b -> p (a b)")
        for qk in range(2):
            for j in range(3):
                w = 96 if j < 2 else 64
                P.tr(pT[0:w, qk * 3 + j, :], nbf[:, qk * 256 + j * 96:qk * 256 + j * 96 + w], C.ident)
        ts_ = slice(t * 128, (t + 1) * 128)
        P.ts(DQT[0:96, :, ts_], pT[0:96, 0:3, :], gcol[0:96, 0:1], None, ALU.mult)
        P.ts(DKT[0:96, :, ts_], pT[0:96, 3:6, :], gcol[0:96, 1:2], None, ALU.mult)
        P.copy(DV[:, t, :, 0:64], ut[:, 512:768].rearrange("p (h d) -> p h d", h=4), 'act')
    A.reset(m1)

    PTs = [A.alloc([512], BF16) for _ in range(3)]
    rd = A.alloc([4], F32)
    otmp = A.alloc([64], F32)
    o2 = A.alloc([64], F32)
    junk = A.alloc([64], F32)
    ss1 = A.alloc([1], F32)
    rs1 = A.alloc([1], F32)
    pi = 0
    oi = 0
    iters = []
    for h in range(4):
        for qb in range(NQB):
            obs = [C.bank(4 + 2 * (oi % 2) + c)[:, 0:260].rearrange("p (a b) -> p a b", a=4) for c in range(2)]
            oi += 1
            first = [True, True]
            for kt in range(4 * qb + 4):
                ks = slice(kt * 128, (kt + 1) * 128)
                diag = kt >= 4 * qb
                for c in range(2):
                    g = h * 2 + c
                    hb = g // 3
                    r0 = (g % 3) * 32
                    rows = slice(r0, r0 + 32)
                    pb = C.bank(pi % 4)
                    PT = PTs[pi % 3]
                    pi += 1
                    pvs = []
                    for qs in range(4):
                        if diag and qs < kt - 4 * qb:
                            continue
                        pvs.append((qs, first[c], kt == 4 * qb + qs))
                        first[c] = False

                    def qk(pb=pb, PT=PT, rows=rows, hb=hb, ks=ks, qb=qb, diag=diag, kt=kt):
                        P.mm(pb, DKT[rows, hb, ks], DQT[rows, hb, qb * 512:(qb + 1) * 512], start=True, stop=not diag)
                        if diag:
                            P.mm(pb, C.ident, C.Cm[:, kt - 4 * qb, :], start=False, stop=True)
                        P.act(PT, pb, AF.Exp)

                    def pv(PT=PT, ob=obs[c], pvs=pvs, kt=kt, h=h):
                        for (qs, st_, sp_) in pvs:
                            P.mm(ob[:, qs, :], PT[:, qs * 128:(qs + 1) * 128], DV[:, kt, h, 0:65], start=st_, stop=sp_)
                    iters.append([qk, pv, None])

            def fin(obs=obs, qb=qb, h=h):
                for qs in range(4):
                    t = qb * 4 + qs
                    P.recip(rd[:, 0:1], obs[0][:, qs, 64:65])
                    P.recip(rd[:, 1:2], obs[1][:, qs, 64:65])
                    P.tt(rd[:, 2:3], rd[:, 1:2], neglam, ALU.mult)
                    P.ts(otmp, obs[1][:, qs, 0:64], rd[:, 2:3], None, ALU.mult)
                    P.stt(o2, obs[0][:, qs, 0:64], rd[:, 0:1], otmp, ALU.mult, ALU.add)
                    P.act(junk, o2, AF.Square, accum=ss1)
                    rstd_from_ss(P, rs1, ss1, 64)
                    P.stt(MIXB[:, t, h * 64:(h + 1) * 64], o2, rs1[:, 0:1], subg, ALU.mult, ALU.mult)
            iters[-1][2] = fin
    run_pipelined(iters)
    P.dma('sp', C.MIX[:, 256:512].rearrange("(t p) c -> p t c", p=128), MIXB)
    A.reset(m0)


def phase_gla(C, L, hgrn):
    P, A, I = C.P, C.A, C.I
    S, NT = C.S, C.NT
    m0 = A.mark()
    if hgrn:
        c0, W, NCH, DK = 2204, 1024, 256, 64
        qo, ko, vo, ogo = 0, 256, 512, 768
        mixcol = 768
        qscale = 1.0
    else:
        c0, W, NCH, DK = 1420, 784, 128, 32
        qo, ko, vo, lro, ogo = 0, 128, 256, 512, 528
        mixcol = 512
        qscale = 32 ** -0.5
    L1 = A.alloc([128], F32)
    L2 = A.alloc([128], F32)
    L3 = A.alloc([128], F32)
    IND = A.alloc([2], F32)
    P.memset(L1, 1.0)
    P.asel(L1, L1, [[1, 128]], 0, -1, 0.0)
    P.memset(L1[0:64, 64:128], 0.0)
    P.memset(L3, 1.0)
    P.asel(L3, L3, [[-1, 128]], -1, 1, 0.0)
    P.memset(L3[64:128, 0:64], 0.0)
    P.memset(L2, 0.0)
    P.memset(L2[0:32, 0:64], 1.0)
    P.memset(L2[64:96, 64:128], 1.0)
    P.tt(L2, L1, L2, ALU.subtract, eng='pool')
    P.memset(IND, 0.0)
    P.memset(IND[0:64, 0:1], 1.0)
    P.memset(IND[64:128, 1:2], 1.0)
    gn = A.alloc([64], F32)
    bcast_row(C, gn, I['hgrn_norm' if hgrn else 'gla_norm'][L], 64)
    if hgrn:
        lb = A.alloc([256], F32)
        oml = A.alloc([256], F32)
        if L == 0:
            P.memset(lb, 0.0)
            P.memset(oml, 1.0)
        else:
            l0 = A.alloc([256], F32)
            bcast_row(C, lb, I['hgrn_lb_logits'][1], 256)
            bcast_row(C, l0, I['hgrn_lb_logits'][0], 256)
            P.tt(lb, lb, l0, ALU.subtract)
            P.act(lb, lb, AF.Sigmoid)
            P.ts(oml, lb, -1.0, 1.0, ALU.mult, ALU.add)
    else:
        w_aug = A.alloc([128], F32, parts=33)
        glrT = A.alloc([128], F32, parts=33)
        P.memset(w_aug, 0.0)
        P.memset(glrT, 0.0)
        P.memset(glrT[32:33, :], 1.0)
        P.dma('sp', w_aug[0:16, :], I['gla_w_gate2'][L])
        P.dma('sp', w_aug[32:33, :], I['gla_b_gate'][L].rearrange("(o n) -> o n", o=1))
    S32 = A.alloc([4, 64], F32)
    P.memset(S32, 0.0)
    ring = [A.alloc([4, 64], BF16) for _ in range(4)]
    ATs = [A.alloc([4, 128], BF16) for _ in range(2)]
    for a_ in ATs:
        P.memset(a_, 0.0)
    MIXC = A.alloc([NT, 256], BF16)
    Af32 = A.alloc([4, 128], F32)
    uts = [A.alloc([W], F32) for _ in range(2)]
    la = A.alloc([NCH], F32)
    kk = A.alloc([NCH], F32)
    e_ = A.alloc([NCH], F32)
    E = A.alloc([4, NCH], F32)
    qt16 = A.alloc([3, NCH], BF16)
    ks16 = A.alloc([NCH], BF16)
    v16 = A.alloc([256], BF16)
    T16 = A.alloc([12, 128], BF16)
    DEC = A.alloc([4, 2], F32)
    o_sb = A.alloc([256], F32)
    sq = A.alloc([256], F32)
    ss4 = A.alloc([4], F32)
    rs4 = A.alloc([4], F32)
    sil = A.alloc([256], F32)
    GS = os.environ.get('GLA_STOP')
    P.dma('sp', uts[0], C.U[0:128, c0:c0 + W])
    for t in range(NT):
        ut = uts[t % 2]
        if t + 1 < NT:
            P.dma('sp', uts[(t + 1) % 2], C.U[(t + 1) * 128:(t + 2) * 128, c0:c0 + W])
        Q = ut[:, qo:qo + NCH]
        if hgrn:
            P.act(e_, ut[:, ko:ko + NCH], AF.Exp, scale=-1.0)
            P.ts(e_, e_, 1.0, None, ALU.add)
            P.recip(e_, e_)
            P.tt(e_, e_, oml, ALU.mult)
            P.tt(e_, e_, lb, ALU.add)
            P.act(la, e_, AF.Ln)
            P.ts(kk, e_, -1.0, 1.0, ALU.mult, ALU.add)
            K = kk
        else:
            pg = C.bank(0)[0:16, 0:128]
            P.tr(pg, ut[:, lro:lro + 16], C.identf)
            P.copy(glrT[0:16, :], pg, 'dve')
            pz = C.bank(1)[:, 0:128]
            P.mm(pz, glrT[0:33, :], w_aug[0:33, :])
            P.act(e_, pz, AF.Exp, scale=-1.0)
            P.act(e_, e_, AF.Ln, bias=1.0)
            P.ts(la, e_, -1.0 / 16.0, None, ALU.mult)
            K = ut[:, ko:ko + NCH]
        if GS == 'la':
            continue
        pB = [C.bank(2 + i)[:, 0:NCH] for i in range(3)]
        P.mm(pB[0], L1, la)
        P.mm(pB[1], L2, la)
        P.mm(pB[2], L3, la)
        pdec = C.bank(0)[:, 256:264].rearrange("p (a b) -> p a b", a=4)
        for h in range(4):
            P.mm(pdec[0:DK, h, :], la[:, h * DK:(h + 1) * DK], IND, start=(h == 0), stop=True)
        P.act(E[:, 0, :], pB[0], AF.Exp)
        P.act(E[:, 1, :], pB[1], AF.Exp)
        P.act(E[:, 2, :], pB[1], AF.Exp, scale=-1.0)
        P.act(E[:, 3, :], pB[2], AF.Exp)
        P.act(DEC[0:DK], pdec[0:DK], AF.Exp)
        if GS == 'cum':
            continue
        P.stt(qt16[:, 0, :], Q, qscale, E[:, 0, :], ALU.mult, ALU.mult)
        P.stt(qt16[:, 1, :], Q, qscale, E[:, 1, :], ALU.mult, ALU.mult)
        P.tt(qt16[:, 2, :], K, E[:, 2, :], ALU.mult)
        P.tt(ks16, K, E[:, 3, :], ALU.mult)
        P.copy(v16, ut[:, vo:vo + 256], 'pool')
        pTa = C.bankbf(5).rearrange("p (a b) -> p a b", a=8)
        pTb = C.bankbf(6).rearrange("p (a b) -> p a b", a=8)
        for wi in range(3):
            for h in range(4):
                idx = wi * 4 + h
                dstp = pTa[0:DK, idx, :] if idx < 8 else pTb[0:DK, idx - 8, :]
                P.tr(dstp, qt16[:, wi, h * DK:(h + 1) * DK], C.ident)
        P.copy(T16[0:DK, 0:8, :], pTa[0:DK, 0:8, :], 'act')
        P.copy(T16[0:DK, 8:12, :], pTb[0:DK, 0:4, :], 'act')
        if GS == 'tr':
            continue
        pA = C.bank(7).rearrange("p (a b) -> p a b", a=4)
        for h in range(4):
            P.mm(pA[:, h, :], T16[0:DK, 8 + h, :], T16[0:DK, 4 + h, :], start=(h == 0), stop=True)
        AT = ATs[t % 2]
        P.copy(Af32, pA, 'act')
        P.asel(AT[0:64, :, 0:64], Af32[0:64, :, 0:64], [[0, 4], [1, 64]], 0, -1, 0.0)
        P.asel(AT[64:128, :, 64:128], Af32[64:128, :, 64:128], [[0, 4], [1, 64]], 0, -1, 0.0)
        if GS == 'A':
            continue
        pU = [C.bank(1)[:, 0:256].rearrange("p (a b) -> p a b", a=4), C.bank(2)[:, 0:256].rearrange("p (a b) -> p a b", a=4)]
        for c in range(2):
            cr = slice(64 * c, 64 * c + 64)
            for h in range(4):
                P.mm(pU[c][0:DK, h, :], ks16[cr, h * DK:(h + 1) * DK], v16[cr, h * 64:(h + 1) * 64], start=(h == 0), stop=True)
        for c in range(2):
            n = 2 * t + c
            P.tt(S32[0:DK], S32[0:DK], DEC[0:DK, :, c:c + 1].to_broadcast([DK, 4, 64]), ALU.mult)
            P.tt(S32[0:DK], S32[0:DK], pU[c][0:DK], ALU.add)
            P.copy(ring[n % 4][0:DK], S32[0:DK], 'pool')
        if GS == 'U':
            continue
        pO = C.bank(3)[:, 0:256].rearrange("p (a b) -> p a b", a=4)
        for h in range(4):
            P.mm(pO[:, h, :], AT[:, h, :], v16[:, h * 64:(h + 1) * 64], start=(h == 0), stop=False)
        for h in range(4):
            for c in range(2):
                n_prev = 2 * t + c - 1
                if n_prev < 0:
                    continue
                P.mm(pO[64 * c:64 * c + 64, h, :], T16[0:DK, h, 64 * c:64 * c + 64], ring[n_prev % 4][0:DK, h, :], start=False, stop=True)
        if GS == 'O':
            continue
        P.copy(o_sb, pO.rearrange("p a b -> p (a b)"), 'act')
        P.tt(sq, o_sb, o_sb, ALU.mult)
        P.reduce(ss4, sq.rearrange("p (a b) -> p a b", a=4))
        rstd_from_ss(P, rs4, ss4, 64)
        P.act(sil, ut[:, ogo:ogo + 256], AF.Silu)
        P.tt(sil.rearrange("p (a b) -> p a b", a=4), sil.rearrange("p (a b) -> p a b", a=4), gn.unsqueeze(1).to_broadcast([128, 4, 64]), ALU.mult)
        P.tt(o_sb.rearrange("p (a b) -> p a b", a=4), o_sb.rearrange("p (a b) -> p a b", a=4), rs4.unsqueeze(2).to_broadcast([128, 4, 64]), ALU.mult)
        P.tt(MIXC[:, t, :], o_sb, sil, ALU.mult)
    P.dma('sp', C.MIX[:, mixcol:mixcol + 256].rearrange("(t p) c -> p t c", p=128), MIXC)
    A.reset(m0)


def phase_out(C, L, src, dst):
    P, A, I = C.P, C.A, C.I
    S, NT = C.S, C.NT
    moe = (L % 2 == 1)
    NBLK = min(S, 1024)
    NB5 = NBLK // 512
    TPB = NBLK // 128
    m0 = A.mark()
    wout = A.alloc([8, D], BF16)
    for kc in range(8):
        P.dma('pool', wout[:, kc, :], I['w_out'][L, kc * 128:(kc + 1) * 128, :])
    gbc = A.alloc([D], F32)
    bcast_row(C, gbc, I['norm_ffn'][L], D)
    if moe:
        rt32 = A.alloc([8, 8], F32)
        P.dma('sp', rt32, I['moe_router'][0].rearrange("(kc p) e -> p kc e", p=128))
        lg = A.alloc([8], F32)
        v8 = A.alloc([8], F32)
        ex = A.alloc([8], F32)
        sm = A.alloc([4], F32)
        msk = A.alloc([8], F32)
        comb = A.alloc([8], F32)
        combT = A.alloc([128], F32, parts=8)
    hts = [A.alloc([D], F32) for _ in range(2)]
    mts = [A.alloc([D], BF16) for _ in range(2)]
    mT = A.alloc([8, 128], BF16)
    h1 = A.alloc([D], F32)
    junk = A.alloc([D], BF16)
    c32 = A.alloc([D], F32)
    cT32 = A.alloc([8, 128], F32)
    cT16 = A.alloc([8, 128], BF16)
    ss = A.alloc([1], F32)
    rs = A.alloc([1], F32)
    P.dma('sp', hts[0], src[0:128, :])
    P.dma('sp', mts[0], C.MIX[0:128, :])
    for t in range(NT):
        ht, mt = hts[t % 2], mts[t % 2]
        if t + 1 < NT:
            P.dma('sp', hts[(t + 1) % 2], src[(t + 1) * 128:(t + 2) * 128, :])
            P.dma('sp', mts[(t + 1) % 2], C.MIX[(t + 1) * 128:(t + 2) * 128, :])
        pT = C.bankbf(0).rearrange("p (a b) -> p a b", a=8)
        for c in range(8):
            P.tr(pT[:, c, :], mt[:, c * 128:(c + 1) * 128], C.ident)
        P.copy(mT, pT, 'act')
        for hf in range(2):
            pb = C.bank(1 + hf)
            for kc in range(8):
                P.mm(pb, mT[:, kc, :], wout[:, kc, hf * 512:(hf + 1) * 512], start=(kc == 0), stop=(kc == 7))
            P.tt(h1[:, hf * 512:(hf + 1) * 512], pb, ht[:, hf * 512:(hf + 1) * 512], ALU.add)
        P.dma('sp', dst[t * 128:(t + 1) * 128, :], h1)
        P.act(junk, h1, AF.Square, accum=ss)
        rstd_from_ss(P, rs, ss, D)
        P.stt(c32, h1, rs[:, 0:1], gbc, ALU.mult, ALU.mult)
        for hf in range(2):
            pc = C.bank(3 + hf).rearrange("p (a b) -> p a b", a=4)
            for c in range(4):
                P.tr(pc[:, c, :], c32[:, (hf * 4 + c) * 128:(hf * 4 + c + 1) * 128], C.identf)
            P.copy(cT16[:, hf * 4:(hf + 1) * 4, :], pc, 'act')
            if moe:
                P.copy(cT32[:, hf * 4:(hf + 1) * 4, :], pc, 'dve')
        P.dma('sp', C.CTD[:, :, t * 128:(t + 1) * 128], cT16)
        if moe:
            pl = C.bank(5)[:, 0:8]
            for kc in range(8):
                P.mm(pl, cT32[:, kc, :], rt32[:, kc, :], start=(kc == 0), stop=(kc == 7))
            P.copy(lg, pl, 'dve')
            P.op('dve', lambda e: e.max(out=v8, in_=lg), [lg], [v8])
            P.ts(sm[:, 0:1], v8[:, 0:1], -1.0, None, ALU.mult)
            P.act(ex, lg, AF.Exp, bias=sm[:, 0:1])
            P.act(sm[:, 1:2], v8[:, 1:2], AF.Exp, bias=sm[:, 0:1])
            P.ts(sm[:, 2:3], sm[:, 1:2], 1.0, None, ALU.add)
            P.recip(sm[:, 2:3], sm[:, 2:3])
            P.ts(msk, lg, v8[:, 1:2], None, ALU.is_ge)
            P.stt(comb, ex, sm[:, 2:3], msk, ALU.mult, ALU.mult)
            pcm = C.bank(6)[0:8, 0:128]
            P.tr(pcm, comb, C.identf)
            P.copy(combT, pcm, 'dve')
            P.dma('sp', C.COMB[:, t * 128:(t + 1) * 128], combT)
    A.reset(m0)

    wpg = A.alloc([8, D], BF16)
    for kc in range(8):
        P.dma('pool', wpg[:, kc, :], I['ple_w_gate'][L, kc * 128:(kc + 1) * 128, :])
    wpp = A.alloc([2, D], BF16)
    for j in range(2):
        P.dma('pool', wpp[:, j, :], I['ple_w_proj'][L, j * 128:(j + 1) * 128, :])
    gpT = A.alloc([8], F32)
    P.dma('sp', gpT, I['ple_norm'][L].rearrange("(c p) -> p c", p=128), allow_slow_non_contiguous=True)
    CT = A.alloc([8, NBLK], BF16)
    hidden = A.alloc([28, NBLK], BF16)
    accT = A.alloc([8, NBLK], F32)
    wgs = [A.alloc([8, 256], BF16) for _ in range(2)]
    wus = [A.alloc([8, 256], BF16) for _ in range(2)]
    wds = [A.alloc([14, 128], BF16) for _ in range(2)]
    sg = [A.alloc([512], BF16) for _ in range(2)]
    tmpo = A.alloc([512], F32)
    if moe:
        sel_all = A.alloc([8, 128], F32, parts=8)
        P.memset(sel_all, 1.0)
        P.asel(sel_all, sel_all, [[1, 8], [0, 128]], 0, -1, 0.0, cmp=ALU.is_equal)
        cmbT = A.alloc([NBLK], F32, parts=8)
        cbc = A.alloc([NBLK], F32)
        WG = lambda e: I['moe_w_gate'][0, e]
        WU = lambda e: I['moe_w_up'][0, e]
        WD = lambda e: I['moe_w_down'][0, e]
        NE = 8
    else:
        WG = lambda e: I['ffn_w_gate'][0]
        WU = lambda e: I['ffn_w_up'][0]
        WD = lambda e: I['ffn_w_down'][0]
        NE = 1
    h1t = A.alloc([D], F32)
    h2 = h1t
    hn = A.alloc([D], BF16)
    aT = A.alloc([8, 128], BF16)
    gate = A.alloc([D], F32)
    pt32 = A.alloc([256], F32)
    pt16 = A.alloc([256], BF16)
    ppT = A.alloc([2, 128], BF16)
    junk = gate[:, 0:512].bitcast(BF16)
    ss = A.alloc([1], F32)
    rs = A.alloc([1], F32)

    def load_gu(e, fg, buf):
        wgv = WG(e).rearrange("(kc p) f -> p kc f", p=128)
        wuv = WU(e).rearrange("(kc p) f -> p kc f", p=128)
        P.dma('pool', wgs[buf], wgv[:, :, fg * 256:(fg + 1) * 256])
        P.dma('pool', wus[buf], wuv[:, :, fg * 256:(fg + 1) * 256])

    def load_d(e, dh, buf):
        dc, hf_ = dh // 2, dh % 2
        wdv = WD(e).rearrange("(f p) d -> p f d", p=128)
        for f0 in range(0, 14, 7):
            P.dma('pool', wds[buf][:, f0:f0 + 7, :], wdv[:, hf_ * 14 + f0:hf_ * 14 + f0 + 7, dc * 128:(dc + 1) * 128])

    gi = 0
    di = 0
    for blk in range(S // NBLK):
        T0 = blk * NBLK
        P.dma('sp', CT, C.CTD[:, :, T0:T0 + NBLK])
        if moe:
            P.dma('sp', cmbT, C.COMB[:, T0:T0 + NBLK])
        for e in range(NE):
            if moe:
                for nb in range(NB5):
                    pbc = C.bank(6 + nb % 2)
                    P.mm(pbc, sel_all[0:8, e, :], cmbT[0:8, nb * 512:(nb + 1) * 512])
                    P.copy(cbc[:, nb * 512:(nb + 1) * 512], pbc, 'act')
            load_gu(e, 0, gi % 2)
            for fg in range(14):
                if fg + 1 < 14:
                    load_gu(e, fg + 1, (gi + 1) % 2)
                wg, wu = wgs[gi % 2], wus[gi % 2]
                gi += 1
                for fc in range(2):
                    f = fg * 2 + fc
                    for nb in range(NB5):
                        pg = C.bank(0 + (f * NB5 + nb) % 2)
                        pu = C.bank(2 + (f * NB5 + nb) % 2)
                        for kc in range(8):
                            P.mm(pg, wg[:, kc, fc * 128:(fc + 1) * 128], CT[:, kc, nb * 512:(nb + 1) * 512], start=(kc == 0), stop=(kc == 7))
                        for kc in range(8):
                            P.mm(pu, wu[:, kc, fc * 128:(fc + 1) * 128], CT[:, kc, nb * 512:(nb + 1) * 512], start=(kc == 0), stop=(kc == 7))
                        sgb = sg[(f * NB5 + nb) % 2]
                        P.act(sgb, pg, AF.Silu)
                        P.tt(hidden[:, f, nb * 512:(nb + 1) * 512], sgb, pu, ALU.mult)
            load_d(e, 0, di % 2)
            for dc in range(8):
                pos_ = [C.bank(4 + nb) for nb in range(NB5)]
                for hf_ in range(2):
                    dh = dc * 2 + hf_
                    if dh + 1 < 16:
                        load_d(e, dh + 1, (di + 1) % 2)
                    wd = wds[di % 2]
                    di += 1
                    for nb in range(NB5):
                        for f in range(14):
                            ff = hf_ * 14 + f
                            P.mm(pos_[nb], wd[:, f, :], hidden[:, ff, nb * 512:(nb + 1) * 512], start=(ff == 0), stop=(ff == 27))
                for nb in range(NB5):
                    po = pos_[nb]
                    dstp = accT[:, dc, nb * 512:(nb + 1) * 512]
                    if not moe:
                        P.copy(dstp, po, 'act')
                    elif e == 0:
                        P.tt(dstp, po, cbc[:, nb * 512:(nb + 1) * 512], ALU.mult)
                    else:
                        P.tt(tmpo, po, cbc[:, nb * 512:(nb + 1) * 512], ALU.mult)
                        P.tt(dstp, dstp, tmpo, ALU.add)
        for tl in range(TPB):
            t = blk * TPB + tl
            rowsl = slice(t * 128, (t + 1) * 128)
            P.dma('sp', h1t, dst[rowsl, :])
            P.dma('sp', pt32, I['p'][L, rowsl, :])
            for hf in range(2):
                pc = C.bank(hf).rearrange("p (a b) -> p a b", a=4)
                for c in range(4):
                    P.tr(pc[:, c, :], accT[:, hf * 4 + c, tl * 128:(tl + 1) * 128], C.identf)
                P.tt(h2[:, hf * 512:(hf + 1) * 512], pc.rearrange("p a b -> p (a b)"), h1t[:, hf * 512:(hf + 1) * 512], ALU.add)
            P.act(junk, h2, AF.Square, accum=ss)
            rstd_from_ss(P, rs, ss, D)
            P.ts(hn, h2, rs[:, 0:1], None, ALU.mult)
            pT = C.bankbf(2).rearrange("p (a b) -> p a b", a=8)
            for c in range(8):
                P.tr(pT[:, c, :], hn[:, c * 128:(c + 1) * 128], C.ident)
            P.tt(aT, pT, gpT.unsqueeze(2).to_broadcast([128, 8, 128]), ALU.mult)
            P.copy(pt16, pt32, 'act')
            pP = C.bankbf(3).rearrange("p (a b) -> p a b", a=8)
            for j in range(2):
                P.tr(pP[:, j, :], pt16[:, j * 128:(j + 1) * 128], C.ident)
            P.copy(ppT, pP[:, 0:2, :], 'act')
            for hf in range(2):
                pgt = C.bank(4 + hf)
                for kc in range(8):
                    P.mm(pgt, aT[:, kc, :], wpg[:, kc, hf * 512:(hf + 1) * 512], start=(kc == 0), stop=(kc == 7))
                P.act(gate[:, hf * 512:(hf + 1) * 512], pgt, AF.Sigmoid)
                ppp = C.bank(6 + hf)
                for j in range(2):
                    P.mm(ppp, ppT[:, j, :], wpp[:, j, hf * 512:(hf + 1) * 512], start=(j == 0), stop=(j == 1))
                P.tt(gate[:, hf * 512:(hf + 1) * 512], ppp, gate[:, hf * 512:(hf + 1) * 512], ALU.mult)
            P.tt(h2, h2, gate, ALU.add)
            P.dma('sp', dst[rowsl, :], h2)
    A.reset(m0)


_CACHE = {}


def kernel(**inputs):
    S = 4096
    if 'nc' not in _CACHE:
        _CACHE['nc'] = build(S)
    nc = _CACHE['nc']
    names = [k for k in inputs if k not in ('x', 'p')]
    in_maps = []
    for b in range(8):
        mp = {'x': np.ascontiguousarray(inputs['x'][b], dtype=np.float32),
              'p': np.ascontiguousarray(inputs['p'][:, b], dtype=np.float32)}
        for k in names:
            mp[k] = np.ascontiguousarray(inputs[k], dtype=np.float32)
        in_maps.append(mp)
    res = run_bass_kernel_spmd(nc, in_maps, core_ids=list(range(8)))
    out = np.stack([np.asarray(r["y"], dtype=np.float32) for r in res.results], axis=0)
    return out
```

```python
import math
import os
import contextlib
import numpy as np
import concourse.bass as bass
import concourse.mybir as mybir
from concourse.bass_utils import run_bass_kernel_spmd
from concourse.alu_op_type import AluOpType as ALU

F32 = mybir.dt.float32
BF16 = mybir.dt.bfloat16
I32 = mybir.dt.int32
AF = mybir.ActivationFunctionType
AX = mybir.AxisListType

NSLOT = 8
D = 1024
INC = 3228
DFF = 3584
NEGM = -30000.0
EPS = 1e-6


def box_of(ap):
    t = ap.tensor
    rowlen = 1
    for s in list(t.shape)[1:]:
        rowlen *= int(s)
    if str(ap.space) != 'DRAM':
        rowlen = int(ap.ap[0][0]) if len(ap.ap) > 0 and int(ap.ap[0][0]) > 0 else rowlen
    off = int(ap.offset)
    p0 = off // rowlen
    f0 = off % rowlen
    pe = 0
    fe = 0
    for (st, cnt) in ap.ap:
        st = int(st); cnt = int(cnt)
        if cnt <= 1 or st == 0:
            continue
        if st % rowlen == 0:
            pe += (st // rowlen) * (cnt - 1)
        else:
            fe += abs(st) * (cnt - 1)
    sz = mybir.dt.size(ap.dtype)
    if str(ap.space) == 'PSUM':
        b0 = (f0 * sz) // 2048 * 2048
        b1 = ((f0 + fe + 1) * sz + 2047) // 2048 * 2048
        q0 = p0 // 32 * 32
        q1 = (p0 + pe + 1 + 31) // 32 * 32
        return (t.name, q0, q1, b0, b1)
    return (t.name, p0, p0 + pe + 1, f0 * sz, (f0 + fe + 1) * sz)


def overlap(a, b):
    return a[1] < b[2] and b[1] < a[2] and a[3] < b[4] and b[3] < a[4]


def covers(a, b):
    return a[1] <= b[1] and a[2] >= b[2] and a[3] <= b[3] and a[4] >= b[4]


def isap(v):
    return v is not None and not isinstance(v, (int, float))


class Prog:
    def __init__(self, nc):
        self.nc = nc
        self.ops = []
        self.track = {}
        self.dma_n = {}

    def op(self, eng, fn, reads=(), writes=(), dma=False):
        idx = len(self.ops)
        rb = [box_of(a) for a in reads]
        wb = [box_of(a) for a in writes]
        wb = wb + [b for b in rb if b[0] == 'psum']
        rb = [b for b in rb if b[0] != 'psum']
        deps = set()
        for b in rb:
            for e in self.track.setdefault(b[0], []):
                if e[3] and overlap(e[0], b):
                    deps.add(e[1])
        for b in wb:
            for e in self.track.setdefault(b[0], []):
                if overlap(e[0], b):
                    deps.add(e[1])
        for b in wb:
            lst = self.track[b[0]]
            lst[:] = [e for e in lst if not covers(b, e[0])]
            lst.append((b, idx, eng, True, dma))
        for b in rb:
            lst = self.track[b[0]]
            lst[:] = [e for e in lst if not (e[2] == eng and (not e[3]) and (not e[4]) and (not dma) and covers(b, e[0]))]
            lst.append((b, idx, eng, False, dma))
        deps.discard(idx)
        o = dict(eng=eng, fn=fn, deps=deps, dma=dma, signal=dma)
        if dma:
            n = self.dma_n.get(eng, 0)
            self.dma_n[eng] = n + 1
            o['slot'] = n % NSLOT
            o['val'] = 16 * (n // NSLOT + 1)
            o['dma_idx'] = n
        self.ops.append(o)
        for d in deps:
            p = self.ops[d]
            if p['eng'] == eng and not p['dma'] and not dma and eng == 'pe':
                continue
            p['signal'] = True
        return idx

    def emit(self):
        nc = self.nc
        engs = ['pe', 'act', 'dve', 'pool', 'sp']
        with contextlib.ExitStack() as st:
            csem = {e: st.enter_context(nc.semaphore('c_' + e)) for e in engs}
            dsem = {}
            for e in self.dma_n:
                dsem[e] = [st.enter_context(nc.semaphore('d_%s_%d' % (e, i))) for i in range(NSLOT)]
            cnt = {e: 0 for e in engs}
            for o in self.ops:
                if o['dma']:
                    o['sem'] = dsem[o['eng']][o['slot']]
                elif o['signal']:
                    cnt[o['eng']] += 1
                    o['sem'] = csem[o['eng']]
                    o['val'] = cnt[o['eng']]
            per = {e: [] for e in engs}
            for i, o in enumerate(self.ops):
                per[o['eng']].append(i)
            self.max_cnt = dict(cnt)
            block = st.enter_context(nc.Block())
            ops = self.ops

            def run(ename, engine):
                seen = {}
                for i in per[ename]:
                    o = ops[i]
                    waits = {}
                    for d in o['deps']:
                        p = ops[d]
                        if p['eng'] == ename and not p['dma'] and not o['dma'] and ename == 'pe':
                            continue
                        key = id(p['sem'])
                        if key not in waits or waits[key][1] < p['val']:
                            waits[key] = (p['sem'], p['val'])
                    if o['dma'] and o['dma_idx'] >= NSLOT:
                        s = o['sem']
                        v = o['val'] - 16
                        key = id(s)
                        if key not in waits or waits[key][1] < v:
                            waits[key] = (s, v)
                    for key, (s, v) in waits.items():
                        if seen.get(key, 0) >= v:
                            continue
                        engine.wait_ge(s, v)
                        seen[key] = v
                    ins = o['fn'](engine)
                    if o['signal']:
                        ins.then_inc(o['sem'], 16 if o['dma'] else 1)
                if ename in dsem:
                    n = self.dma_n[ename]
                    for sl in range(NSLOT):
                        k = (n - sl + NSLOT - 1) // NSLOT
                        if k > 0:
                            engine.wait_ge(dsem[ename][sl], 16 * k)

            block.tensor(lambda e: run('pe', e))
            block.scalar(lambda e: run('act', e))
            block.vector(lambda e: run('dve', e))
            block.gpsimd(lambda e: run('pool', e))
            block.sync(lambda e: run('sp', e))

    def dma(self, q, out, in_, **kw):
        return self.op(q, lambda e: e.dma_start(out=out, in_=in_, **kw), [in_], [out], dma=True)

    def mm(self, out, lhsT, rhs, start=True, stop=True):
        return self.op('pe', lambda e: e.matmul(out=out, lhsT=lhsT, rhs=rhs, start=start, stop=stop, skip_group_check=True), [lhsT, rhs], [out])

    def tr(self, out, in_, ident):
        return self.op('pe', lambda e: e.transpose(out=out, in_=in_, identity=ident), [in_, ident], [out])

    def act(self, out, in_, func, bias=None, scale=None, accum=None):
        kw = {}
        rd = [in_]
        wr = [out]
        if bias is not None:
            kw['bias'] = bias
            if isap(bias):
                rd.append(bias)
        if scale is not None:
            kw['scale'] = scale
            if isap(scale):
                rd.append(scale)
        if accum is not None:
            kw['accum_out'] = accum
            wr.append(accum)
        return self.op('act', lambda e: e.activation(out=out, in_=in_, func=func, **kw), rd, wr)

    def ts(self, out, in0, s1, s2=None, op0=ALU.mult, op1=None, eng='dve', accum=None):
        rd = [in0] + [s for s in (s1, s2) if isap(s)]
        wr = [out] + ([accum] if accum is not None else [])
        kw = {}
        if op1 is not None:
            kw['op1'] = op1
        if accum is not None:
            kw['accum_out'] = accum
        return self.op(eng, lambda e: e.tensor_scalar(out=out, in0=in0, scalar1=s1, scalar2=s2, op0=op0, **kw), rd, wr)

    def tt(self, out, in0, in1, op, eng='dve'):
        return self.op(eng, lambda e: e.tensor_tensor(out=out, in0=in0, in1=in1, op=op), [in0, in1], [out])

    def stt(self, out, in0, scalar, in1, op0, op1):
        rd = [in0, in1] + ([scalar] if isap(scalar) else [])
        return self.op('dve', lambda e: e.scalar_tensor_tensor(out=out, in0=in0, scalar=scalar, in1=in1, op0=op0, op1=op1), rd, [out])

    def copy(self, out, in_, eng='dve'):
        if eng == 'act':
            return self.op('act', lambda e: e.copy(out=out, in_=in_), [in_], [out])
        return self.op(eng, lambda e: e.tensor_copy(out=out, in_=in_), [in_], [out])

    def memset(self, ap, val, eng='pool'):
        return self.op(eng, lambda e: e.memset(ap, val), [], [ap])

    def reduce(self, out, in_, op=ALU.add, axis=AX.X):
        return self.op('dve', lambda e: e.tensor_reduce(out=out, in_=in_, axis=axis, op=op), [in_], [out])

    def recip(self, out, in_):
        return self.op('dve', lambda e: e.reciprocal(out=out, in_=in_), [in_], [out])

    def asel(self, out, in_, pattern, base, cm, fill, cmp=ALU.is_ge):
        return self.op('pool', lambda e: e.affine_select(out=out, in_=in_, pattern=pattern, compare_op=cmp, fill=fill, base=base, channel_multiplier=cm), [in_], [out])


class Arena:
    def __init__(self, t, nwords):
        self.t = t
        self.n = nwords
        self.off = 0
        self.peak = 0

    def mark(self):
        return self.off

    def reset(self, m):
        self.off = m

    def alloc(self, free_shape, dtype, parts=128):
        n = 1
        for s in free_shape:
            n *= s
        sz = mybir.dt.size(dtype)
        words = (n * sz + 3) // 4
        words = (words + 7) // 8 * 8
        assert self.off + words <= self.n, "arena overflow %d+%d>%d" % (self.off, words, self.n)
        ap = self.t[0:parts, self.off:self.off + words]
        self.off += words
        self.peak = max(self.peak, self.off)
        if dtype != F32:
            ap = ap.bitcast(dtype)
        ap = ap[:, 0:n]
        if len(free_shape) == 2:
            ap = ap.rearrange("p (a b) -> p a b", a=free_shape[0])
        elif len(free_shape) == 3:
            ap = ap.rearrange("p (a b c) -> p a b c", a=free_shape[0], b=free_shape[1])
        return ap


def rstd_from_ss(P, out, ss, n):
    P.act(out, ss, AF.Ln, bias=EPS, scale=1.0 / n)
    P.act(out, out, AF.Exp, scale=-0.5)


class Ctx:
    pass


def build(S=4096, dbg=None, phases=None):
    NT = S // 128
    NQB = S // 512
    NSEL = S // 64
    NCMP = S // 16 - 1
    NBT = (NCMP + 127) // 128
    nc = bass.Bass("TRN2", target_bir_lowering=False)

    def din(name, shape):
        return nc.dram_tensor(name, shape, F32, kind="ExternalInput").ap()

    I = {}
    I['x'] = din("x", [S, D])
    I['p'] = din("p", [2, S, 256])
    for name, shape in [("norm_attn", [2, D]), ("w_in", [2, D, INC]), ("w_out", [2, D, D]),
                        ("nsa_cmp_pos", [2, 2, 32, 64]), ("nsa_cmp_w1", [2, 2, 2048, 256]), ("nsa_cmp_w2", [2, 2, 256, 64]),
                        ("nsa_qk_gain", [2, 4, 64]), ("diff_qk_gain", [2, 2, 32]), ("diff_lambda", [2, 4, 32]),
                        ("diff_norm", [2, 64]), ("gla_w_gate2", [2, 16, 128]), ("gla_b_gate", [2, 128]), ("gla_norm", [2, 64]),
                        ("hgrn_lb_logits", [2, 256]), ("hgrn_norm", [2, 64]), ("norm_ffn", [2, D]),
                        ("ffn_w_gate", [1, D, DFF]), ("ffn_w_up", [1, D, DFF]), ("ffn_w_down", [1, DFF, D]),
                        ("moe_router", [1, D, 8]), ("moe_w_gate", [1, 8, D, DFF]), ("moe_w_up", [1, 8, D, DFF]),
                        ("moe_w_down", [1, 8, DFF, D]), ("ple_norm", [2, D]), ("ple_w_gate", [2, D, D]), ("ple_w_proj", [2, 256, D])]:
        I[name] = din(name, shape)
    Y = nc.dram_tensor("y", [S, D], F32, kind="ExternalOutput").ap()

    def scratch(name, shape, dt):
        kind = "ExternalOutput" if (dbg and name in dbg) else "Internal"
        return nc.dram_tensor(name, shape, dt, kind=kind).ap()

    U = scratch("U", [S, INC], F32)
    MIX = scratch("MIX", [S, D], BF16)
    H = scratch("H", [S, D], F32)
    CTD = scratch("CTD", [128, 8, S], BF16)
    COMB = scratch("COMB", [8, S], F32)

    with contextlib.ExitStack() as st:
        ARW = 49 * 1024
        arena_t = st.enter_context(nc.sbuf_tensor("arena", [128, ARW], F32))
        psum = st.enter_context(nc.psum_tensor("psum", [128, 8, 512], F32))
        P = Prog(nc)
        A = Arena(arena_t, ARW)
        C = Ctx()
        C.nc, C.P, C.A, C.psum, C.I = nc, P, A, psum, I
        C.S, C.NT, C.NQB, C.NSEL, C.NCMP, C.NBT = S, NT, NQB, NSEL, NCMP, NBT
        C.U, C.MIX, C.H, C.Y = U, MIX, H, Y
        C.CTD, C.COMB = CTD, COMB
        C.bank = lambda b: psum[:, b, :]
        C.bankbf = lambda b: psum[:, b, :].bitcast(BF16)

        C.identf = A.alloc([128], F32)
        C.ident = A.alloc([128], BF16)
        P.memset(C.identf, 1.0)
        P.asel(C.identf, C.identf, [[1, 128]], 0, -1, 0.0, cmp=ALU.is_equal)
        P.copy(C.ident, C.identf, 'dve')
        C.ones_row = A.alloc([128], F32, parts=1)
        P.memset(C.ones_row, 1.0)
        C.Cm = A.alloc([4, 512], BF16)
        C.Cw = A.alloc([4, 512], BF16)
        tmpf = A.alloc([512], F32)
        for r in range(4):
            P.memset(tmpf, 0.0)
            P.asel(tmpf, tmpf, [[1, 512]], -128 * r, -1, NEGM)
            P.copy(C.Cm[:, r, :], tmpf, 'dve')
            P.memset(tmpf, 0.0)
            P.asel(tmpf, tmpf, [[-1, 512]], 128 * r - 1, 1, NEGM)
            P.copy(C.Cw[:, r, :], tmpf, 'dve')

        ph = phases or ['inproj', 'nsa', 'diff', 'gla', 'hgrn', 'out']
        for L in range(2):
            src = I['x'] if L == 0 else H
            dst = H if L == 0 else Y
            if ('inproj', L) in ph or 'inproj' in ph:
                phase_inproj(C, L, src)
            if 'nsa' in ph:
                phase_nsa(C, L)
            if 'diff' in ph:
                phase_diff(C, L)
            if 'gla' in ph:
                phase_gla(C, L, hgrn=False)
            if 'hgrn' in ph:
                phase_gla(C, L, hgrn=True)
            if 'out' in ph:
                phase_out(C, L, src, dst)
            if dbg and dbg.get('stop_after_layer') == L:
                break
        P.emit()
        C.peak = A.peak
    nc._ctx = C
    return nc


def load_cast(P, out, in_, q='pool'):
    n = out.shape[-1]
    if n <= 2048:
        P.dma(q, out, in_)
    else:
        k = (n + 2047) // 2048
        step = (n + k - 1) // k
        for c0 in range(0, n, step):
            c1 = min(n, c0 + step)
            P.dma(q, out[..., c0:c1], in_[..., c0:c1])


def norm_transpose(C, xt, gT, hn, aT, tb, ss, rs, junk):
    P = C.P
    P.act(junk, xt, AF.Square, accum=ss)
    rstd_from_ss(P, rs, ss, D)
    P.ts(hn, xt, rs[:, 0:1], None, ALU.mult)
    pT = C.bankbf(tb).rearrange("p (a b) -> p a b", a=8)
    for c in range(8):
        P.tr(pT[:, c, :], hn[:, c * 128:(c + 1) * 128], C.ident)
    P.tt(aT, pT, gT.unsqueeze(2).to_broadcast([128, 8, 128]), ALU.mult)


def phase_inproj(C, L, src):
    P, A, I = C.P, C.A, C.I
    m = A.mark()
    win = A.alloc([8, INC], BF16)
    for kc in range(8):
        load_cast(P, win[:, kc, :], I['w_in'][L, kc * 128:(kc + 1) * 128, :])
    gT = A.alloc([8], F32)
    P.dma('sp', gT, I['norm_attn'][L].rearrange("(c p) -> p c", p=128), allow_slow_non_contiguous=True)
    xts = [A.alloc([D], F32) for _ in range(2)]
    junk = A.alloc([D], BF16)
    hn = A.alloc([D], BF16)
    aTs = [A.alloc([8, 128], BF16) for _ in range(2)]
    us = [A.alloc([INC], F32) for _ in range(2)]
    ss = A.alloc([1], F32)
    rs = A.alloc([1], F32)
    chunks = [(c0, min(INC, c0 + 512)) for c0 in range(0, INC, 512)]
    P.dma('sp', xts[0], src[0:128, :])
    bk = 0
    for t in range(C.NT):
        xt = xts[t % 2]
        if t + 1 < C.NT:
            P.dma('sp', xts[(t + 1) % 2], src[(t + 1) * 128:(t + 2) * 128, :])
        aT = aTs[t % 2]
        norm_transpose(C, xt, gT, hn, aT, t % 2, ss, rs, junk)
        u = us[t % 2]
        for ci, (c0, c1) in enumerate(chunks):
            b = 2 + (bk % 6)
            bk += 1
            pb = C.bank(b)[:, 0:c1 - c0]
            for kc in range(8):
                P.mm(pb, aT[:, kc, :], win[:, kc, c0:c1], start=(kc == 0), stop=(kc == 7))
            if ci % 2 == 0:
                P.copy(u[:, c0:c1], pb, 'act')
            else:
                P.copy(u[:, c0:c1], pb, 'dve')
        P.dma('sp', C.U[t * 128:(t + 1) * 128, :], u)
    A.reset(m)


def run_pipelined(iters, la=2):
    n = len(iters)
    for i in range(n + la):
        if i < n:
            iters[i][0]()
        j = i - la
        if j >= 0:
            iters[j][1]()
            if iters[j][2] is not None:
                iters[j][2]()


def bcast_load(P, out, vec, q='sp'):
    P.dma(q, out, vec.partition_broadcast(128))


def phase_nsa(C, L):
    P, A, I = C.P, C.A, C.I
    S, NT, NQB, NSEL, NCMP, NBT = C.S, C.NT, C.NQB, C.NSEL, C.NCMP, C.NBT
    m0 = A.mark()
    QT = A.alloc([2, S], BF16)
    KsT = A.alloc([S], BF16)
    KwT = A.alloc([S], BF16)
    KVcT = A.alloc([S], BF16)
    Vs = A.alloc([NT, 66], BF16)
    Vw = A.alloc([NT, 66], BF16)
    G = A.alloc([NT, 12], F32)
    MIXA = A.alloc([NT, 256], F32)
    SelT = A.alloc([S], BF16)
    Rall = A.alloc([S], BF16)
    gcol = A.alloc([4], F32)
    gsrc = I['nsa_qk_gain'][L]
    for hh in range(2):
        P.dma('sp', gcol[hh * 64:(hh + 1) * 64, :], gsrc.rearrange("j d -> d j"), allow_slow_non_contiguous=True)
    P.ts(gcol[:, 0:1], gcol[:, 0:1], 0.125, None, ALU.mult)
    P.memset(Vs[:, :, 64:65], 1.0)
    P.memset(Vw[:, :, 64:65], 1.0)
    m1 = A.mark()
    rf = A.alloc([S], F32, parts=64)
    P.memset(rf, 1.0)
    P.asel(rf, rf, [[1, S]], 0, -64, 0.0)
    P.asel(rf, rf, [[-1, S]], 63, 64, 0.0)
    P.copy(Rall[0:64], rf, 'dve')
    P.dma('sp', Rall[64:128, :], Rall[0:64, :])
    A.reset(m1)

    if os.environ.get('NSA_STOP') == '0':
        A.reset(m0)
        return
    m1 = A.mark()
    uts = [A.alloc([652], F32) for _ in range(2)]
    sq = A.alloc([640], F32)
    ssq = A.alloc([10], F32)
    rsq = A.alloc([10], F32)
    nb16 = A.alloc([5, 128], BF16)
    P.dma('sp', uts[0], C.U[0:128, 0:652])
    for t in range(NT):
        ut = uts[t % 2]
        if t + 1 < NT:
            P.dma('sp', uts[(t + 1) % 2], C.U[(t + 1) * 128:(t + 2) * 128, 0:652])
        SK = os.environ.get('NSA_SKIP', '').split(',')
        P.act(sq, ut[:, 0:640], AF.Square)
        P.reduce(ssq, sq.rearrange("p (a b) -> p a b", a=10))
        rstd_from_ss(P, rsq, ssq, 64)
        if 'nb' not in SK:
            P.tt(nb16[:, 0:2, :].rearrange("p a (h d) -> p (a h) d", h=2), ut[:, 0:256].rearrange("p (h d) -> p h d", h=4),
                 rsq[:, 0:4].unsqueeze(2).to_broadcast([128, 4, 64]), ALU.mult)
            for hh in range(2):
                P.ts(nb16[:, 2, hh * 64:(hh + 1) * 64], ut[:, 384:448], rsq[:, 6:7], None, ALU.mult)
                P.ts(nb16[:, 3, hh * 64:(hh + 1) * 64], ut[:, 512:576], rsq[:, 8:9], None, ALU.mult)
        if 'pc' not in SK:
            P.copy(nb16[:, 4, :], ut[:, 256:384], 'pool')
        ts_ = slice(t * 128, (t + 1) * 128)
        if 'tr' not in SK:
            pT = C.bankbf(t % 2).rearrange("p (a b) -> p a b", a=8)
            for c in range(5):
                P.tr(pT[:, c, :], nb16[:, c, :], C.ident)
            if 'ev1' not in SK:
                P.act(QT[:, :, ts_], pT[:, 0:2, :], AF.Identity, scale=gcol[:, 0:1])
            if 'ev2' not in SK:
                P.ts(KsT[:, ts_], pT[:, 2, :], gcol[:, 2:3], None, ALU.mult)
                P.ts(KwT[:, ts_], pT[:, 3, :], gcol[:, 3:4], None, ALU.mult)
            if 'ev3' not in SK:
                P.copy(KVcT[:, ts_], pT[:, 4, :], 'act')
        if 'v' not in SK:
            P.copy(Vs[:, t, 0:64], ut[:, 448:512], 'pool')
            P.copy(Vw[:, t, 0:64], ut[:, 576:640], 'pool')
        if 'g' not in SK:
            P.act(G[:, t, :], ut[:, 640:652], AF.Exp, scale=-1.0)
            P.ts(G[:, t, :], G[:, t, :], 1.0, None, ALU.add)
            P.recip(G[:, t, :], G[:, t, :])
    if not os.environ.get('NSA_NORESET'):
        A.reset(m1)

    if os.environ.get('NSA_STOP') == 'a':
        A.reset(m0)
        return
    m1 = A.mark()
    w1 = A.alloc([32, 256], BF16)
    SKB = os.environ.get('NSA_SKIPB', '').split(',')
    for j in range(2):
        w1v = I['nsa_cmp_w1'][L, j].rearrange("(l d) h -> d l h", d=64)
        for l0 in range(0, 32, 8):
            if 'w1' not in SKB:
                P.dma('pool', w1[j * 64:(j + 1) * 64, l0:l0 + 8, :], w1v[:, l0:l0 + 8, :])
    w2 = A.alloc([2, 2, 64], BF16)
    for j in range(2):
        if 'w2' not in SKB:
            P.dma('pool', w2[:, j, :, :], I['nsa_cmp_w2'][L, j].rearrange("(t p) d -> p t d", p=128))
    posl = A.alloc([128], F32, parts=32)
    posT = A.alloc([32], BF16)
    if 'pos' not in SKB:
        for j in range(2):
            P.dma('sp', posl[:, j * 64:(j + 1) * 64], I['nsa_cmp_pos'][L, j])
        ppos = C.bank(2)[:, 0:32]
        P.tr(ppos, posl, C.identf[0:32, 0:32])
        P.copy(posT, ppos, 'dve')
    if os.environ.get('NSA_STOP') == 'b1':
        A.reset(m0)
        return
    hidT = A.alloc([2, 2, 256], BF16)
    biasc = A.alloc([4], F32)
    rhs_c = A.alloc([NBT, 129], F32)
    kcT = A.alloc([NBT * 128], BF16)
    kcn = A.alloc([128], BF16)
    kss = A.alloc([1], F32)
    krs = A.alloc([1], F32)
    kjunk = A.alloc([64], F32)
    for kv in range(2):
        rows = slice(kv * 64, (kv + 1) * 64)
        for ht in range(2):
            pb = C.bank(2 + ht)[:, 0:NCMP]
            for l in range(32):
                P.mm(pb, w1[rows, l, ht * 128:(ht + 1) * 128], KVcT[rows, l:l + 16 * (NCMP - 1) + 1:16], start=(l == 0), stop=(l == 31))
            pbb = C.bank(4 + ht)[:, 0:1]
            for l in range(32):
                P.mm(pbb, w1[rows, l, ht * 128:(ht + 1) * 128], posT[rows, l:l + 1], start=(l == 0), stop=(l == 31))
            bc = biasc[:, kv * 2 + ht:kv * 2 + ht + 1]
            P.copy(bc, pbb, 'dve')
            P.act(hidT[:, kv, ht, 0:NCMP], pb, AF.Silu, bias=bc)
    if os.environ.get('NSA_STOP') == 'b2':
        A.reset(m0)
        return
    for bt in range(NBT):
        nb = min(128, NCMP - bt * 128)
        bs = slice(bt * 128, bt * 128 + nb)
        pk = C.bank(2)[0:nb, 0:64]
        for ht in range(2):
            P.mm(pk, hidT[:, 0, ht, bs], w2[:, 0, ht, :], start=(ht == 0), stop=(ht == 1))
        P.act(kjunk[0:nb], pk, AF.Square, accum=kss[0:nb])
        rstd_from_ss(P, krs[0:nb], kss[0:nb], 64)
        for hh in range(2):
            P.ts(kcn[0:nb, hh * 64:(hh + 1) * 64], pk, krs[0:nb, 0:1], None, ALU.mult)
        pkt = C.bankbf(3)[:, 0:nb]
        P.tr(pkt, kcn[0:nb, :], C.ident[0:nb, 0:nb])
        P.ts(kcT[:, bs], pkt, gcol[:, 1:2], None, ALU.mult)
        pv = C.bank(4)[0:nb, 0:64]
        for ht in range(2):
            P.mm(pv, hidT[:, 1, ht, bs], w2[:, 1, ht, :], start=(ht == 0), stop=(ht == 1))
        P.copy(rhs_c[0:nb, bt, 0:64], pv, 'act')
        ov = rhs_c[:, bt, 64:64 + NSEL]
        P.memset(rhs_c[:, bt, 64:129], 1.0)
        P.asel(ov, ov, [[64, NSEL]], 63 - 16 * 128 * bt, -16, 0.0)
        P.asel(ov, ov, [[-64, NSEL]], 31 + 16 * 128 * bt, 16, 0.0)

    if os.environ.get('NSA_STOP') == 'b':
        A.reset(m0)
        return
    PcT = A.alloc([4, NBT, 512], F32)
    BSb = A.alloc([128], F32)
    BIGV = 1.0e4
    P.memset(BSb, 0.0)
    P.memset(BSb[0:64, 63:65], BIGV)
    P.memset(BSb[0:64, 65:128], -BIGV)
    P.memset(BSb[64:128, 64:66], BIGV)
    P.memset(BSb[64:128, 66:128], -BIGV)
    imp = A.alloc([64], F32)
    imp2 = A.alloc([64], F32)
    m8a = A.alloc([8], F32)
    m8b = A.alloc([8], F32)
    seln = A.alloc([128], BF16)
    P.memset(seln, 0.0)
    den = A.alloc([4], F32)
    cmb = A.alloc([4], F32)
    for tb in range(NQB):
        T0 = tb * 512
        live = []
        for h in range(4):
            hr = slice((h % 2) * 64, (h % 2) * 64 + 64)
            for bt in range(NBT):
                nb = min(128, NCMP - bt * 128)
                if 16 * (128 * bt) + 31 > T0 + 511:
                    continue
                live.append((h, bt))
                pb = C.bank(2 + (h * NBT + bt) % 4)[0:nb, :]
                P.mm(pb, kcT[hr, bt * 128:bt * 128 + nb], QT[hr, h // 2, T0:T0 + 512])
                P.act(PcT[0:nb, h, bt, :], pb, AF.Exp)
                P.asel(PcT[0:nb, h, bt, :], PcT[0:nb, h, bt, :], [[1, 512]], T0 - 2048 * bt - 31, -16, 0.0)
        for qs in range(4):
            t = tb * 4 + qs
            pos = [C.bank(6)[:, 0:258].rearrange("p (a b) -> p a b", a=2), C.bank(7)[:, 0:258].rearrange("p (a b) -> p a b", a=2)]
            for h in range(4):
                bts = [bt for (hh, bt) in live if hh == h]
                o = pos[h // 2][:, h % 2, :]
                if not bts:
                    continue
                for i, bt in enumerate(bts):
                    nb = min(128, NCMP - bt * 128)
                    P.mm(o, PcT[0:nb, h, bt, qs * 128:(qs + 1) * 128], rhs_c[0:nb, bt, :], start=(h % 2 == 0 and i == 0), stop=(i == len(bts) - 1))
            if not live:
                P.memset(MIXA[:, t, :], 0.0, 'dve')
                P.memset(imp[:, 0:NSEL], 0.0, 'dve')
            else:
                for h in range(4):
                    o = pos[h // 2][:, h % 2, :]
                    P.ts(den[:, h:h + 1], o[:, 128:129], 1e-30, None, ALU.max)
                    P.recip(den[:, h:h + 1], den[:, h:h + 1])
                    if h == 0:
                        P.ts(imp[:, 0:NSEL], o[:, 64:64 + NSEL], den[:, 0:1], None, ALU.mult)
                    else:
                        P.stt(imp[:, 0:NSEL], o[:, 64:64 + NSEL], den[:, h:h + 1], imp[:, 0:NSEL], ALU.mult, ALU.add)
                    P.tt(cmb[:, h:h + 1], den[:, h:h + 1], G[:, t, 3 * h:3 * h + 1], ALU.mult)
                    P.ts(MIXA[:, t, h * 64:(h + 1) * 64], o[:, 0:64], cmb[:, h:h + 1], None, ALU.mult)
            if NSEL > 16:
                P.tt(imp[:, 0:NSEL], imp[:, 0:NSEL], BSb[:, 64 - 2 * t:64 - 2 * t + NSEL], ALU.add)
                P.memset(imp[:, 0:1], BIGV, 'dve')
                P.op('dve', lambda e: e.max(out=m8a, in_=imp[:, 0:NSEL]), [imp[:, 0:NSEL]], [m8a])
                P.op('dve', lambda e: e.match_replace(out=imp2[:, 0:NSEL], in_to_replace=m8a, in_values=imp[:, 0:NSEL], imm_value=-1e9), [imp[:, 0:NSEL], m8a], [imp2[:, 0:NSEL]])
                P.op('dve', lambda e: e.max(out=m8b, in_=imp2[:, 0:NSEL]), [imp2[:, 0:NSEL]], [m8b])
                P.ts(seln[:, 0:NSEL], imp[:, 0:NSEL], m8b[:, 7:8], NEGM, ALU.is_lt, ALU.mult)
                P.ts(seln[:, 64:64 + NSEL], imp[:, 0:NSEL], m8b[:, 7:8], NEGM, ALU.is_lt, ALU.mult)
            pst = C.bankbf(t % 2)[:, 0:128]
            P.tr(pst, seln, C.ident)
            P.copy(SelT[:, t * 128:(t + 1) * 128], pst, 'act')
    A.reset(m1)

    if os.environ.get('NSA_STOP') == 'c':
        A.reset(m0)
        return
    m1 = A.mark()
    PTs = [A.alloc([512], BF16) for _ in range(3)]
    rd2 = A.alloc([8], F32)
    pi = 0
    oi = 0
    iters = []
    for branch in range(2):
        KT = KsT if branch == 0 else KwT
        V = Vs if branch == 0 else Vw
        for h in range(4):
            hr = slice((h % 2) * 64, (h % 2) * 64 + 64)
            for qb in range(NQB):
                kts = list(range(0, 4 * qb + 4)) if branch == 0 else list(range(max(0, 4 * qb - 4), 4 * qb + 4))
                ob = C.bank(6 + oi % 2)[:, 0:260].rearrange("p (a b) -> p a b", a=4)
                oi += 1
                first = True
                lastkt = {qs: 4 * qb + qs for qs in range(4)}
                for kt in kts:
                    pb = C.bank(2 + pi % 4)
                    PT = PTs[pi % 3]
                    pi += 1
                    ks = slice(kt * 128, (kt + 1) * 128)
                    diag = kt >= 4 * qb
                    pvs = []
                    for qs in range(4):
                        if diag and qs < kt - 4 * qb:
                            continue
                        if (not diag) and branch == 1 and qs > kt - (4 * qb - 4):
                            continue
                        pvs.append((qs, first, kt == lastkt[qs]))
                        first = False

                    def qk(pb=pb, PT=PT, KT=KT, hr=hr, ks=ks, h=h, qb=qb, diag=diag, kt=kt, branch=branch):
                        P.mm(pb, KT[hr, ks], QT[hr, h // 2, qb * 512:(qb + 1) * 512], start=True, stop=False)
                        if branch == 0:
                            r0 = (h % 2) * 64
                            P.mm(pb, Rall[r0:r0 + NSEL, ks], SelT[r0:r0 + NSEL, qb * 512:(qb + 1) * 512], start=False, stop=not diag)
                            if diag:
                                P.mm(pb, C.ident, C.Cm[:, kt - 4 * qb, :], start=False, stop=True)
                        else:
                            mk = C.Cm[:, kt - 4 * qb, :] if diag else C.Cw[:, kt - (4 * qb - 4), :]
                            P.mm(pb, C.ident, mk, start=False, stop=True)
                        P.act(PT, pb, AF.Exp)

                    def pv(PT=PT, ob=ob, pvs=pvs, V=V, kt=kt):
                        for (qs, st_, sp_) in pvs:
                            P.mm(ob[:, qs, :], PT[:, qs * 128:(qs + 1) * 128], V[:, kt, 0:65], start=st_, stop=sp_)
                    iters.append([qk, pv, None])

                def fin(ob=ob, qb=qb, h=h, branch=branch):
                    for qs in range(4):
                        t = qb * 4 + qs
                        P.recip(rd2[:, qs:qs + 1], ob[:, qs, 64:65])
                        P.tt(rd2[:, 4 + qs:5 + qs], rd2[:, qs:qs + 1], G[:, t, 3 * h + 1 + branch:3 * h + 2 + branch], ALU.mult)
                        P.stt(MIXA[:, t, h * 64:(h + 1) * 64], ob[:, qs, 0:64], rd2[:, 4 + qs:5 + qs], MIXA[:, t, h * 64:(h + 1) * 64], ALU.mult, ALU.add)
                iters[-1][2] = fin
    run_pipelined(iters)
    mb = A.alloc([NT, 256], BF16)
    P.copy(mb, MIXA, 'dve')
    P.dma('sp', C.MIX[:, 0:256].rearrange("(t p) c -> p t c", p=128), mb)
    A.reset(m1)
    A.reset(m0)


def bcast_row(C, out_sb, row_dram, n, scale=None, q='sp'):
    P, A = C.P, C.A
    m = A.mark()
    tmp = A.alloc([n], F32, parts=1)
    P.dma(q, tmp, row_dram.rearrange("(o n) -> o n", o=1))
    for c0 in range(0, n, 512):
        c1 = min(n, c0 + 512)
        pb = C.bank(2 + (c0 // 512) % 2)[:, 0:c1 - c0]
        P.mm(pb, C.ones_row, tmp[0:1, c0:c1])
        if scale is None:
            P.copy(out_sb[:, c0:c1], pb, 'dve')
        else:
            P.ts(out_sb[:, c0:c1], pb, scale, None, ALU.mult)
    A.reset(m)


def phase_diff(C, L):
    P, A, I = C.P, C.A, C.I
    S, NT, NQB = C.S, C.NT, C.NQB
    m0 = A.mark()
    lam_init = 0.8 - 0.6 * math.exp(-0.3 * L)
    DQT = A.alloc([3, S], BF16)
    DKT = A.alloc([3, S], BF16)
    DV = A.alloc([NT, 4, 66], BF16)
    MIXB = A.alloc([NT, 256], BF16)
    gcol = A.alloc([2], F32)
    gsrc = I['diff_qk_gain'][L]
    for r in range(4):
        P.dma('sp', gcol[r * 32:(r + 1) * 32, :], gsrc.rearrange("j d -> d j"), allow_slow_non_contiguous=True)
    P.ts(gcol[:, 0:1], gcol[:, 0:1], 32 ** -0.5, None, ALU.mult)
    P.memset(DV[:, :, :, 64:66], 1.0)
    lam1 = A.alloc([128], F32, parts=1)
    P.dma('sp', lam1, I['diff_lambda'][L].rearrange("(o a) b -> o (a b)", o=1))
    lp = A.alloc([64], F32, parts=1)
    lsum = A.alloc([2], F32, parts=1)
    P.tt(lp[:, 0:32], lam1[:, 0:32], lam1[:, 32:64], ALU.mult)
    P.tt(lp[:, 32:64], lam1[:, 64:96], lam1[:, 96:128], ALU.mult)
    P.reduce(lsum, lp.rearrange("p (a b) -> p a b", a=2))
    P.act(lsum, lsum, AF.Exp)
    lf = A.alloc([1], F32, parts=1)
    P.tt(lf, lsum[:, 1:2], lsum[:, 0:1], ALU.subtract)
    P.ts(lf, lf, -lam_init, None, ALU.add)
    neglam = A.alloc([1], F32)
    pbl = C.bank(2)[:, 0:1]
    P.mm(pbl, C.ones_row, lf[0:1, 0:1])
    P.copy(neglam, pbl, 'dve')
    subg = A.alloc([64], F32)
    bcast_row(C, subg, I['diff_norm'][L], 64, scale=(1.0 - lam_init))

    m1 = A.mark()
    uts = [A.alloc([768], F32) for _ in range(2)]
    sq = A.alloc([512], F32)
    ssq = A.alloc([16], F32)
    rsq = A.alloc([16], F32)
    nb16 = A.alloc([4, 128], BF16)
    P.dma('sp', uts[0], C.U[0:128, 652:1420])
    for t in range(NT):
        ut = uts[t % 2]
        if t + 1 < NT:
            P.dma('sp', uts[(t + 1) % 2], C.U[(t + 1) * 128:(t + 2) * 128, 652:1420])
        P.act(sq, ut[:, 0:512], AF.Square)
        P.reduce(ssq, sq.rearrange("p (a b) -> p a b", a=16))
        rstd_from_ss(P, rsq, ssq, 32)
        P.tt(nb16.rearrange("p a (g d) -> p (a g) d", g=4), ut[:, 0:512].rearrange("p (g d) -> p g d", g=16),
             rsq.unsqueeze(2).to_broadcast([128, 16, 32]), ALU.mult)
        pT = C.bankbf(t % 2).rearrange("p (a b) -> p a b", a=8)
        nbf = nb16.rearrange("p a b -> p (a b)")
        for qk in range(2):
            for j in range(3):
                w = 96 if j < 2 else 64
                P.tr(pT[0:w, qk * 3 + j, :], nbf[:, qk * 256 + j * 96:qk * 256 + j * 96 + w], C.ident)
        ts_ = slice(t * 128, (t + 1) * 128)
        P.ts(DQT[0:96, :, ts_], pT[0:96, 0:3, :], gcol[0:96, 0:1], None, ALU.mult)
        P.ts(DKT[0:96, :, ts_], pT[0:96, 3:6, :], gcol[0:96, 1:2], None, ALU.mult)
        P.copy(DV[:, t, :, 0:64], ut[:, 512:768].rearrange("p (h d) -> p h d", h=4), 'act')
    A.reset(m1)

    PTs = [A.alloc([512], BF16) for _ in range(3)]
    rd = A.alloc([4], F32)
    otmp = A.alloc([64], F32)
    o2 = A.alloc([64], F32)
    junk = A.alloc([64], F32)
    ss1 = A.alloc([1], F32)
    rs1 = A.alloc([1], F32)
    pi = 0
    oi = 0
    iters = []
    for h in range(4):
        for qb in range(NQB):
            obs = [C.bank(4 + 2 * (oi % 2) + c)[:, 0:260].rearrange("p (a b) -> p a b", a=4) for c in range(2)]
            oi += 1
            first = [True, True]
            for kt in range(4 * qb + 4):
                ks = slice(kt * 128, (kt + 1) * 128)
                diag = kt >= 4 * qb
                for c in range(2):
                    g = h * 2 + c
                    hb = g // 3
                    r0 = (g % 3) * 32
                    rows = slice(r0, r0 + 32)
                    pb = C.bank(pi % 4)
                    PT = PTs[pi % 3]
                    pi += 1
                    pvs = []
                    for qs in range(4):
                        if diag and qs < kt - 4 * qb:
                            continue
                        pvs.append((qs, first[c], kt == 4 * qb + qs))
                        first[c] = False

                    def qk(pb=pb, PT=PT, rows=rows, hb=hb, ks=ks, qb=qb, diag=diag, kt=kt):
                        P.mm(pb, DKT[rows, hb, ks], DQT[rows, hb, qb * 512:(qb + 1) * 512], start=True, stop=not diag)
                        if diag:
                            P.mm(pb, C.ident, C.Cm[:, kt - 4 * qb, :], start=False, stop=True)
                        P.act(PT, pb, AF.Exp)

                    def pv(PT=PT, ob=obs[c], pvs=pvs, kt=kt, h=h):
                        for (qs, st_, sp_) in pvs:
                            P.mm(ob[:, qs, :], PT[:, qs * 128:(qs + 1) * 128], DV[:, kt, h, 0:65], start=st_, stop=sp_)
                    iters.append([qk, pv, None])

            def fin(obs=obs, qb=qb, h=h):
                for qs in range(4):
                    t = qb * 4 + qs
                    P.recip(rd[:, 0:1], obs[0][:, qs, 64:65])
                    P.recip(rd[:, 1:2], obs[1][:, qs, 64:65])
                    P.tt(rd[:, 2:3], rd[:, 1:2], neglam, ALU.mult)
                    P.ts(otmp, obs[1][:, qs, 0:64], rd[:, 2:3], None, ALU.mult)
                    P.stt(o2, obs[0][:, qs, 0:64], rd[:, 0:1], otmp, ALU.mult, ALU.add)
                    P.act(junk, o2, AF.Square, accum=ss1)
                    rstd_from_ss(P, rs1, ss1, 64)
                    P.stt(MIXB[:, t, h * 64:(h + 1) * 64], o2, rs1[:, 0:1], subg, ALU.mult, ALU.mult)
            iters[-1][2] = fin
    run_pipelined(iters)
    P.dma('sp', C.MIX[:, 256:512].rearrange("(t p) c -> p t c", p=128), MIXB)
    A.reset(m0)


def phase_gla(C, L, hgrn):
    P, A, I = C.P, C.A, C.I
    S, NT = C.S, C.NT
    m0 = A.mark()
    if hgrn:
        c0, W, NCH, DK = 2204, 1024, 256, 64
        qo, ko, vo, ogo = 0, 256, 512, 768
        mixcol = 768
        qscale = 1.0
    else:
        c0, W, NCH, DK = 1420, 784, 128, 32
        qo, ko, vo, lro, ogo = 0, 128, 256, 512, 528
        mixcol = 512
        qscale = 32 ** -0.5
    L1 = A.alloc([128], F32)
    L2 = A.alloc([128], F32)
    L3 = A.alloc([128], F32)
    IND = A.alloc([2], F32)
    P.memset(L1, 1.0)
    P.asel(L1, L1, [[1, 128]], 0, -1, 0.0)
    P.memset(L1[0:64, 64:128], 0.0)
    P.memset(L3, 1.0)
    P.asel(L3, L3, [[-1, 128]], -1, 1, 0.0)
    P.memset(L3[64:128, 0:64], 0.0)
    P.memset(L2, 0.0)
    P.memset(L2[0:32, 0:64], 1.0)
    P.memset(L2[64:96, 64:128], 1.0)
    P.tt(L2, L1, L2, ALU.subtract, eng='pool')
    P.memset(IND, 0.0)
    P.memset(IND[0:64, 0:1], 1.0)
    P.memset(IND[64:128, 1:2], 1.0)
    gn = A.alloc([64], F32)
    bcast_row(C, gn, I['hgrn_norm' if hgrn else 'gla_norm'][L], 64)
    if hgrn:
        lb = A.alloc([256], F32)
        oml = A.alloc([256], F32)
        if L == 0:
            P.memset(lb, 0.0)
            P.memset(oml, 1.0)
        else:
            l0 = A.alloc([256], F32)
            bcast_row(C, lb, I['hgrn_lb_logits'][1], 256)
            bcast_row(C, l0, I['hgrn_lb_logits'][0], 256)
            P.tt(lb, lb, l0, ALU.subtract)
            P.act(lb, lb, AF.Sigmoid)
            P.ts(oml, lb, -1.0, 1.0, ALU.mult, ALU.add)
    else:
        w_aug = A.alloc([128], F32, parts=33)
        glrT = A.alloc([128], F32, parts=33)
        P.memset(w_aug, 0.0)
        P.memset(glrT, 0.0)
        P.memset(glrT[32:33, :], 1.0)
        P.dma('sp', w_aug[0:16, :], I['gla_w_gate2'][L])
        P.dma('sp', w_aug[32:33, :], I['gla_b_gate'][L].rearrange("(o n) -> o n", o=1))
    S32 = A.alloc([4, 64], F32)
    P.memset(S32, 0.0)
    ring = [A.alloc([4, 64], BF16) for _ in range(4)]
    ATs = [A.alloc([4, 128], BF16) for _ in range(2)]
    for a_ in ATs:
        P.memset(a_, 0.0)
    MIXC = A.alloc([NT, 256], BF16)
    Af32 = A.alloc([4, 128], F32)
    uts = [A.alloc([W], F32) for _ in range(2)]
    la = A.alloc([NCH], F32)
    kk = A.alloc([NCH], F32)
    e_ = A.alloc([NCH], F32)
    E = A.alloc([4, NCH], F32)
    qt16 = A.alloc([3, NCH], BF16)
    ks16 = A.alloc([NCH], BF16)
    v16 = A.alloc([256], BF16)
    T16 = A.alloc([12, 128], BF16)
    DEC = A.alloc([4, 2], F32)
    o_sb = A.alloc([256], F32)
    sq = A.alloc([256], F32)
    ss4 = A.alloc([4], F32)
    rs4 = A.alloc([4], F32)
    sil = A.alloc([256], F32)
    GS = os.environ.get('GLA_STOP')
    P.dma('sp', uts[0], C.U[0:128, c0:c0 + W])
    for t in range(NT):
        ut = uts[t % 2]
        if t + 1 < NT:
            P.dma('sp', uts[(t + 1) % 2], C.U[(t + 1) * 128:(t + 2) * 128, c0:c0 + W])
        Q = ut[:, qo:qo + NCH]
        if hgrn:
            P.act(e_, ut[:, ko:ko + NCH], AF.Exp, scale=-1.0)
            P.ts(e_, e_, 1.0, None, ALU.add)
            P.recip(e_, e_)
            P.tt(e_, e_, oml, ALU.mult)
            P.tt(e_, e_, lb, ALU.add)
            P.act(la, e_, AF.Ln)
            P.ts(kk, e_, -1.0, 1.0, ALU.mult, ALU.add)
            K = kk
        else:
            pg = C.bank(0)[0:16, 0:128]
            P.tr(pg, ut[:, lro:lro + 16], C.identf)
            P.copy(glrT[0:16, :], pg, 'dve')
            pz = C.bank(1)[:, 0:128]
            P.mm(pz, glrT[0:33, :], w_aug[0:33, :])
            P.act(e_, pz, AF.Exp, scale=-1.0)
            P.act(e_, e_, AF.Ln, bias=1.0)
            P.ts(la, e_, -1.0 / 16.0, None, ALU.mult)
            K = ut[:, ko:ko + NCH]
        if GS == 'la':
            continue
        pB = [C.bank(2 + i)[:, 0:NCH] for i in range(3)]
        P.mm(pB[0], L1, la)
        P.mm(pB[1], L2, la)
        P.mm(pB[2], L3, la)
        pdec = C.bank(0)[:, 256:264].rearrange("p (a b) -> p a b", a=4)
        for h in range(4):
            P.mm(pdec[0:DK, h, :], la[:, h * DK:(h + 1) * DK], IND, start=(h == 0), stop=True)
        P.act(E[:, 0, :], pB[0], AF.Exp)
        P.act(E[:, 1, :], pB[1], AF.Exp)
        P.act(E[:, 2, :], pB[1], AF.Exp, scale=-1.0)
        P.act(E[:, 3, :], pB[2], AF.Exp)
        P.act(DEC[0:DK], pdec[0:DK], AF.Exp)
        if GS == 'cum':
            continue
        P.stt(qt16[:, 0, :], Q, qscale, E[:, 0, :], ALU.mult, ALU.mult)
        P.stt(qt16[:, 1, :], Q, qscale, E[:, 1, :], ALU.mult, ALU.mult)
        P.tt(qt16[:, 2, :], K, E[:, 2, :], ALU.mult)
        P.tt(ks16, K, E[:, 3, :], ALU.mult)
        P.copy(v16, ut[:, vo:vo + 256], 'pool')
        pTa = C.bankbf(5).rearrange("p (a b) -> p a b", a=8)
        pTb = C.bankbf(6).rearrange("p (a b) -> p a b", a=8)
        for wi in range(3):
            for h in range(4):
                idx = wi * 4 + h
                dstp = pTa[0:DK, idx, :] if idx < 8 else pTb[0:DK, idx - 8, :]
                P.tr(dstp, qt16[:, wi, h * DK:(h + 1) * DK], C.ident)
        P.copy(T16[0:DK, 0:8, :], pTa[0:DK, 0:8, :], 'act')
        P.copy(T16[0:DK, 8:12, :], pTb[0:DK, 0:4, :], 'act')
        if GS == 'tr':
            continue
        pA = C.bank(7).rearrange("p (a b) -> p a b", a=4)
        for h in range(4):
            P.mm(pA[:, h, :], T16[0:DK, 8 + h, :], T16[0:DK, 4 + h, :], start=(h == 0), stop=True)
        AT = ATs[t % 2]
        P.copy(Af32, pA, 'act')
        P.asel(AT[0:64, :, 0:64], Af32[0:64, :, 0:64], [[0, 4], [1, 64]], 0, -1, 0.0)
        P.asel(AT[64:128, :, 64:128], Af32[64:128, :, 64:128], [[0, 4], [1, 64]], 0, -1, 0.0)
        if GS == 'A':
            continue
        pU = [C.bank(1)[:, 0:256].rearrange("p (a b) -> p a b", a=4), C.bank(2)[:, 0:256].rearrange("p (a b) -> p a b", a=4)]
        for c in range(2):
            cr = slice(64 * c, 64 * c + 64)
            for h in range(4):
                P.mm(pU[c][0:DK, h, :], ks16[cr, h * DK:(h + 1) * DK], v16[cr, h * 64:(h + 1) * 64], start=(h == 0), stop=True)
        for c in range(2):
            n = 2 * t + c
            P.tt(S32[0:DK], S32[0:DK], DEC[0:DK, :, c:c + 1].to_broadcast([DK, 4, 64]), ALU.mult)
            P.tt(S32[0:DK], S32[0:DK], pU[c][0:DK], ALU.add)
            P.copy(ring[n % 4][0:DK], S32[0:DK], 'pool')
        if GS == 'U':
            continue
        pO = C.bank(3)[:, 0:256].rearrange("p (a b) -> p a b", a=4)
        for h in range(4):
            P.mm(pO[:, h, :], AT[:, h, :], v16[:, h * 64:(h + 1) * 64], start=(h == 0), stop=False)
        for h in range(4):
            for c in range(2):
                n_prev = 2 * t + c - 1
                if n_prev < 0:
                    continue
                P.mm(pO[64 * c:64 * c + 64, h, :], T16[0:DK, h, 64 * c:64 * c + 64], ring[n_prev % 4][0:DK, h, :], start=False, stop=True)
        if GS == 'O':
            continue
        P.copy(o_sb, pO.rearrange("p a b -> p (a b)"), 'act')
        P.tt(sq, o_sb, o_sb, ALU.mult)
        P.reduce(ss4, sq.rearrange("p (a b) -> p a b", a=4))
        rstd_from_ss(P, rs4, ss4, 64)
        P.act(sil, ut[:, ogo:ogo + 256], AF.Exp, scale=-1.0)
        P.ts(sil, sil, 1.0, None, ALU.add)
        P.recip(sil, sil)
        P.tt(sil, sil, ut[:, ogo:ogo + 256], ALU.mult)
        P.tt(sil.rearrange("p (a b) -> p a b", a=4), sil.rearrange("p (a b) -> p a b", a=4), gn.unsqueeze(1).to_broadcast([128, 4, 64]), ALU.mult)
        P.tt(o_sb.rearrange("p (a b) -> p a b", a=4), o_sb.rearrange("p (a b) -> p a b", a=4), rs4.unsqueeze(2).to_broadcast([128, 4, 64]), ALU.mult)
        P.tt(MIXC[:, t, :], o_sb, sil, ALU.mult)
    P.dma('sp', C.MIX[:, mixcol:mixcol + 256].rearrange("(t p) c -> p t c", p=128), MIXC)
    A.reset(m0)


def phase_out(C, L, src, dst):
    P, A, I = C.P, C.A, C.I
    S, NT = C.S, C.NT
    moe = (L % 2 == 1)
    NBLK = min(S, 1024)
    NB5 = NBLK // 512
    TPB = NBLK // 128
    m0 = A.mark()
    wout = A.alloc([8, D], BF16)
    for kc in range(8):
        P.dma('pool', wout[:, kc, :], I['w_out'][L, kc * 128:(kc + 1) * 128, :])
    gbc = A.alloc([D], F32)
    bcast_row(C, gbc, I['norm_ffn'][L], D)
    if moe:
        rt32 = A.alloc([8, 8], F32)
        P.dma('sp', rt32, I['moe_router'][0].rearrange("(kc p) e -> p kc e", p=128))
        lg = A.alloc([8], F32)
        v8 = A.alloc([8], F32)
        ex = A.alloc([8], F32)
        sm = A.alloc([4], F32)
        msk = A.alloc([8], F32)
        comb = A.alloc([8], F32)
        combT = A.alloc([128], F32, parts=8)
    hts = [A.alloc([D], F32) for _ in range(2)]
    mts = [A.alloc([D], BF16) for _ in range(2)]
    mT = A.alloc([8, 128], BF16)
    h1 = A.alloc([D], F32)
    junk = A.alloc([D], BF16)
    c32 = A.alloc([D], F32)
    cT32 = A.alloc([8, 128], F32)
    cT16 = A.alloc([8, 128], BF16)
    ss = A.alloc([1], F32)
    rs = A.alloc([1], F32)
    P.dma('sp', hts[0], src[0:128, :])
    P.dma('sp', mts[0], C.MIX[0:128, :])
    for t in range(NT):
        ht, mt = hts[t % 2], mts[t % 2]
        if t + 1 < NT:
            P.dma('sp', hts[(t + 1) % 2], src[(t + 1) * 128:(t + 2) * 128, :])
            P.dma('sp', mts[(t + 1) % 2], C.MIX[(t + 1) * 128:(t + 2) * 128, :])
        pT = C.bankbf(0).rearrange("p (a b) -> p a b", a=8)
        for c in range(8):
            P.tr(pT[:, c, :], mt[:, c * 128:(c + 1) * 128], C.ident)
        P.copy(mT, pT, 'act')
        for hf in range(2):
            pb = C.bank(1 + hf)
            for kc in range(8):
                P.mm(pb, mT[:, kc, :], wout[:, kc, hf * 512:(hf + 1) * 512], start=(kc == 0), stop=(kc == 7))
            P.tt(h1[:, hf * 512:(hf + 1) * 512], pb, ht[:, hf * 512:(hf + 1) * 512], ALU.add)
        P.dma('sp', dst[t * 128:(t + 1) * 128, :], h1)
        P.act(junk, h1, AF.Square, accum=ss)
        rstd_from_ss(P, rs, ss, D)
        P.stt(c32, h1, rs[:, 0:1], gbc, ALU.mult, ALU.mult)
        for hf in range(2):
            pc = C.bank(3 + hf).rearrange("p (a b) -> p a b", a=4)
            for c in range(4):
                P.tr(pc[:, c, :], c32[:, (hf * 4 + c) * 128:(hf * 4 + c + 1) * 128], C.identf)
            P.copy(cT16[:, hf * 4:(hf + 1) * 4, :], pc, 'act')
            if moe:
                P.copy(cT32[:, hf * 4:(hf + 1) * 4, :], pc, 'dve')
        P.dma('sp', C.CTD[:, :, t * 128:(t + 1) * 128], cT16)
        if moe:
            pl = C.bank(5)[:, 0:8]
            for kc in range(8):
                P.mm(pl, cT32[:, kc, :], rt32[:, kc, :], start=(kc == 0), stop=(kc == 7))
            P.copy(lg, pl, 'dve')
            P.op('dve', lambda e: e.max(out=v8, in_=lg), [lg], [v8])
            P.ts(sm[:, 0:1], v8[:, 0:1], -1.0, None, ALU.mult)
            P.act(ex, lg, AF.Exp, bias=sm[:, 0:1])
            P.act(sm[:, 1:2], v8[:, 1:2], AF.Exp, bias=sm[:, 0:1])
            P.ts(sm[:, 2:3], sm[:, 1:2], 1.0, None, ALU.add)
            P.recip(sm[:, 2:3], sm[:, 2:3])
            P.ts(msk, lg, v8[:, 1:2], None, ALU.is_ge)
            P.stt(comb, ex, sm[:, 2:3], msk, ALU.mult, ALU.mult)
            pcm = C.bank(6)[0:8, 0:128]
            P.tr(pcm, comb, C.identf)
            P.copy(combT, pcm, 'dve')
            P.dma('sp', C.COMB[:, t * 128:(t + 1) * 128], combT)
    A.reset(m0)

    wpg = A.alloc([8, D], BF16)
    for kc in range(8):
        P.dma('pool', wpg[:, kc, :], I['ple_w_gate'][L, kc * 128:(kc + 1) * 128, :])
    wpp = A.alloc([2, D], BF16)
    for j in range(2):
        P.dma('pool', wpp[:, j, :], I['ple_w_proj'][L, j * 128:(j + 1) * 128, :])
    gpT = A.alloc([8], F32)
    P.dma('sp', gpT, I['ple_norm'][L].rearrange("(c p) -> p c", p=128), allow_slow_non_contiguous=True)
    CT = A.alloc([8, NBLK], BF16)
    hidden = A.alloc([28, NBLK], BF16)
    accT = A.alloc([8, NBLK], F32)
    wgs = [A.alloc([8, 256], BF16) for _ in range(2)]
    wus = [A.alloc([8, 256], BF16) for _ in range(2)]
    wds = [A.alloc([14, 128], BF16) for _ in range(2)]
    sg = [A.alloc([512], BF16) for _ in range(2)]
    tmpo = A.alloc([512], F32)
    if moe:
        sel_all = A.alloc([8, 128], F32, parts=8)
        P.memset(sel_all, 1.0)
        P.asel(sel_all, sel_all, [[1, 8], [0, 128]], 0, -1, 0.0, cmp=ALU.is_equal)
        cmbT = A.alloc([NBLK], F32, parts=8)
        cbc = A.alloc([NBLK], F32)
        WG = lambda e: I['moe_w_gate'][0, e]
        WU = lambda e: I['moe_w_up'][0, e]
        WD = lambda e: I['moe_w_down'][0, e]
        NE = 8
    else:
        WG = lambda e: I['ffn_w_gate'][0]
        WU = lambda e: I['ffn_w_up'][0]
        WD = lambda e: I['ffn_w_down'][0]
        NE = 1
    h1t = A.alloc([D], F32)
    h2 = h1t
    hn = A.alloc([D], BF16)
    aT = A.alloc([8, 128], BF16)
    gate = A.alloc([D], F32)
    pt32 = A.alloc([256], F32)
    pt16 = A.alloc([256], BF16)
    ppT = A.alloc([2, 128], BF16)
    junk = gate[:, 0:512].bitcast(BF16)
    ss = A.alloc([1], F32)
    rs = A.alloc([1], F32)

    def load_gu(e, fg, buf):
        wgv = WG(e).rearrange("(kc p) f -> p kc f", p=128)
        wuv = WU(e).rearrange("(kc p) f -> p kc f", p=128)
        P.dma('pool', wgs[buf], wgv[:, :, fg * 256:(fg + 1) * 256])
        P.dma('pool', wus[buf], wuv[:, :, fg * 256:(fg + 1) * 256])

    def load_d(e, dh, buf):
        dc, hf_ = dh // 2, dh % 2
        wdv = WD(e).rearrange("(f p) d -> p f d", p=128)
        for f0 in range(0, 14, 7):
            P.dma('pool', wds[buf][:, f0:f0 + 7, :], wdv[:, hf_ * 14 + f0:hf_ * 14 + f0 + 7, dc * 128:(dc + 1) * 128])

    gi = 0
    di = 0
    for blk in range(S // NBLK):
        T0 = blk * NBLK
        P.dma('sp', CT, C.CTD[:, :, T0:T0 + NBLK])
        if moe:
            P.dma('sp', cmbT, C.COMB[:, T0:T0 + NBLK])
        for e in range(NE):
            if moe:
                for nb in range(NB5):
                    pbc = C.bank(6 + nb % 2)
                    P.mm(pbc, sel_all[0:8, e, :], cmbT[0:8, nb * 512:(nb + 1) * 512])
                    P.copy(cbc[:, nb * 512:(nb + 1) * 512], pbc, 'act')
            load_gu(e, 0, gi % 2)
            for fg in range(14):
                if fg + 1 < 14:
                    load_gu(e, fg + 1, (gi + 1) % 2)
                wg, wu = wgs[gi % 2], wus[gi % 2]
                gi += 1
                for fc in range(2):
                    f = fg * 2 + fc
                    for nb in range(NB5):
                        pg = C.bank(0 + (f * NB5 + nb) % 2)
                        pu = C.bank(2 + (f * NB5 + nb) % 2)
                        for kc in range(8):
                            P.mm(pg, wg[:, kc, fc * 128:(fc + 1) * 128], CT[:, kc, nb * 512:(nb + 1) * 512], start=(kc == 0), stop=(kc == 7))
                        for kc in range(8):
                            P.mm(pu, wu[:, kc, fc * 128:(fc + 1) * 128], CT[:, kc, nb * 512:(nb + 1) * 512], start=(kc == 0), stop=(kc == 7))
                        sgb = sg[(f * NB5 + nb) % 2]
                        P.act(sgb, pg, AF.Silu)
                        P.tt(hidden[:, f, nb * 512:(nb + 1) * 512], sgb, pu, ALU.mult)
            load_d(e, 0, di % 2)
            for dc in range(8):
                pos_ = [C.bank(4 + nb) for nb in range(NB5)]
                for hf_ in range(2):
                    dh = dc * 2 + hf_
                    if dh + 1 < 16:
                        load_d(e, dh + 1, (di + 1) % 2)
                    wd = wds[di % 2]
                    di += 1
                    for nb in range(NB5):
                        for f in range(14):
                            ff = hf_ * 14 + f
                            P.mm(pos_[nb], wd[:, f, :], hidden[:, ff, nb * 512:(nb + 1) * 512], start=(ff == 0), stop=(ff == 27))
                for nb in range(NB5):
                    po = pos_[nb]
                    dstp = accT[:, dc, nb * 512:(nb + 1) * 512]
                    if not moe:
                        P.copy(dstp, po, 'act')
                    elif e == 0:
                        P.tt(dstp, po, cbc[:, nb * 512:(nb + 1) * 512], ALU.mult)
                    else:
                        P.tt(tmpo, po, cbc[:, nb * 512:(nb + 1) * 512], ALU.mult)
                        P.tt(dstp, dstp, tmpo, ALU.add)
        for tl in range(TPB):
            t = blk * TPB + tl
            rowsl = slice(t * 128, (t + 1) * 128)
            P.dma('sp', h1t, dst[rowsl, :])
            P.dma('sp', pt32, I['p'][L, rowsl, :])
            for hf in range(2):
                pc = C.bank(hf).rearrange("p (a b) -> p a b", a=4)
                for c in range(4):
                    P.tr(pc[:, c, :], accT[:, hf * 4 + c, tl * 128:(tl + 1) * 128], C.identf)
                P.tt(h2[:, hf * 512:(hf + 1) * 512], pc.rearrange("p a b -> p (a b)"), h1t[:, hf * 512:(hf + 1) * 512], ALU.add)
            P.act(junk, h2, AF.Square, accum=ss)
            rstd_from_ss(P, rs, ss, D)
            P.ts(hn, h2, rs[:, 0:1], None, ALU.mult)
            pT = C.bankbf(2).rearrange("p (a b) -> p a b", a=8)
            for c in range(8):
                P.tr(pT[:, c, :], hn[:, c * 128:(c + 1) * 128], C.ident)
            P.tt(aT, pT, gpT.unsqueeze(2).to_broadcast([128, 8, 128]), ALU.mult)
            P.copy(pt16, pt32, 'act')
            pP = C.bankbf(3).rearrange("p (a b) -> p a b", a=8)
            for j in range(2):
                P.tr(pP[:, j, :], pt16[:, j * 128:(j + 1) * 128], C.ident)
            P.copy(ppT, pP[:, 0:2, :], 'act')
            for hf in range(2):
                pgt = C.bank(4 + hf)
                for kc in range(8):
                    P.mm(pgt, aT[:, kc, :], wpg[:, kc, hf * 512:(hf + 1) * 512], start=(kc == 0), stop=(kc == 7))
                P.act(gate[:, hf * 512:(hf + 1) * 512], pgt, AF.Exp, scale=-1.0)
                P.ts(gate[:, hf * 512:(hf + 1) * 512], gate[:, hf * 512:(hf + 1) * 512], 1.0, None, ALU.add)
                P.recip(gate[:, hf * 512:(hf + 1) * 512], gate[:, hf * 512:(hf + 1) * 512])
                ppp = C.bank(6 + hf)
                for j in range(2):
                    P.mm(ppp, ppT[:, j, :], wpp[:, j, hf * 512:(hf + 1) * 512], start=(j == 0), stop=(j == 1))
                P.tt(gate[:, hf * 512:(hf + 1) * 512], ppp, gate[:, hf * 512:(hf + 1) * 512], ALU.mult)
            P.tt(h2, h2, gate, ALU.add)
            P.dma('sp', dst[rowsl, :], h2)
    A.reset(m0)


_CACHE = {}


def kernel(**inputs):
    S = 4096
    if 'nc' not in _CACHE:
        _CACHE['nc'] = build(S)
    nc = _CACHE['nc']
    names = [k for k in inputs if k not in ('x', 'p')]
    in_maps = []
    for b in range(8):
        mp = {'x': np.ascontiguousarray(inputs['x'][b], dtype=np.float32),
              'p': np.ascontiguousarray(inputs['p'][:, b], dtype=np.float32)}
        for k in names:
            mp[k] = np.ascontiguousarray(inputs[k], dtype=np.float32)
        in_maps.append(mp)
    res = run_bass_kernel_spmd(nc, in_maps, core_ids=list(range(8)))
    out = np.stack([np.asarray(r["y"], dtype=np.float32) for r in res.results], axis=0)
    return out
```

```python
import math
import os
import contextlib
import numpy as np
import concourse.bass as bass
import concourse.mybir as mybir
from concourse.bass_utils import run_bass_kernel_spmd
from concourse.alu_op_type import AluOpType as ALU

F32 = mybir.dt.float32
BF16 = mybir.dt.bfloat16
I32 = mybir.dt.int32
AF = mybir.ActivationFunctionType
AX = mybir.AxisListType

NSLOT = 8
D = 1024
INC = 3228
DFF = 3584
NEGM = -30000.0
EPS = 1e-6


def box_of(ap):
    t = ap.tensor
    rowlen = 1
    for s in list(t.shape)[1:]:
        rowlen *= int(s)
    if str(ap.space) != 'DRAM':
        rowlen = int(ap.ap[0][0]) if len(ap.ap) > 0 and int(ap.ap[0][0]) > 0 else rowlen
    off = int(ap.offset)
    p0 = off // rowlen
    f0 = off % rowlen
    pe = 0
    fe = 0
    for (st, cnt) in ap.ap:
        st = int(st); cnt = int(cnt)
        if cnt <= 1 or st == 0:
            continue
        if st % rowlen == 0:
            pe += (st // rowlen) * (cnt - 1)
        else:
            fe += abs(st) * (cnt - 1)
    sz = mybir.dt.size(ap.dtype)
    if str(ap.space) == 'PSUM':
        b0 = (f0 * sz) // 2048 * 2048
        b1 = ((f0 + fe + 1) * sz + 2047) // 2048 * 2048
        q0 = p0 // 32 * 32
        q1 = (p0 + pe + 1 + 31) // 32 * 32
        return (t.name, q0, q1, b0, b1)
    return (t.name, p0, p0 + pe + 1, f0 * sz, (f0 + fe + 1) * sz)


def overlap(a, b):
    return a[1] < b[2] and b[1] < a[2] and a[3] < b[4] and b[3] < a[4]


def covers(a, b):
    return a[1] <= b[1] and a[2] >= b[2] and a[3] <= b[3] and a[4] >= b[4]


def isap(v):
    return v is not None and not isinstance(v, (int, float))


class Prog:
    def __init__(self, nc):
        self.nc = nc
        self.ops = []
        self.track = {}
        self.dma_n = {}

    def op(self, eng, fn, reads=(), writes=(), dma=False):
        idx = len(self.ops)
        rb = [box_of(a) for a in reads]
        wb = [box_of(a) for a in writes]
        wb = wb + [b for b in rb if b[0] == 'psum']
        rb = [b for b in rb if b[0] != 'psum']
        deps = set()
        for b in rb:
            for e in self.track.setdefault(b[0], []):
                if e[3] and overlap(e[0], b):
                    deps.add(e[1])
        for b in wb:
            for e in self.track.setdefault(b[0], []):
                if overlap(e[0], b):
                    deps.add(e[1])
        for b in wb:
            lst = self.track[b[0]]
            lst[:] = [e for e in lst if not covers(b, e[0])]
            lst.append((b, idx, eng, True, dma))
        for b in rb:
            lst = self.track[b[0]]
            lst[:] = [e for e in lst if not (e[2] == eng and (not e[3]) and (not e[4]) and (not dma) and covers(b, e[0]))]
            lst.append((b, idx, eng, False, dma))
        deps.discard(idx)
        o = dict(eng=eng, fn=fn, deps=deps, dma=dma, signal=dma)
        if dma:
            n = self.dma_n.get(eng, 0)
            self.dma_n[eng] = n + 1
            o['slot'] = n % NSLOT
            o['val'] = 16 * (n // NSLOT + 1)
            o['dma_idx'] = n
        self.ops.append(o)
        for d in deps:
            p = self.ops[d]
            if p['eng'] == eng and not p['dma'] and not dma and eng == 'pe':
                continue
            p['signal'] = True
        return idx

    def emit(self):
        nc = self.nc
        engs = ['pe', 'act', 'dve', 'pool', 'sp']
        with contextlib.ExitStack() as st:
            csem = {e: st.enter_context(nc.semaphore('c_' + e)) for e in engs}
            dsem = {}
            for e in self.dma_n:
                dsem[e] = [st.enter_context(nc.semaphore('d_%s_%d' % (e, i))) for i in range(NSLOT)]
            cnt = {e: 0 for e in engs}
            for o in self.ops:
                if o['dma']:
                    o['sem'] = dsem[o['eng']][o['slot']]
                elif o['signal']:
                    cnt[o['eng']] += 1
                    o['sem'] = csem[o['eng']]
                    o['val'] = cnt[o['eng']]
            per = {e: [] for e in engs}
            for i, o in enumerate(self.ops):
                per[o['eng']].append(i)
            self.max_cnt = dict(cnt)
            block = st.enter_context(nc.Block())
            ops = self.ops

            def run(ename, engine):
                seen = {}
                for i in per[ename]:
                    o = ops[i]
                    waits = {}
                    for d in o['deps']:
                        p = ops[d]
                        if p['eng'] == ename and not p['dma'] and not o['dma'] and ename == 'pe':
                            continue
                        key = id(p['sem'])
                        if key not in waits or waits[key][1] < p['val']:
                            waits[key] = (p['sem'], p['val'])
                    if o['dma'] and o['dma_idx'] >= NSLOT:
                        s = o['sem']
                        v = o['val'] - 16
                        key = id(s)
                        if key not in waits or waits[key][1] < v:
                            waits[key] = (s, v)
                    for key, (s, v) in waits.items():
                        if seen.get(key, 0) >= v:
                            continue
                        engine.wait_ge(s, v)
                        seen[key] = v
                    ins = o['fn'](engine)
                    if o['signal']:
                        ins.then_inc(o['sem'], 16 if o['dma'] else 1)
                if ename in dsem:
                    n = self.dma_n[ename]
                    for sl in range(NSLOT):
                        k = (n - sl + NSLOT - 1) // NSLOT
                        if k > 0:
                            engine.wait_ge(dsem[ename][sl], 16 * k)

            block.tensor(lambda e: run('pe', e))
            block.scalar(lambda e: run('act', e))
            block.vector(lambda e: run('dve', e))
            block.gpsimd(lambda e: run('pool', e))
            block.sync(lambda e: run('sp', e))

    def dma(self, q, out, in_, **kw):
        return self.op(q, lambda e: e.dma_start(out=out, in_=in_, **kw), [in_], [out], dma=True)

    def mm(self, out, lhsT, rhs, start=True, stop=True):
        return self.op('pe', lambda e: e.matmul(out=out, lhsT=lhsT, rhs=rhs, start=start, stop=stop, skip_group_check=True), [lhsT, rhs], [out])

    def tr(self, out, in_, ident):
        return self.op('pe', lambda e: e.transpose(out=out, in_=in_, identity=ident), [in_, ident], [out])

    def act(self, out, in_, func, bias=None, scale=None, accum=None):
        kw = {}
        rd = [in_]
        wr = [out]
        if bias is not None:
            kw['bias'] = bias
            if isap(bias):
                rd.append(bias)
        if scale is not None:
            kw['scale'] = scale
            if isap(scale):
                rd.append(scale)
        if accum is not None:
            kw['accum_out'] = accum
            wr.append(accum)
        return self.op('act', lambda e: e.activation(out=out, in_=in_, func=func, **kw), rd, wr)

    def ts(self, out, in0, s1, s2=None, op0=ALU.mult, op1=None, eng='dve', accum=None):
        rd = [in0] + [s for s in (s1, s2) if isap(s)]
        wr = [out] + ([accum] if accum is not None else [])
        kw = {}
        if op1 is not None:
            kw['op1'] = op1
        if accum is not None:
            kw['accum_out'] = accum
        return self.op(eng, lambda e: e.tensor_scalar(out=out, in0=in0, scalar1=s1, scalar2=s2, op0=op0, **kw), rd, wr)

    def tt(self, out, in0, in1, op, eng='dve'):
        return self.op(eng, lambda e: e.tensor_tensor(out=out, in0=in0, in1=in1, op=op), [in0, in1], [out])

    def stt(self, out, in0, scalar, in1, op0, op1):
        rd = [in0, in1] + ([scalar] if isap(scalar) else [])
        return self.op('dve', lambda e: e.scalar_tensor_tensor(out=out, in0=in0, scalar=scalar, in1=in1, op0=op0, op1=op1), rd, [out])

    def copy(self, out, in_, eng='dve'):
        if eng == 'act':
            return self.op('act', lambda e: e.copy(out=out, in_=in_), [in_], [out])
        return self.op(eng, lambda e: e.tensor_copy(out=out, in_=in_), [in_], [out])

    def memset(self, ap, val, eng='pool'):
        return self.op(eng, lambda e: e.memset(ap, val), [], [ap])

    def reduce(self, out, in_, op=ALU.add, axis=AX.X):
        return self.op('dve', lambda e: e.tensor_reduce(out=out, in_=in_, axis=axis, op=op), [in_], [out])

    def recip(self, out, in_):
        return self.op('dve', lambda e: e.reciprocal(out=out, in_=in_), [in_], [out])

    def asel(self, out, in_, pattern, base, cm, fill, cmp=ALU.is_ge):
        return self.op('pool', lambda e: e.affine_select(out=out, in_=in_, pattern=pattern, compare_op=cmp, fill=fill, base=base, channel_multiplier=cm), [in_], [out])


class Arena:
    def __init__(self, t, nwords):
        self.t = t
        self.n = nwords
        self.off = 0
        self.peak = 0

    def mark(self):
        return self.off

    def reset(self, m):
        self.off = m

    def alloc(self, free_shape, dtype, parts=128):
        n = 1
        for s in free_shape:
            n *= s
        sz = mybir.dt.size(dtype)
        words = (n * sz + 3) // 4
        words = (words + 7) // 8 * 8
        assert self.off + words <= self.n, "arena overflow %d+%d>%d" % (self.off, words, self.n)
        ap = self.t[0:parts, self.off:self.off + words]
        self.off += words
        self.peak = max(self.peak, self.off)
        if dtype != F32:
            ap = ap.bitcast(dtype)
        ap = ap[:, 0:n]
        if len(free_shape) == 2:
            ap = ap.rearrange("p (a b) -> p a b", a=free_shape[0])
        elif len(free_shape) == 3:
            ap = ap.rearrange("p (a b c) -> p a b c", a=free_shape[0], b=free_shape[1])
        return ap


def rstd_from_ss(P, out, ss, n):
    P.act(out, ss, AF.Sqrt, bias=EPS, scale=1.0 / n)
    P.recip(out, out)


class Ctx:
    pass


def build(S=4096, dbg=None, phases=None):
    NT = S // 128
    NQB = S // 512
    NSEL = S // 64
    NCMP = S // 16 - 1
    NBT = (NCMP + 127) // 128
    nc = bass.Bass("TRN2", target_bir_lowering=False)

    def din(name, shape):
        return nc.dram_tensor(name, shape, F32, kind="ExternalInput").ap()

    I = {}
    I['x'] = din("x", [S, D])
    I['p'] = din("p", [2, S, 256])
    for name, shape in [("norm_attn", [2, D]), ("w_in", [2, D, INC]), ("w_out", [2, D, D]),
                        ("nsa_cmp_pos", [2, 2, 32, 64]), ("nsa_cmp_w1", [2, 2, 2048, 256]), ("nsa_cmp_w2", [2, 2, 256, 64]),
                        ("nsa_qk_gain", [2, 4, 64]), ("diff_qk_gain", [2, 2, 32]), ("diff_lambda", [2, 4, 32]),
                        ("diff_norm", [2, 64]), ("gla_w_gate2", [2, 16, 128]), ("gla_b_gate", [2, 128]), ("gla_norm", [2, 64]),
                        ("hgrn_lb_logits", [2, 256]), ("hgrn_norm", [2, 64]), ("norm_ffn", [2, D]),
                        ("ffn_w_gate", [1, D, DFF]), ("ffn_w_up", [1, D, DFF]), ("ffn_w_down", [1, DFF, D]),
                        ("moe_router", [1, D, 8]), ("moe_w_gate", [1, 8, D, DFF]), ("moe_w_up", [1, 8, D, DFF]),
                        ("moe_w_down", [1, 8, DFF, D]), ("ple_norm", [2, D]), ("ple_w_gate", [2, D, D]), ("ple_w_proj", [2, 256, D])]:
        I[name] = din(name, shape)
    Y = nc.dram_tensor("y", [S, D], F32, kind="ExternalOutput").ap()

    def scratch(name, shape, dt):
        kind = "ExternalOutput" if (dbg and name in dbg) else "Internal"
        return nc.dram_tensor(name, shape, dt, kind=kind).ap()

    U = scratch("U", [S, INC], F32)
    MIX = scratch("MIX", [S, D], BF16)
    H = scratch("H", [S, D], F32)
    CTD = scratch("CTD", [128, 8, S], BF16)
    COMB = scratch("COMB", [8, S], F32)

    with contextlib.ExitStack() as st:
        ARW = 49 * 1024
        arena_t = st.enter_context(nc.sbuf_tensor("arena", [128, ARW], F32))
        psum = st.enter_context(nc.psum_tensor("psum", [128, 8, 512], F32))
        P = Prog(nc)
        A = Arena(arena_t, ARW)
        C = Ctx()
        C.nc, C.P, C.A, C.psum, C.I = nc, P, A, psum, I
        C.S, C.NT, C.NQB, C.NSEL, C.NCMP, C.NBT = S, NT, NQB, NSEL, NCMP, NBT
        C.U, C.MIX, C.H, C.Y = U, MIX, H, Y
        C.CTD, C.COMB = CTD, COMB
        C.bank = lambda b: psum[:, b, :]
        C.bankbf = lambda b: psum[:, b, :].bitcast(BF16)

        C.identf = A.alloc([128], F32)
        C.ident = A.alloc([128], BF16)
        P.memset(C.identf, 1.0)
        P.asel(C.identf, C.identf, [[1, 128]], 0, -1, 0.0, cmp=ALU.is_equal)
        P.copy(C.ident, C.identf, 'dve')
        C.ones_row = A.alloc([128], F32, parts=1)
        P.memset(C.ones_row, 1.0)
        C.Cm = A.alloc([4, 512], BF16)
        C.Cw = A.alloc([4, 512], BF16)
        tmpf = A.alloc([512], F32)
        for r in range(4):
            P.memset(tmpf, 0.0)
            P.asel(tmpf, tmpf, [[1, 512]], -128 * r, -1, NEGM)
            P.copy(C.Cm[:, r, :], tmpf, 'dve')
            P.memset(tmpf, 0.0)
            P.asel(tmpf, tmpf, [[-1, 512]], 128 * r - 1, 1, NEGM)
            P.copy(C.Cw[:, r, :], tmpf, 'dve')

        ph = phases or ['inproj', 'nsa', 'diff', 'gla', 'hgrn', 'out']
        for L in range(2):
            src = I['x'] if L == 0 else H
            dst = H if L == 0 else Y
            if ('inproj', L) in ph or 'inproj' in ph:
                phase_inproj(C, L, src)
            if 'nsa' in ph:
                phase_nsa(C, L)
            if 'diff' in ph:
                phase_diff(C, L)
            if 'gla' in ph:
                phase_gla(C, L, hgrn=False)
            if 'hgrn' in ph:
                phase_gla(C, L, hgrn=True)
            if 'out' in ph:
                phase_out(C, L, src, dst)
            if dbg and dbg.get('stop_after_layer') == L:
                break
        P.emit()
        C.peak = A.peak
    nc._ctx = C
    return nc


def load_cast(P, out, in_, q='pool'):
    n = out.shape[-1]
    if n <= 2048:
        P.dma(q, out, in_)
    else:
        k = (n + 2047) // 2048
        step = (n + k - 1) // k
        for c0 in range(0, n, step):
            c1 = min(n, c0 + step)
            P.dma(q, out[..., c0:c1], in_[..., c0:c1])


def norm_transpose(C, xt, gT, hn, aT, tb, ss, rs, junk):
    P = C.P
    P.act(junk, xt, AF.Square, accum=ss)
    rstd_from_ss(P, rs, ss, D)
    P.ts(hn, xt, rs[:, 0:1], None, ALU.mult)
    pT = C.bankbf(tb).rearrange("p (a b) -> p a b", a=8)
    for c in range(8):
        P.tr(pT[:, c, :], hn[:, c * 128:(c + 1) * 128], C.ident)
    P.tt(aT, pT, gT.unsqueeze(2).to_broadcast([128, 8, 128]), ALU.mult)


def phase_inproj(C, L, src):
    P, A, I = C.P, C.A, C.I
    m = A.mark()
    win = A.alloc([8, INC], BF16)
    for kc in range(8):
        load_cast(P, win[:, kc, :], I['w_in'][L, kc * 128:(kc + 1) * 128, :])
    gT = A.alloc([8], F32)
    P.dma('sp', gT, I['norm_attn'][L].rearrange("(c p) -> p c", p=128), allow_slow_non_contiguous=True)
    xts = [A.alloc([D], F32) for _ in range(2)]
    junk = A.alloc([D], BF16)
    hn = A.alloc([D], BF16)
    aTs = [A.alloc([8, 128], BF16) for _ in range(2)]
    us = [A.alloc([INC], F32) for _ in range(2)]
    ss = A.alloc([1], F32)
    rs = A.alloc([1], F32)
    chunks = [(c0, min(INC, c0 + 512)) for c0 in range(0, INC, 512)]
    P.dma('sp', xts[0], src[0:128, :])
    bk = 0
    for t in range(C.NT):
        xt = xts[t % 2]
        if t + 1 < C.NT:
            P.dma('sp', xts[(t + 1) % 2], src[(t + 1) * 128:(t + 2) * 128, :])
        aT = aTs[t % 2]
        norm_transpose(C, xt, gT, hn, aT, t % 2, ss, rs, junk)
        u = us[t % 2]
        for ci, (c0, c1) in enumerate(chunks):
            b = 2 + (bk % 6)
            bk += 1
            pb = C.bank(b)[:, 0:c1 - c0]
            for kc in range(8):
                P.mm(pb, aT[:, kc, :], win[:, kc, c0:c1], start=(kc == 0), stop=(kc == 7))
            if ci % 2 == 0:
                P.copy(u[:, c0:c1], pb, 'act')
            else:
                P.copy(u[:, c0:c1], pb, 'dve')
        P.dma('sp', C.U[t * 128:(t + 1) * 128, :], u)
    A.reset(m)


def run_pipelined(iters, la=2):
    n = len(iters)
    for i in range(n + la):
        if i < n:
            iters[i][0]()
        j = i - la
        if j >= 0:
            iters[j][1]()
            if iters[j][2] is not None:
                iters[j][2]()


def bcast_load(P, out, vec, q='sp'):
    P.dma(q, out, vec.partition_broadcast(128))


def phase_nsa(C, L):
    P, A, I = C.P, C.A, C.I
    S, NT, NQB, NSEL, NCMP, NBT = C.S, C.NT, C.NQB, C.NSEL, C.NCMP, C.NBT
    m0 = A.mark()
    QT = A.alloc([2, S], BF16)
    KsT = A.alloc([S], BF16)
    KwT = A.alloc([S], BF16)
    KVcT = A.alloc([S], BF16)
    Vs = A.alloc([NT, 66], BF16)
    Vw = A.alloc([NT, 66], BF16)
    G = A.alloc([NT, 12], F32)
    MIXA = A.alloc([NT, 256], F32)
    SelT = A.alloc([S], BF16)
    Rall = A.alloc([S], BF16)
    gcol = A.alloc([4], F32)
    gsrc = I['nsa_qk_gain'][L]
    for hh in range(2):
        P.dma('sp', gcol[hh * 64:(hh + 1) * 64, :], gsrc.rearrange("j d -> d j"), allow_slow_non_contiguous=True)
    P.ts(gcol[:, 0:1], gcol[:, 0:1], 0.125, None, ALU.mult)
    P.memset(Vs[:, :, 64:65], 1.0)
    P.memset(Vw[:, :, 64:65], 1.0)
    m1 = A.mark()
    rf = A.alloc([S], F32, parts=64)
    P.memset(rf, 1.0)
    P.asel(rf, rf, [[1, S]], 0, -64, 0.0)
    P.asel(rf, rf, [[-1, S]], 63, 64, 0.0)
    P.copy(Rall[0:64], rf, 'dve')
    P.dma('sp', Rall[64:128, :], Rall[0:64, :])
    A.reset(m1)

    if os.environ.get('NSA_STOP') == '0':
        A.reset(m0)
        return
    m1 = A.mark()
    uts = [A.alloc([652], F32) for _ in range(2)]
    sq = A.alloc([640], F32)
    ssq = A.alloc([10], F32)
    rsq = A.alloc([10], F32)
    nb16 = A.alloc([5, 128], BF16)
    P.dma('sp', uts[0], C.U[0:128, 0:652])
    for t in range(NT):
        ut = uts[t % 2]
        if t + 1 < NT:
            P.dma('sp', uts[(t + 1) % 2], C.U[(t + 1) * 128:(t + 2) * 128, 0:652])
        SK = os.environ.get('NSA_SKIP', '').split(',')
        P.act(sq, ut[:, 0:640], AF.Square)
        P.reduce(ssq, sq.rearrange("p (a b) -> p a b", a=10))
        rstd_from_ss(P, rsq, ssq, 64)
        if 'nb' not in SK:
            P.tt(nb16[:, 0:2, :].rearrange("p a (h d) -> p (a h) d", h=2), ut[:, 0:256].rearrange("p (h d) -> p h d", h=4),
                 rsq[:, 0:4].unsqueeze(2).to_broadcast([128, 4, 64]), ALU.mult)
            for hh in range(2):
                P.ts(nb16[:, 2, hh * 64:(hh + 1) * 64], ut[:, 384:448], rsq[:, 6:7], None, ALU.mult)
                P.ts(nb16[:, 3, hh * 64:(hh + 1) * 64], ut[:, 512:576], rsq[:, 8:9], None, ALU.mult)
        if 'pc' not in SK:
            P.copy(nb16[:, 4, :], ut[:, 256:384], 'pool')
        ts_ = slice(t * 128, (t + 1) * 128)
        if 'tr' not in SK:
            pT = C.bankbf(t % 2).rearrange("p (a b) -> p a b", a=8)
            for c in range(5):
                P.tr(pT[:, c, :], nb16[:, c, :], C.ident)
            if 'ev1' not in SK:
                P.act(QT[:, :, ts_], pT[:, 0:2, :], AF.Identity, scale=gcol[:, 0:1])
            if 'ev2' not in SK:
                P.ts(KsT[:, ts_], pT[:, 2, :], gcol[:, 2:3], None, ALU.mult)
                P.ts(KwT[:, ts_], pT[:, 3, :], gcol[:, 3:4], None, ALU.mult)
            if 'ev3' not in SK:
                P.copy(KVcT[:, ts_], pT[:, 4, :], 'act')
        if 'v' not in SK:
            P.copy(Vs[:, t, 0:64], ut[:, 448:512], 'pool')
            P.copy(Vw[:, t, 0:64], ut[:, 576:640], 'pool')
        if 'g' not in SK:
            P.copy(G[:, t, :], ut[:, 640:652], 'pool')
    Gf = G.rearrange("p a b -> p (a b)")
    P.act(Gf, Gf, AF.Sigmoid)
    if not os.environ.get('NSA_NORESET'):
        A.reset(m1)

    if os.environ.get('NSA_STOP') == 'a':
        A.reset(m0)
        return
    m1 = A.mark()
    w1 = A.alloc([32, 256], BF16)
    SKB = os.environ.get('NSA_SKIPB', '').split(',')
    for j in range(2):
        w1v = I['nsa_cmp_w1'][L, j].rearrange("(l d) h -> d l h", d=64)
        for l0 in range(0, 32, 8):
            if 'w1' not in SKB:
                P.dma('pool', w1[j * 64:(j + 1) * 64, l0:l0 + 8, :], w1v[:, l0:l0 + 8, :])
    w2 = A.alloc([2, 2, 64], BF16)
    for j in range(2):
        if 'w2' not in SKB:
            P.dma('pool', w2[:, j, :, :], I['nsa_cmp_w2'][L, j].rearrange("(t p) d -> p t d", p=128))
    posl = A.alloc([128], F32, parts=32)
    posT = A.alloc([32], BF16)
    if 'pos' not in SKB:
        for j in range(2):
            P.dma('sp', posl[:, j * 64:(j + 1) * 64], I['nsa_cmp_pos'][L, j])
        ppos = C.bank(2)[:, 0:32]
        P.tr(ppos, posl, C.identf[0:32, 0:32])
        P.copy(posT, ppos, 'dve')
    if os.environ.get('NSA_STOP') == 'b1':
        A.reset(m0)
        return
    hidT = A.alloc([2, 2, 256], BF16)
    biasc = A.alloc([4], F32)
    rhs_c = A.alloc([NBT, 129], F32)
    kcT = A.alloc([NBT * 128], BF16)
    kcn = A.alloc([128], BF16)
    kss = A.alloc([1], F32)
    krs = A.alloc([1], F32)
    kjunk = A.alloc([64], F32)
    for kv in range(2):
        rows = slice(kv * 64, (kv + 1) * 64)
        for ht in range(2):
            pb = C.bank(2 + ht)[:, 0:NCMP]
            for l in range(32):
                P.mm(pb, w1[rows, l, ht * 128:(ht + 1) * 128], KVcT[rows, l:l + 16 * (NCMP - 1) + 1:16], start=(l == 0), stop=(l == 31))
            pbb = C.bank(4 + ht)[:, 0:1]
            for l in range(32):
                P.mm(pbb, w1[rows, l, ht * 128:(ht + 1) * 128], posT[rows, l:l + 1], start=(l == 0), stop=(l == 31))
            bc = biasc[:, kv * 2 + ht:kv * 2 + ht + 1]
            P.copy(bc, pbb, 'dve')
            P.act(hidT[:, kv, ht, 0:NCMP], pb, AF.Silu, bias=bc)
    if os.environ.get('NSA_STOP') == 'b2':
        A.reset(m0)
        return
    for bt in range(NBT):
        nb = min(128, NCMP - bt * 128)
        bs = slice(bt * 128, bt * 128 + nb)
        pk = C.bank(2)[0:nb, 0:64]
        for ht in range(2):
            P.mm(pk, hidT[:, 0, ht, bs], w2[:, 0, ht, :], start=(ht == 0), stop=(ht == 1))
        P.act(kjunk[0:nb], pk, AF.Square, accum=kss[0:nb])
        rstd_from_ss(P, krs[0:nb], kss[0:nb], 64)
        for hh in range(2):
            P.ts(kcn[0:nb, hh * 64:(hh + 1) * 64], pk, krs[0:nb, 0:1], None, ALU.mult)
        pkt = C.bankbf(3)[:, 0:nb]
        P.tr(pkt, kcn[0:nb, :], C.ident[0:nb, 0:nb])
        P.ts(kcT[:, bs], pkt, gcol[:, 1:2], None, ALU.mult)
        pv = C.bank(4)[0:nb, 0:64]
        for ht in range(2):
            P.mm(pv, hidT[:, 1, ht, bs], w2[:, 1, ht, :], start=(ht == 0), stop=(ht == 1))
        P.copy(rhs_c[0:nb, bt, 0:64], pv, 'act')
        ov = rhs_c[:, bt, 64:64 + NSEL]
        P.memset(rhs_c[:, bt, 64:129], 1.0)
        P.asel(ov, ov, [[64, NSEL]], 63 - 16 * 128 * bt, -16, 0.0)
        P.asel(ov, ov, [[-64, NSEL]], 31 + 16 * 128 * bt, 16, 0.0)

    if os.environ.get('NSA_STOP') == 'b':
        A.reset(m0)
        return
    PcT = A.alloc([4, NBT, 512], F32)
    BSb = A.alloc([128], F32)
    BIGV = 1.0e4
    P.memset(BSb, 0.0)
    P.memset(BSb[0:64, 63:65], BIGV)
    P.memset(BSb[0:64, 65:128], -BIGV)
    P.memset(BSb[64:128, 64:66], BIGV)
    P.memset(BSb[64:128, 66:128], -BIGV)
    imp = A.alloc([64], F32)
    imp2 = A.alloc([64], F32)
    m8a = A.alloc([8], F32)
    m8b = A.alloc([8], F32)
    seln = A.alloc([128], BF16)
    P.memset(seln, 0.0)
    den = A.alloc([4], F32)
    cmb = A.alloc([4], F32)
    for tb in range(NQB):
        T0 = tb * 512
        live = []
        for h in range(4):
            hr = slice((h % 2) * 64, (h % 2) * 64 + 64)
            for bt in range(NBT):
                nb = min(128, NCMP - bt * 128)
                if 16 * (128 * bt) + 31 > T0 + 511:
                    continue
                live.append((h, bt))
                pb = C.bank(2 + (h * NBT + bt) % 4)[0:nb, :]
                P.mm(pb, kcT[hr, bt * 128:bt * 128 + nb], QT[hr, h // 2, T0:T0 + 512])
                P.act(PcT[0:nb, h, bt, :], pb, AF.Exp)
                P.asel(PcT[0:nb, h, bt, :], PcT[0:nb, h, bt, :], [[1, 512]], T0 - 2048 * bt - 31, -16, 0.0)
        for qs in range(4):
            t = tb * 4 + qs
            pos = [C.bank(6)[:, 0:258].rearrange("p (a b) -> p a b", a=2), C.bank(7)[:, 0:258].rearrange("p (a b) -> p a b", a=2)]
            for h in range(4):
                bts = [bt for (hh, bt) in live if hh == h]
                o = pos[h // 2][:, h % 2, :]
                if not bts:
                    continue
                for i, bt in enumerate(bts):
                    nb = min(128, NCMP - bt * 128)
                    P.mm(o, PcT[0:nb, h, bt, qs * 128:(qs + 1) * 128], rhs_c[0:nb, bt, :], start=(h % 2 == 0 and i == 0), stop=(i == len(bts) - 1))
            if not live:
                P.memset(MIXA[:, t, :], 0.0, 'dve')
                P.memset(imp[:, 0:NSEL], 0.0, 'dve')
            else:
                for h in range(4):
                    o = pos[h // 2][:, h % 2, :]
                    P.ts(den[:, h:h + 1], o[:, 128:129], 1e-30, None, ALU.max)
                    P.recip(den[:, h:h + 1], den[:, h:h + 1])
                    if h == 0:
                        P.ts(imp[:, 0:NSEL], o[:, 64:64 + NSEL], den[:, 0:1], None, ALU.mult)
                    else:
                        P.stt(imp[:, 0:NSEL], o[:, 64:64 + NSEL], den[:, h:h + 1], imp[:, 0:NSEL], ALU.mult, ALU.add)
                    P.tt(cmb[:, h:h + 1], den[:, h:h + 1], G[:, t, 3 * h:3 * h + 1], ALU.mult)
                    P.ts(MIXA[:, t, h * 64:(h + 1) * 64], o[:, 0:64], cmb[:, h:h + 1], None, ALU.mult)
            if NSEL > 16:
                P.tt(imp[:, 0:NSEL], imp[:, 0:NSEL], BSb[:, 64 - 2 * t:64 - 2 * t + NSEL], ALU.add)
                P.memset(imp[:, 0:1], BIGV, 'dve')
                P.op('dve', lambda e: e.max(out=m8a, in_=imp[:, 0:NSEL]), [imp[:, 0:NSEL]], [m8a])
                P.op('dve', lambda e: e.match_replace(out=imp2[:, 0:NSEL], in_to_replace=m8a, in_values=imp[:, 0:NSEL], imm_value=-1e9), [imp[:, 0:NSEL], m8a], [imp2[:, 0:NSEL]])
                P.op('dve', lambda e: e.max(out=m8b, in_=imp2[:, 0:NSEL]), [imp2[:, 0:NSEL]], [m8b])
                P.ts(seln[:, 0:NSEL], imp[:, 0:NSEL], m8b[:, 7:8], NEGM, ALU.is_lt, ALU.mult)
                P.ts(seln[:, 64:64 + NSEL], imp[:, 0:NSEL], m8b[:, 7:8], NEGM, ALU.is_lt, ALU.mult)
            pst = C.bankbf(t % 2)[:, 0:128]
            P.tr(pst, seln, C.ident)
            P.copy(SelT[:, t * 128:(t + 1) * 128], pst, 'act')
    A.reset(m1)

    if os.environ.get('NSA_STOP') == 'c':
        A.reset(m0)
        return
    m1 = A.mark()
    PTs = [A.alloc([512], BF16) for _ in range(3)]
    rd2 = A.alloc([8], F32)
    pi = 0
    oi = 0
    iters = []
    for branch in range(2):
        KT = KsT if branch == 0 else KwT
        V = Vs if branch == 0 else Vw
        for h in range(4):
            hr = slice((h % 2) * 64, (h % 2) * 64 + 64)
            for qb in range(NQB):
                kts = list(range(0, 4 * qb + 4)) if branch == 0 else list(range(max(0, 4 * qb - 4), 4 * qb + 4))
                ob = C.bank(6 + oi % 2)[:, 0:260].rearrange("p (a b) -> p a b", a=4)
                oi += 1
                first = True
                lastkt = {qs: 4 * qb + qs for qs in range(4)}
                for kt in kts:
                    pb = C.bank(2 + pi % 4)
                    PT = PTs[pi % 3]
                    pi += 1
                    ks = slice(kt * 128, (kt + 1) * 128)
                    diag = kt >= 4 * qb
                    pvs = []
                    for qs in range(4):
                        if diag and qs < kt - 4 * qb:
                            continue
                        if (not diag) and branch == 1 and qs > kt - (4 * qb - 4):
                            continue
                        pvs.append((qs, first, kt == lastkt[qs]))
                        first = False

                    def qk(pb=pb, PT=PT, KT=KT, hr=hr, ks=ks, h=h, qb=qb, diag=diag, kt=kt, branch=branch):
                        if diag:
                            c0, c1 = (kt - 4 * qb) * 128, 512
                        elif branch == 1:
                            c0, c1 = 0, (kt - (4 * qb - 4) + 1) * 128
                        else:
                            c0, c1 = 0, 512
                        q0 = qb * 512
                        pbs = pb[:, c0:c1]
                        P.mm(pbs, KT[hr, ks], QT[hr, h // 2, q0 + c0:q0 + c1], start=True, stop=False)
                        if branch == 0:
                            r0 = (h % 2) * 64
                            P.mm(pbs, Rall[r0:r0 + NSEL, ks], SelT[r0:r0 + NSEL, q0 + c0:q0 + c1], start=False, stop=not diag)
                            if diag:
                                P.mm(pbs, C.ident, C.Cm[:, kt - 4 * qb, c0:c1], start=False, stop=True)
                        else:
                            mk = C.Cm[:, kt - 4 * qb, c0:c1] if diag else C.Cw[:, kt - (4 * qb - 4), c0:c1]
                            P.mm(pbs, C.ident, mk, start=False, stop=True)
                        P.act(PT[:, c0:c1], pbs, AF.Exp)

                    def pv(PT=PT, ob=ob, pvs=pvs, V=V, kt=kt):
                        for (qs, st_, sp_) in pvs:
                            P.mm(ob[:, qs, :], PT[:, qs * 128:(qs + 1) * 128], V[:, kt, 0:65], start=st_, stop=sp_)
                    iters.append([qk, pv, None])

                def fin(ob=ob, qb=qb, h=h, branch=branch):
                    for qs in range(4):
                        t = qb * 4 + qs
                        P.recip(rd2[:, qs:qs + 1], ob[:, qs, 64:65])
                        P.tt(rd2[:, 4 + qs:5 + qs], rd2[:, qs:qs + 1], G[:, t, 3 * h + 1 + branch:3 * h + 2 + branch], ALU.mult)
                        P.stt(MIXA[:, t, h * 64:(h + 1) * 64], ob[:, qs, 0:64], rd2[:, 4 + qs:5 + qs], MIXA[:, t, h * 64:(h + 1) * 64], ALU.mult, ALU.add)
                iters[-1][2] = fin
    run_pipelined(iters)
    mb = A.alloc([NT, 256], BF16)
    P.copy(mb, MIXA, 'dve')
    P.dma('sp', C.MIX[:, 0:256].rearrange("(t p) c -> p t c", p=128), mb)
    A.reset(m1)
    A.reset(m0)


def bcast_row(C, out_sb, row_dram, n, scale=None, q='sp'):
    P, A = C.P, C.A
    m = A.mark()
    tmp = A.alloc([n], F32, parts=1)
    P.dma(q, tmp, row_dram.rearrange("(o n) -> o n", o=1))
    for c0 in range(0, n, 512):
        c1 = min(n, c0 + 512)
        pb = C.bank(2 + (c0 // 512) % 2)[:, 0:c1 - c0]
        P.mm(pb, C.ones_row, tmp[0:1, c0:c1])
        if scale is None:
            P.copy(out_sb[:, c0:c1], pb, 'dve')
        else:
            P.ts(out_sb[:, c0:c1], pb, scale, None, ALU.mult)
    A.reset(m)


def phase_diff(C, L):
    P, A, I = C.P, C.A, C.I
    S, NT, NQB = C.S, C.NT, C.NQB
    m0 = A.mark()
    lam_init = 0.8 - 0.6 * math.exp(-0.3 * L)
    DQT = A.alloc([3, S], BF16)
    DKT = A.alloc([3, S], BF16)
    DV = A.alloc([NT, 4, 66], BF16)
    MIXB = A.alloc([NT, 256], BF16)
    gcol = A.alloc([2], F32)
    gsrc = I['diff_qk_gain'][L]
    for r in range(4):
        P.dma('sp', gcol[r * 32:(r + 1) * 32, :], gsrc.rearrange("j d -> d j"), allow_slow_non_contiguous=True)
    P.ts(gcol[:, 0:1], gcol[:, 0:1], 32 ** -0.5, None, ALU.mult)
    P.memset(DV[:, :, :, 64:66], 1.0)
    lam1 = A.alloc([128], F32, parts=1)
    P.dma('sp', lam1, I['diff_lambda'][L].rearrange("(o a) b -> o (a b)", o=1))
    lp = A.alloc([64], F32, parts=1)
    lsum = A.alloc([2], F32, parts=1)
    P.tt(lp[:, 0:32], lam1[:, 0:32], lam1[:, 32:64], ALU.mult)
    P.tt(lp[:, 32:64], lam1[:, 64:96], lam1[:, 96:128], ALU.mult)
    P.reduce(lsum, lp.rearrange("p (a b) -> p a b", a=2))
    P.act(lsum, lsum, AF.Exp)
    lf = A.alloc([1], F32, parts=1)
    P.tt(lf, lsum[:, 1:2], lsum[:, 0:1], ALU.subtract)
    P.ts(lf, lf, -lam_init, None, ALU.add)
    neglam = A.alloc([1], F32)
    pbl = C.bank(2)[:, 0:1]
    P.mm(pbl, C.ones_row, lf[0:1, 0:1])
    P.copy(neglam, pbl, 'dve')
    subg = A.alloc([64], F32)
    bcast_row(C, subg, I['diff_norm'][L], 64, scale=(1.0 - lam_init))

    m1 = A.mark()
    uts = [A.alloc([768], F32) for _ in range(2)]
    sq = A.alloc([512], F32)
    ssq = A.alloc([16], F32)
    rsq = A.alloc([16], F32)
    nb16 = A.alloc([4, 128], BF16)
    P.dma('sp', uts[0], C.U[0:128, 652:1420])
    for t in range(NT):
        ut = uts[t % 2]
        if t + 1 < NT:
            P.dma('sp', uts[(t + 1) % 2], C.U[(t + 1) * 128:(t + 2) * 128, 652:1420])
        P.act(sq, ut[:, 0:512], AF.Square)
        P.reduce(ssq, sq.rearrange("p (a b) -> p a b", a=16))
        rstd_from_ss(P, rsq, ssq, 32)
        P.tt(nb16.rearrange("p a (g d) -> p (a g) d", g=4), ut[:, 0:512].rearrange("p (g d) -> p g d", g=16),
             rsq.unsqueeze(2).to_broadcast([128, 16, 32]), ALU.mult)
        pT = C.bankbf(t % 2).rearrange("p (a b) -> p a b", a=8)
        nbf = nb16.rearrange("p a b -> p (a b)")
        for qk in range(2):
            for j in range(3):
                w = 96 if j < 2 else 64
                P.tr(pT[0:w, qk * 3 + j, :], nbf[:, qk * 256 + j * 96:qk * 256 + j * 96 + w], C.ident)
        ts_ = slice(t * 128, (t + 1) * 128)
        P.ts(DQT[0:96, :, ts_], pT[0:96, 0:3, :], gcol[0:96, 0:1], None, ALU.mult)
        P.ts(DKT[0:96, :, ts_], pT[0:96, 3:6, :], gcol[0:96, 1:2], None, ALU.mult)
        P.copy(DV[:, t, :, 0:64], ut[:, 512:768].rearrange("p (h d) -> p h d", h=4), 'act')
    A.reset(m1)

    PTs = [A.alloc([512], BF16) for _ in range(3)]
    rd = A.alloc([4], F32)
    otmp = A.alloc([64], F32)
    o2 = A.alloc([64], F32)
    junk = A.alloc([64], F32)
    ss1 = A.alloc([1], F32)
    rs1 = A.alloc([1], F32)
    pi = 0
    oi = 0
    iters = []
    for h in range(4):
        for qb in range(NQB):
            obs = [C.bank(4 + 2 * (oi % 2) + c)[:, 0:260].rearrange("p (a b) -> p a b", a=4) for c in range(2)]
            oi += 1
            first = [True, True]
            for kt in range(4 * qb + 4):
                ks = slice(kt * 128, (kt + 1) * 128)
                diag = kt >= 4 * qb
                for c in range(2):
                    g = h * 2 + c
                    hb = g // 3
                    r0 = (g % 3) * 32
                    rows = slice(r0, r0 + 32)
                    pb = C.bank(pi % 4)
                    PT = PTs[pi % 3]
                    pi += 1
                    pvs = []
                    for qs in range(4):
                        if diag and qs < kt - 4 * qb:
                            continue
                        pvs.append((qs, first[c], kt == 4 * qb + qs))
                        first[c] = False

                    def qk(pb=pb, PT=PT, rows=rows, hb=hb, ks=ks, qb=qb, diag=diag, kt=kt):
                        c0 = (kt - 4 * qb) * 128 if diag else 0
                        P.mm(pb[:, c0:512], DKT[rows, hb, ks], DQT[rows, hb, qb * 512 + c0:(qb + 1) * 512], start=True, stop=not diag)
                        if diag:
                            P.mm(pb[:, c0:512], C.ident, C.Cm[:, kt - 4 * qb, c0:512], start=False, stop=True)
                        P.act(PT[:, c0:512], pb[:, c0:512], AF.Exp)

                    def pv(PT=PT, ob=obs[c], pvs=pvs, kt=kt, h=h):
                        for (qs, st_, sp_) in pvs:
                            P.mm(ob[:, qs, :], PT[:, qs * 128:(qs + 1) * 128], DV[:, kt, h, 0:65], start=st_, stop=sp_)
                    iters.append([qk, pv, None])

            def fin(obs=obs, qb=qb, h=h):
                for qs in range(4):
                    t = qb * 4 + qs
                    P.recip(rd[:, 0:1], obs[0][:, qs, 64:65])
                    P.recip(rd[:, 1:2], obs[1][:, qs, 64:65])
                    P.tt(rd[:, 2:3], rd[:, 1:2], neglam, ALU.mult)
                    P.ts(otmp, obs[1][:, qs, 0:64], rd[:, 2:3], None, ALU.mult)
                    P.stt(o2, obs[0][:, qs, 0:64], rd[:, 0:1], otmp, ALU.mult, ALU.add)
                    P.act(junk, o2, AF.Square, accum=ss1)
                    rstd_from_ss(P, rs1, ss1, 64)
                    P.stt(MIXB[:, t, h * 64:(h + 1) * 64], o2, rs1[:, 0:1], subg, ALU.mult, ALU.mult)
            iters[-1][2] = fin
    run_pipelined(iters)
    P.dma('sp', C.MIX[:, 256:512].rearrange("(t p) c -> p t c", p=128), MIXB)
    A.reset(m0)


def phase_gla(C, L, hgrn):
    P, A, I = C.P, C.A, C.I
    S, NT = C.S, C.NT
    m0 = A.mark()
    if hgrn:
        c0, W, NCH, DK = 2204, 1024, 256, 64
        qo, ko, vo, ogo = 0, 256, 512, 768
        mixcol = 768
        qscale = 1.0
    else:
        c0, W, NCH, DK = 1420, 784, 128, 32
        qo, ko, vo, lro, ogo = 0, 128, 256, 512, 528
        mixcol = 512
        qscale = 32 ** -0.5
    L1 = A.alloc([128], F32)
    L2 = A.alloc([128], F32)
    L3 = A.alloc([128], F32)
    IND = A.alloc([2], F32)
    P.memset(L1, 1.0)
    P.asel(L1, L1, [[1, 128]], 0, -1, 0.0)
    P.memset(L1[0:64, 64:128], 0.0)
    P.memset(L3, 1.0)
    P.asel(L3, L3, [[-1, 128]], -1, 1, 0.0)
    P.memset(L3[64:128, 0:64], 0.0)
    P.memset(L2, 0.0)
    P.memset(L2[0:32, 0:64], 1.0)
    P.memset(L2[64:96, 64:128], 1.0)
    P.tt(L2, L1, L2, ALU.subtract, eng='pool')
    P.memset(IND, 0.0)
    P.memset(IND[0:64, 0:1], 1.0)
    P.memset(IND[64:128, 1:2], 1.0)
    gn = A.alloc([64], F32)
    bcast_row(C, gn, I['hgrn_norm' if hgrn else 'gla_norm'][L], 64)
    if hgrn:
        lb = A.alloc([256], F32)
        oml = A.alloc([256], F32)
        if L == 0:
            P.memset(lb, 0.0)
            P.memset(oml, 1.0)
        else:
            l0 = A.alloc([256], F32)
            bcast_row(C, lb, I['hgrn_lb_logits'][1], 256)
            bcast_row(C, l0, I['hgrn_lb_logits'][0], 256)
            P.tt(lb, lb, l0, ALU.subtract)
            P.act(lb, lb, AF.Sigmoid)
            P.ts(oml, lb, -1.0, 1.0, ALU.mult, ALU.add)
    else:
        w_aug = A.alloc([128], F32, parts=33)
        glrT = A.alloc([128], F32, parts=33)
        P.memset(w_aug, 0.0)
        P.memset(glrT, 0.0)
        P.memset(glrT[32:33, :], 1.0)
        P.dma('sp', w_aug[0:16, :], I['gla_w_gate2'][L])
        P.dma('sp', w_aug[32:33, :], I['gla_b_gate'][L].rearrange("(o n) -> o n", o=1))
    S32 = A.alloc([4, 64], F32)
    P.memset(S32, 0.0)
    ring = [A.alloc([4, 64], BF16) for _ in range(4)]
    ATs = [A.alloc([4, 128], BF16) for _ in range(2)]
    for a_ in ATs:
        P.memset(a_, 0.0)
    MIXC = A.alloc([NT, 256], BF16)
    Af32 = A.alloc([4, 128], F32)
    uts = [A.alloc([W], F32) for _ in range(2)]
    la = A.alloc([NCH], F32)
    kk = A.alloc([NCH], F32)
    e_ = A.alloc([NCH], F32)
    E = A.alloc([4, NCH], F32)
    qt16 = A.alloc([3, NCH], BF16)
    ks16 = A.alloc([NCH], BF16)
    v16 = A.alloc([256], BF16)
    T16 = A.alloc([12, 128], BF16)
    DEC = A.alloc([4, 2], F32)
    o_sbs = [A.alloc([256], F32) for _ in range(2)]
    sq = A.alloc([256], F32)
    ss4s = [A.alloc([4], F32) for _ in range(2)]
    rs4 = A.alloc([4], F32)
    sils = [A.alloc([256], F32) for _ in range(2)]

    def epi_ln(tp):
        P.act(rs4, ss4s[tp % 2], AF.Ln, bias=EPS, scale=1.0 / 64)

    def epi_fin(tp):
        P.act(rs4, rs4, AF.Exp, scale=-0.5)
        ob = o_sbs[tp % 2]
        P.tt(ob.rearrange("p (a b) -> p a b", a=4), ob.rearrange("p (a b) -> p a b", a=4), rs4.unsqueeze(2).to_broadcast([128, 4, 64]), ALU.mult)
        P.tt(MIXC[:, tp, :], ob, sils[tp % 2], ALU.mult)
    GS = os.environ.get('GLA_STOP')
    P.dma('sp', uts[0], C.U[0:128, c0:c0 + W])
    for t in range(NT):
        ut = uts[t % 2]
        if t + 1 < NT:
            P.dma('sp', uts[(t + 1) % 2], C.U[(t + 1) * 128:(t + 2) * 128, c0:c0 + W])
        Q = ut[:, qo:qo + NCH]
        if hgrn:
            P.act(e_, ut[:, ko:ko + NCH], AF.Exp, scale=-1.0)
            P.ts(e_, e_, 1.0, None, ALU.add)
            P.recip(e_, e_)
            P.tt(e_, e_, oml, ALU.mult)
            P.tt(e_, e_, lb, ALU.add)
            P.act(la, e_, AF.Ln)
            if t >= 1:
                epi_ln(t - 1)
            P.ts(kk, e_, -1.0, 1.0, ALU.mult, ALU.add)
            K = kk
        else:
            pg = C.bank(0)[0:16, 0:128]
            P.tr(pg, ut[:, lro:lro + 16], C.identf)
            P.copy(glrT[0:16, :], pg, 'dve')
            pz = C.bank(1)[:, 0:128]
            P.mm(pz, glrT[0:33, :], w_aug[0:33, :])
            P.act(e_, pz, AF.Exp, scale=-1.0)
            P.act(e_, e_, AF.Ln, bias=1.0)
            if t >= 1:
                epi_ln(t - 1)
            P.ts(la, e_, -1.0 / 16.0, None, ALU.mult)
            K = ut[:, ko:ko + NCH]
        if GS == 'la':
            continue
        pB = [C.bank(2 + i)[:, 0:NCH] for i in range(3)]
        P.mm(pB[0], L1, la)
        P.mm(pB[1], L2, la)
        P.mm(pB[2], L3, la)
        pdec = C.bank(0)[:, 256:264].rearrange("p (a b) -> p a b", a=4)
        for h in range(4):
            P.mm(pdec[0:DK, h, :], la[:, h * DK:(h + 1) * DK], IND, start=(h == 0), stop=True)
        if t >= 1:
            epi_fin(t - 1)
        P.act(E[:, 0, :], pB[0], AF.Exp)
        P.act(E[:, 1, :], pB[1], AF.Exp)
        P.act(E[:, 2, :], pB[1], AF.Exp, scale=-1.0)
        P.act(E[:, 3, :], pB[2], AF.Exp)
        P.act(DEC[0:DK], pdec[0:DK], AF.Exp)
        if GS == 'cum':
            continue
        P.stt(qt16[:, 0, :], Q, qscale, E[:, 0, :], ALU.mult, ALU.mult)
        P.stt(qt16[:, 1, :], Q, qscale, E[:, 1, :], ALU.mult, ALU.mult)
        P.tt(qt16[:, 2, :], K, E[:, 2, :], ALU.mult)
        P.tt(ks16, K, E[:, 3, :], ALU.mult)
        P.copy(v16, ut[:, vo:vo + 256], 'pool')
        pTa = C.bankbf(5).rearrange("p (a b) -> p a b", a=8)
        pTb = C.bankbf(6).rearrange("p (a b) -> p a b", a=8)
        for wi in range(3):
            for h in range(4):
                idx = wi * 4 + h
                dstp = pTa[0:DK, idx, :] if idx < 8 else pTb[0:DK, idx - 8, :]
                P.tr(dstp, qt16[:, wi, h * DK:(h + 1) * DK], C.ident)
        P.copy(T16[0:DK, 0:8, :], pTa[0:DK, 0:8, :], 'act')
        P.copy(T16[0:DK, 8:12, :], pTb[0:DK, 0:4, :], 'act')
        if GS == 'tr':
            continue
        pA = C.bank(7).rearrange("p (a b) -> p a b", a=4)
        for h in range(4):
            P.mm(pA[:, h, :], T16[0:DK, 8 + h, :], T16[0:DK, 4 + h, :], start=(h == 0), stop=True)
        AT = ATs[t % 2]
        P.copy(Af32, pA, 'act')
        P.asel(AT[0:64, :, 0:64], Af32[0:64, :, 0:64], [[0, 4], [1, 64]], 0, -1, 0.0)
        P.asel(AT[64:128, :, 64:128], Af32[64:128, :, 64:128], [[0, 4], [1, 64]], 0, -1, 0.0)
        if GS == 'A':
            continue
        pU = [C.bank(1)[:, 0:256].rearrange("p (a b) -> p a b", a=4), C.bank(2)[:, 0:256].rearrange("p (a b) -> p a b", a=4)]
        for c in range(2):
            cr = slice(64 * c, 64 * c + 64)
            for h in range(4):
                P.mm(pU[c][0:DK, h, :], ks16[cr, h * DK:(h + 1) * DK], v16[cr, h * 64:(h + 1) * 64], start=(h == 0), stop=True)
        for c in range(2):
            n = 2 * t + c
            P.tt(S32[0:DK], S32[0:DK], DEC[0:DK, :, c:c + 1].to_broadcast([DK, 4, 64]), ALU.mult)
            P.tt(S32[0:DK], S32[0:DK], pU[c][0:DK], ALU.add)
            P.copy(ring[n % 4][0:DK], S32[0:DK], 'pool')
        if GS == 'U':
            continue
        pO = C.bank(3)[:, 0:256].rearrange("p (a b) -> p a b", a=4)
        for h in range(4):
            P.mm(pO[:, h, :], AT[:, h, :], v16[:, h * 64:(h + 1) * 64], start=(h == 0), stop=False)
        for h in range(4):
            for c in range(2):
                n_prev = 2 * t + c - 1
                if n_prev < 0:
                    continue
                P.mm(pO[64 * c:64 * c + 64, h, :], T16[0:DK, h, 64 * c:64 * c + 64], ring[n_prev % 4][0:DK, h, :], start=False, stop=True)
        if GS == 'O':
            continue
        o_sb = o_sbs[t % 2]
        sil = sils[t % 2]
        og = ut[:, ogo:ogo + 256]
        P.copy(o_sb, pO.rearrange("p a b -> p (a b)"), 'act')
        P.tt(sq, o_sb, o_sb, ALU.mult)
        P.reduce(ss4s[t % 2], sq.rearrange("p (a b) -> p a b", a=4))
        P.act(sil, og, AF.Exp, scale=-1.0)
        P.ts(sil, sil, 1.0, None, ALU.add)
        P.recip(sil, sil)
        P.tt(sil, sil, og, ALU.mult)
        P.tt(sil.rearrange("p (a b) -> p a b", a=4), sil.rearrange("p (a b) -> p a b", a=4), gn.unsqueeze(1).to_broadcast([128, 4, 64]), ALU.mult)
    epi_ln(NT - 1)
    epi_fin(NT - 1)
    P.dma('sp', C.MIX[:, mixcol:mixcol + 256].rearrange("(t p) c -> p t c", p=128), MIXC)
    A.reset(m0)


def phase_out(C, L, src, dst):
    P, A, I = C.P, C.A, C.I
    S, NT = C.S, C.NT
    moe = (L % 2 == 1)
    NBLK = min(S, 1024)
    NB5 = NBLK // 512
    TPB = NBLK // 128
    m0 = A.mark()
    wout = A.alloc([8, D], BF16)
    for kc in range(8):
        P.dma('pool', wout[:, kc, :], I['w_out'][L, kc * 128:(kc + 1) * 128, :])
    gbc = A.alloc([D], F32)
    bcast_row(C, gbc, I['norm_ffn'][L], D)
    if moe:
        rt32 = A.alloc([8, 8], F32)
        P.dma('sp', rt32, I['moe_router'][0].rearrange("(kc p) e -> p kc e", p=128))
        lg = A.alloc([8], F32)
        v8 = A.alloc([8], F32)
        ex = A.alloc([8], F32)
        sm = A.alloc([4], F32)
        msk = A.alloc([8], F32)
        comb = A.alloc([8], F32)
        combT = A.alloc([128], F32, parts=8)
    hts = [A.alloc([D], F32) for _ in range(2)]
    mts = [A.alloc([D], BF16) for _ in range(2)]
    mT = A.alloc([8, 128], BF16)
    h1 = A.alloc([D], F32)
    junk = A.alloc([D], BF16)
    c32 = A.alloc([D], F32)
    cT32 = A.alloc([8, 128], F32)
    cT16 = A.alloc([8, 128], BF16)
    ss = A.alloc([1], F32)
    rs = A.alloc([1], F32)
    P.dma('sp', hts[0], src[0:128, :])
    P.dma('sp', mts[0], C.MIX[0:128, :])
    for t in range(NT):
        ht, mt = hts[t % 2], mts[t % 2]
        if t + 1 < NT:
            P.dma('sp', hts[(t + 1) % 2], src[(t + 1) * 128:(t + 2) * 128, :])
            P.dma('sp', mts[(t + 1) % 2], C.MIX[(t + 1) * 128:(t + 2) * 128, :])
        pT = C.bankbf(0).rearrange("p (a b) -> p a b", a=8)
        for c in range(8):
            P.tr(pT[:, c, :], mt[:, c * 128:(c + 1) * 128], C.ident)
        P.copy(mT, pT, 'act')
        for hf in range(2):
            pb = C.bank(1 + hf)
            for kc in range(8):
                P.mm(pb, mT[:, kc, :], wout[:, kc, hf * 512:(hf + 1) * 512], start=(kc == 0), stop=(kc == 7))
            P.tt(h1[:, hf * 512:(hf + 1) * 512], pb, ht[:, hf * 512:(hf + 1) * 512], ALU.add)
        P.dma('sp', dst[t * 128:(t + 1) * 128, :], h1)
        P.act(junk, h1, AF.Square, accum=ss)
        rstd_from_ss(P, rs, ss, D)
        P.stt(c32, h1, rs[:, 0:1], gbc, ALU.mult, ALU.mult)
        for hf in range(2):
            pc = C.bank(3 + hf).rearrange("p (a b) -> p a b", a=4)
            for c in range(4):
                P.tr(pc[:, c, :], c32[:, (hf * 4 + c) * 128:(hf * 4 + c + 1) * 128], C.identf)
            P.copy(cT16[:, hf * 4:(hf + 1) * 4, :], pc, 'act')
            if moe:
                P.copy(cT32[:, hf * 4:(hf + 1) * 4, :], pc, 'dve')
        P.dma('sp', C.CTD[:, :, t * 128:(t + 1) * 128], cT16)
        if moe:
            pl = C.bank(5)[:, 0:8]
            for kc in range(8):
                P.mm(pl, cT32[:, kc, :], rt32[:, kc, :], start=(kc == 0), stop=(kc == 7))
            P.copy(lg, pl, 'dve')
            P.op('dve', lambda e: e.max(out=v8, in_=lg), [lg], [v8])
            P.ts(sm[:, 0:1], v8[:, 0:1], -1.0, None, ALU.mult)
            P.act(ex, lg, AF.Exp, bias=sm[:, 0:1])
            P.act(sm[:, 1:2], v8[:, 1:2], AF.Exp, bias=sm[:, 0:1])
            P.ts(sm[:, 2:3], sm[:, 1:2], 1.0, None, ALU.add)
            P.recip(sm[:, 2:3], sm[:, 2:3])
            P.ts(msk, lg, v8[:, 1:2], None, ALU.is_ge)
            P.stt(comb, ex, sm[:, 2:3], msk, ALU.mult, ALU.mult)
            pcm = C.bank(6)[0:8, 0:128]
            P.tr(pcm, comb, C.identf)
            P.copy(combT, pcm, 'dve')
            P.dma('sp', C.COMB[:, t * 128:(t + 1) * 128], combT)
    A.reset(m0)

    wpg = A.alloc([8, D], BF16)
    for kc in range(8):
        P.dma('pool', wpg[:, kc, :], I['ple_w_gate'][L, kc * 128:(kc + 1) * 128, :])
    wpp = A.alloc([2, D], BF16)
    for j in range(2):
        P.dma('pool', wpp[:, j, :], I['ple_w_proj'][L, j * 128:(j + 1) * 128, :])
    gpT = A.alloc([8], F32)
    P.dma('sp', gpT, I['ple_norm'][L].rearrange("(c p) -> p c", p=128), allow_slow_non_contiguous=True)
    CT = A.alloc([8, NBLK], BF16)
    hidden = A.alloc([28, NBLK], BF16)
    accT = A.alloc([8, NBLK], F32)
    wgs = [A.alloc([8, 256], BF16) for _ in range(2)]
    wus = [A.alloc([8, 256], BF16) for _ in range(2)]
    wds = [A.alloc([14, 128], BF16) for _ in range(2)]
    sg = [A.alloc([512], BF16) for _ in range(2)]
    tmpo = A.alloc([512], F32)
    if moe:
        sel_all = A.alloc([8, 128], F32, parts=8)
        P.memset(sel_all, 1.0)
        P.asel(sel_all, sel_all, [[1, 8], [0, 128]], 0, -1, 0.0, cmp=ALU.is_equal)
        cmbT = A.alloc([NBLK], F32, parts=8)
        cbc = A.alloc([NBLK], F32)
        WG = lambda e: I['moe_w_gate'][0, e]
        WU = lambda e: I['moe_w_up'][0, e]
        WD = lambda e: I['moe_w_down'][0, e]
        NE = 8
    else:
        WG = lambda e: I['ffn_w_gate'][0]
        WU = lambda e: I['ffn_w_up'][0]
        WD = lambda e: I['ffn_w_down'][0]
        NE = 1
    h1t = A.alloc([D], F32)
    h2 = h1t
    hn = A.alloc([D], BF16)
    aT = A.alloc([8, 128], BF16)
    gate = A.alloc([D], F32)
    pt32 = A.alloc([256], F32)
    pt16 = A.alloc([256], BF16)
    ppT = A.alloc([2, 128], BF16)
    junk = gate[:, 0:512].bitcast(BF16)
    ss = A.alloc([1], F32)
    rs = A.alloc([1], F32)

    def load_gu(e, fg, buf):
        wgv = WG(e).rearrange("(kc p) f -> p kc f", p=128)
        wuv = WU(e).rearrange("(kc p) f -> p kc f", p=128)
        P.dma('pool', wgs[buf], wgv[:, :, fg * 256:(fg + 1) * 256])
        P.dma('pool', wus[buf], wuv[:, :, fg * 256:(fg + 1) * 256])

    def load_d(e, dh, buf):
        dc, hf_ = dh // 2, dh % 2
        wdv = WD(e).rearrange("(f p) d -> p f d", p=128)
        for f0 in range(0, 14, 7):
            P.dma('pool', wds[buf][:, f0:f0 + 7, :], wdv[:, hf_ * 14 + f0:hf_ * 14 + f0 + 7, dc * 128:(dc + 1) * 128])

    gi = 0
    di = 0
    for blk in range(S // NBLK):
        T0 = blk * NBLK
        P.dma('sp', CT, C.CTD[:, :, T0:T0 + NBLK])
        if moe:
            P.dma('sp', cmbT, C.COMB[:, T0:T0 + NBLK])
        for e in range(NE):
            if moe:
                for nb in range(NB5):
                    pbc = C.bank(6 + nb % 2)
                    P.mm(pbc, sel_all[0:8, e, :], cmbT[0:8, nb * 512:(nb + 1) * 512])
                    P.copy(cbc[:, nb * 512:(nb + 1) * 512], pbc, 'act')
            load_gu(e, 0, gi % 2)
            for fg in range(14):
                if fg + 1 < 14:
                    load_gu(e, fg + 1, (gi + 1) % 2)
                wg, wu = wgs[gi % 2], wus[gi % 2]
                gi += 1
                for fc in range(2):
                    f = fg * 2 + fc
                    for nb in range(NB5):
                        pg = C.bank(0 + (f * NB5 + nb) % 2)
                        pu = C.bank(2 + (f * NB5 + nb) % 2)
                        for kc in range(8):
                            P.mm(pg, wg[:, kc, fc * 128:(fc + 1) * 128], CT[:, kc, nb * 512:(nb + 1) * 512], start=(kc == 0), stop=(kc == 7))
                        for kc in range(8):
                            P.mm(pu, wu[:, kc, fc * 128:(fc + 1) * 128], CT[:, kc, nb * 512:(nb + 1) * 512], start=(kc == 0), stop=(kc == 7))
                        sgb = sg[(f * NB5 + nb) % 2]
                        P.act(sgb, pg, AF.Silu)
                        P.tt(hidden[:, f, nb * 512:(nb + 1) * 512], sgb, pu, ALU.mult)
            load_d(e, 0, di % 2)
            for dc in range(8):
                pos_ = [C.bank(4 + nb) for nb in range(NB5)]
                for hf_ in range(2):
                    dh = dc * 2 + hf_
                    if dh + 1 < 16:
                        load_d(e, dh + 1, (di + 1) % 2)
                    wd = wds[di % 2]
                    di += 1
                    for nb in range(NB5):
                        for f in range(14):
                            ff = hf_ * 14 + f
                            P.mm(pos_[nb], wd[:, f, :], hidden[:, ff, nb * 512:(nb + 1) * 512], start=(ff == 0), stop=(ff == 27))
                for nb in range(NB5):
                    po = pos_[nb]
                    dstp = accT[:, dc, nb * 512:(nb + 1) * 512]
                    if not moe:
                        P.copy(dstp, po, 'act')
                    elif e == 0:
                        P.tt(dstp, po, cbc[:, nb * 512:(nb + 1) * 512], ALU.mult)
                    else:
                        P.tt(tmpo, po, cbc[:, nb * 512:(nb + 1) * 512], ALU.mult)
                        P.tt(dstp, dstp, tmpo, ALU.add)
        for tl in range(TPB):
            t = blk * TPB + tl
            rowsl = slice(t * 128, (t + 1) * 128)
            P.dma('sp', h1t, dst[rowsl, :])
            P.dma('sp', pt32, I['p'][L, rowsl, :])
            for hf in range(2):
                pc = C.bank(hf).rearrange("p (a b) -> p a b", a=4)
                for c in range(4):
                    P.tr(pc[:, c, :], accT[:, hf * 4 + c, tl * 128:(tl + 1) * 128], C.identf)
                P.tt(h2[:, hf * 512:(hf + 1) * 512], pc.rearrange("p a b -> p (a b)"), h1t[:, hf * 512:(hf + 1) * 512], ALU.add)
            P.act(junk, h2, AF.Square, accum=ss)
            rstd_from_ss(P, rs, ss, D)
            P.ts(hn, h2, rs[:, 0:1], None, ALU.mult)
            pT = C.bankbf(2).rearrange("p (a b) -> p a b", a=8)
            for c in range(8):
                P.tr(pT[:, c, :], hn[:, c * 128:(c + 1) * 128], C.ident)
            P.tt(aT, pT, gpT.unsqueeze(2).to_broadcast([128, 8, 128]), ALU.mult)
            P.copy(pt16, pt32, 'act')
            pP = C.bankbf(3).rearrange("p (a b) -> p a b", a=8)
            for j in range(2):
                P.tr(pP[:, j, :], pt16[:, j * 128:(j + 1) * 128], C.ident)
            P.copy(ppT, pP[:, 0:2, :], 'act')
            for hf in range(2):
                pgt = C.bank(4 + hf)
                for kc in range(8):
                    P.mm(pgt, aT[:, kc, :], wpg[:, kc, hf * 512:(hf + 1) * 512], start=(kc == 0), stop=(kc == 7))
                P.act(gate[:, hf * 512:(hf + 1) * 512], pgt, AF.Sigmoid)
                ppp = C.bank(6 + hf)
                for j in range(2):
                    P.mm(ppp, ppT[:, j, :], wpp[:, j, hf * 512:(hf + 1) * 512], start=(j == 0), stop=(j == 1))
                P.tt(gate[:, hf * 512:(hf + 1) * 512], ppp, gate[:, hf * 512:(hf + 1) * 512], ALU.mult)
            P.tt(h2, h2, gate, ALU.add)
            P.dma('sp', dst[rowsl, :], h2)
    A.reset(m0)


_CACHE = {}


def kernel(**inputs):
    S = 4096
    if 'nc' not in _CACHE:
        _CACHE['nc'] = build(S)
    nc = _CACHE['nc']
    names = [k for k in inputs if k not in ('x', 'p')]
    in_maps = []
    for b in range(8):
        mp = {'x': np.ascontiguousarray(inputs['x'][b], dtype=np.float32),
              'p': np.ascontiguousarray(inputs['p'][:, b], dtype=np.float32)}
        for k in names:
            mp[k] = np.ascontiguousarray(inputs[k], dtype=np.float32)
        in_maps.append(mp)
    res = run_bass_kernel_spmd(nc, in_maps, core_ids=list(range(8)))
    out = np.stack([np.asarray(r["y"], dtype=np.float32) for r in res.results], axis=0)
    return out
```
